# Optimizing a Trainium2 kernel written in Bass

```python
import math
import jax
import jax.numpy as jnp
from jax import lax
import numpy as np

D_MODEL = 2048
BATCH = 2
SEQ = 8192
DEPTH = 2

GRID_W = 64
CTX_LEN = 256
N_BRANCH = 4
BRANCH_WIDTH = D_MODEL // 2

MLA_Q_LORA = D_MODEL // 4
MLA_KV_LORA = D_MODEL // 4
QK_NOPE = 128
QK_ROPE = 64
V_HEAD = 128
MLA_HEADS = BRANCH_WIDTH // V_HEAD
ROPE_BASE = 10000.0
Q_BLOCK = 128

SSM_HEAD_DIM = 64
SSM_HEADS = BRANCH_WIDTH // SSM_HEAD_DIM
SSM_INNER = SSM_HEADS * SSM_HEAD_DIM
SSM_GROUPS = 2
SSM_STATE = 128
SSM_CHUNK = 128
SSM_XBC = SSM_INNER + 2 * SSM_GROUPS * SSM_STATE

HY_WIDTH = BRANCH_WIDTH
HY_EMB = 33
HY_BANDS = (HY_EMB - 1) // 2
HY_FILTER_HIDDEN = 64
HY_TARGET = 1e-2
HY_SLOW_FRAC = 1.5
HY_QUICK_FRAC = 0.3
HY_MIN_DECAY = math.log(HY_TARGET) / HY_SLOW_FRAC
HY_MAX_DECAY = math.log(HY_TARGET) / HY_QUICK_FRAC

RW_HEAD_DIM = 64
RW_WIDTH = BRANCH_WIDTH
RW_HEADS = RW_WIDTH // RW_HEAD_DIM
RW_DECAY_LORA = 64
RW_ICLR_LORA = 64
RW_GATE_LORA = 160
RW_GN_EPS = 64e-5

N_EXPERTS = 16
EXPERT_FF = 1408
EC_CAPACITY = 2

ALPHA = (2 * DEPTH) ** 0.25
BETA = (8 * DEPTH) ** -0.25

MLA_COLS = MLA_Q_LORA + MLA_KV_LORA + QK_ROPE
SSM_COLS = SSM_INNER + SSM_XBC + 2 * SSM_HEADS
HY_COLS = 3 * HY_WIDTH
RW_COLS = 3 * RW_WIDTH + 2 * RW_DECAY_LORA + 2 * RW_ICLR_LORA + RW_GATE_LORA
GATE_COLS = N_BRANCH * D_MODEL
N_IN = MLA_COLS + SSM_COLS + HY_COLS + RW_COLS + GATE_COLS

kernel_name = 'hybrid_mla_ssd_hyena_rwkv7_ecmoe_dit'

F32 = jnp.float32


def layer_norm(x, eps=1e-6):
    xf = x.astype(F32)
    mu = jnp.mean(xf, -1, keepdims=True)
    var = jnp.mean(jnp.square(xf - mu), -1, keepdims=True)
    return (xf - mu) * lax.rsqrt(var + eps)


def rms_norm(x, g, eps=1e-6):
    xf = x.astype(F32)
    return (xf * lax.rsqrt(jnp.mean(xf * xf, -1, keepdims=True) + eps) * g).astype(x.dtype)


def modulate(x, shift, scale):
    return (layer_norm(x) * (1.0 + scale) + shift).astype(x.dtype)


def post_norm(x, y, g, b):
    return (layer_norm(ALPHA * x + y) * g + b).astype(x.dtype)


def dwconv3(u, w, b):
    up = jnp.pad(u, ((0, 0), (1, 1), (0, 0)))
    return up[:, :-2] * w[0] + up[:, 1:-1] * w[1] + up[:, 2:] * w[2] + b


def token_shift(u, mu):
    up = jnp.pad(u, ((0, 0), (1, 1), (0, 0)))
    return u + mu[0] * (up[:, :-2] - u) + mu[1] * (up[:, 2:] - u)


def axial_rope_tables(n_tokens):
    rows = n_tokens // GRID_W
    row = jnp.repeat(jnp.arange(rows), GRID_W).astype(F32)
    col = jnp.tile(jnp.arange(GRID_W), rows).astype(F32)
    half = QK_ROPE // 2
    inv = ROPE_BASE ** (-jnp.arange(0, half, 2, dtype=F32) / half)
    ang = jnp.stack([row[:, None] * inv, col[:, None] * inv], 1)
    return jnp.cos(ang), jnp.sin(ang)


def apply_axial_rope(x, cos, sin):
    xs = x.reshape(x.shape[:-1] + (2, 2, QK_ROPE // 4)).astype(F32)
    x1, x2 = xs[..., 0, :], xs[..., 1, :]
    out = jnp.stack([x1 * cos - x2 * sin, x2 * cos + x1 * sin], -2)
    return out.reshape(x.shape).astype(x.dtype)


def mla_project(u, p, rope):
    b, n, _ = u.shape
    cq, ckv, kpe = jnp.split(u, [MLA_Q_LORA, MLA_Q_LORA + MLA_KV_LORA], -1)
    q = (rms_norm(cq, p['mla_q_norm']) @ p['mla_w_q_up']).reshape(b, n, MLA_HEADS, QK_NOPE + QK_ROPE)
    kv = (rms_norm(ckv, p['mla_kv_norm']) @ p['mla_w_kv_up']).reshape(b, n, MLA_HEADS, QK_NOPE + V_HEAD)
    q_nope, q_pe = jnp.split(q, [QK_NOPE], -1)
    k_nope, v = jnp.split(kv, [QK_NOPE], -1)
    if rope is not None:
        cos, sin = rope
        q_pe = apply_axial_rope(q_pe, cos[:, None], sin[:, None])
        kpe = apply_axial_rope(kpe, cos, sin)
    q = jnp.concatenate([q_nope, q_pe], -1)
    k = jnp.concatenate([k_nope, jnp.broadcast_to(kpe[:, :, None], (b, n, MLA_HEADS, QK_ROPE))], -1)
    return q, k, v


def block_attention(q, k, v):
    b, n, h, dk = q.shape
    scale = dk ** -0.5
    qb = jnp.moveaxis(q.reshape(b, n // Q_BLOCK, Q_BLOCK, h, dk), 1, 0)

    def attend(qi):
        s = jnp.einsum('bqhd,bkhd->bhqk', qi, k).astype(F32) * scale
        pr = jax.nn.softmax(s, -1).astype(v.dtype)
        return jnp.einsum('bhqk,bkhd->bqhd', pr, v)

    o = lax.map(attend, qb)
    return jnp.moveaxis(o, 0, 1).reshape(b, n, h * v.shape[-1])


def mla_branch(uc, ul, rope, p, need_ctx):
    qc, kc, vc = mla_project(uc, p, None)
    ql, kl, vl = mla_project(ul, p, rope)
    yl = block_attention(ql, jnp.concatenate([kc, kl], 1), jnp.concatenate([vc, vl], 1))
    yc = block_attention(qc, kc, vc) if need_ctx else None
    return yc, yl


def segsum(x):
    t = x.shape[-1]
    xr = jnp.broadcast_to(x[..., :, None], x.shape + (t,))
    xr = jnp.where(jnp.tril(jnp.ones((t, t), bool), -1), xr, 0.0)
    cs = jnp.cumsum(xr, axis=-2)
    return jnp.where(jnp.tril(jnp.ones((t, t), bool)), cs, -jnp.inf)


def ssd_chunked(X, dA, Bm, Cm, init):
    b, L, g, j, p_ = X.shape
    c = L // SSM_CHUNK
    X = X.reshape(b, c, SSM_CHUNK, g, j, p_)
    Bm = Bm.reshape(b, c, SSM_CHUNK, g, -1)
    Cm = Cm.reshape(b, c, SSM_CHUNK, g, -1)
    A = jnp.moveaxis(dA.reshape(b, c, SSM_CHUNK, g, j), (1, 2), (3, 4))
    A_cs = jnp.cumsum(A, -1)
    CB = jnp.einsum('bclgn,bcsgn->bgcls', Cm, Bm)
    M = CB[:, :, None] * jnp.exp(segsum(A))
    y_diag = jnp.einsum('bgjcls,bcsgjp->bclgjp', M, X)
    ds = jnp.moveaxis(jnp.exp(A_cs[..., -1:] - A_cs), (3, 4), (1, 2))
    states = jnp.einsum('bclgn,bclgjp->bcgjpn', Bm, X * ds[..., None])
    states = jnp.concatenate([init[:, None], states], 1)
    chunk_A = jnp.pad(A_cs[..., -1], ((0, 0), (0, 0), (0, 0), (1, 0)))
    new_states = jnp.einsum('bgjzc,bcgjpn->bzgjpn', jnp.exp(segsum(chunk_A)), states)
    prev_states, final = new_states[:, :-1], new_states[:, -1]
    sdo = jnp.moveaxis(jnp.exp(A_cs), (3, 4), (1, 2))
    y_off = jnp.einsum('bclgn,bcgjpn->bclgjp', Cm, prev_states) * sdo[..., None]
    return (y_diag + y_off).reshape(b, L, g, j, p_), final


def mamba_prep(u, p):
    b, n, _ = u.shape
    z, xbc, dt = jnp.split(u, [SSM_INNER, SSM_INNER + SSM_XBC], -1)
    xbc = jax.nn.silu(dwconv3(xbc, p['ssm_conv_w'], p['ssm_conv_b']).astype(F32))
    xs, bm, cm = jnp.split(xbc, [SSM_INNER, SSM_INNER + SSM_GROUPS * SSM_STATE], -1)
    xs = xs.reshape(b, n, SSM_HEADS, SSM_HEAD_DIM)
    bm = bm.reshape(b, n, SSM_GROUPS, SSM_STATE)
    cm = cm.reshape(b, n, SSM_GROUPS, SSM_STATE)
    dt = jax.nn.softplus(dt.astype(F32).reshape(b, n, 2, SSM_HEADS) + p['ssm_dt_bias'])
    return z, xs, bm, cm, dt


def ssd_direction(xs, dt, a, bm, cm, init, reverse):
    if reverse:
        xs, dt, bm, cm = (jnp.flip(t, 1) for t in (xs, dt, bm, cm))
    b, n = dt.shape[:2]
    hg = SSM_HEADS // SSM_GROUPS
    X = (xs * dt[..., None]).reshape(b, n, SSM_GROUPS, hg, SSM_HEAD_DIM)
    dA = (dt * a).reshape(b, n, SSM_GROUPS, hg)
    y, final = ssd_chunked(X, dA, bm, cm, init)
    y = y.reshape(b, n, SSM_HEADS, SSM_HEAD_DIM)
    if reverse:
        y = jnp.flip(y, 1)
    return y, final


def mamba_out(y, xs, z, p):
    b, n = y.shape[:2]
    y = (y + xs * p['ssm_d'][:, None]).reshape(b, n, SSM_INNER) * jax.nn.silu(z.astype(F32))
    yg = y.reshape(b, n, SSM_GROUPS, SSM_INNER // SSM_GROUPS)
    yg = yg * lax.rsqrt(jnp.mean(yg * yg, -1, keepdims=True) + 1e-5)
    return (yg.reshape(b, n, SSM_INNER) * p['ssm_norm']).astype(z.dtype)


def mamba_branch(uc, ul, p, need_ctx):
    a = -jnp.exp(p['ssm_a_log'].astype(F32))
    zc, xc, bc, cc, dtc = mamba_prep(uc, p)
    zl, xl, bl, cl, dtl = mamba_prep(ul, p)
    zero = jnp.zeros((ul.shape[0], SSM_GROUPS, SSM_HEADS // SSM_GROUPS, SSM_HEAD_DIM, SSM_STATE), F32)
    yc = yl = 0.0
    for d, rev in enumerate((False, True)):
        yc_d, sc = ssd_direction(xc, dtc[:, :, d], a[d], bc, cc, zero, rev)
        yl_d, _ = ssd_direction(xl, dtl[:, :, d], a[d], bl, cl, sc, rev)
        yc = yc + yc_d
        yl = yl + yl_d
    return (mamba_out(yc, xc, zc, p) if need_ctx else None), mamba_out(yl, xl, zl, p)


def hyena_filters(n, p):
    t = jnp.linspace(0.0, 1.0, n, dtype=F32)[:, None]
    wpos = 2.0 * math.pi * jnp.arange(n, dtype=F32)[:, None] / n
    f = jnp.linspace(1e-4, HY_BANDS - 1, HY_BANDS, dtype=F32)[None]
    z = jnp.concatenate([t, jnp.cos(f * wpos), -jnp.sin(f * wpos)], -1)
    freq = p['hy_freq']
    hdn = jnp.sin(freq * (z @ p['hy_w1'] + p['hy_b1']))
    hdn = jnp.sin(freq * (hdn @ p['hy_w2'] + p['hy_b2']))
    filt = (hdn @ p['hy_w3']).astype(F32).reshape(n, 2, HY_WIDTH)
    deltas = jnp.abs(jnp.linspace(HY_MIN_DECAY, HY_MAX_DECAY, HY_WIDTH, dtype=F32))
    return filt * jnp.exp(-t * deltas)[:, None]


def bidir_long_conv(v, filt):
    n, ch = v.shape[1], v.shape[2]
    k_full = jnp.concatenate([filt[:, 0], jnp.zeros((1, ch), F32), filt[:0:-1, 1]], 0)
    kf = jnp.fft.rfft(k_full, axis=0)
    vf = jnp.fft.rfft(v, n=2 * n, axis=1)
    return jnp.fft.irfft(vf * kf, n=2 * n, axis=1)[:, :n]


def hyena_seq(u, p):
    n = u.shape[1]
    uc = dwconv3(u, p['hy_conv_w'], p['hy_conv_b']).astype(F32)
    x0, x1, v = jnp.split(uc, 3, -1)
    v = v * x1
    y = bidir_long_conv(v, hyena_filters(n, p)) + v * p['hy_d']
    return (y * x0).astype(u.dtype)


def hyena_branch(uc, ul, p, need_ctx):
    return (hyena_seq(uc, p) if need_ctx else None), hyena_seq(ul, p)


def rwkv_prep(u, p):
    b, n, _ = u.shape
    heads = lambda t: t.reshape(b, n, RW_HEADS, RW_HEAD_DIM)
    us = token_shift(u, p['rw_mu']).astype(F32)
    cuts = np.cumsum([RW_WIDTH, RW_WIDTH, RW_WIDTH, 2 * RW_DECAY_LORA, 2 * RW_ICLR_LORA]).tolist()
    r, k, v, wd, ad, gd = jnp.split(us, cuts, -1)
    g = jax.nn.sigmoid(gd) @ p['rw_g_up']
    kk = heads(k * p['rw_kk'])
    kk = kk / jnp.maximum(jnp.sqrt(jnp.sum(kk * kk, -1, keepdims=True)), 1e-12)
    wd = wd.reshape(b, n, 2, RW_DECAY_LORA)
    ad = ad.reshape(b, n, 2, RW_ICLR_LORA)
    dirs = []
    for d in range(2):
        w_log = -jax.nn.softplus(-(p['rw_w0'][d] + jnp.tanh(wd[:, :, d]) @ p['rw_w_up'][d])) - 0.5
        iclr = jax.nn.sigmoid(p['rw_a0'][d] + ad[:, :, d] @ p['rw_a_up'][d])
        k_d = heads(k * (1.0 + (iclr - 1.0) * p['rw_ka']))
        dirs.append((heads(jnp.exp(-jnp.exp(w_log))), k_d, kk * heads(iclr)))
    return heads(r), heads(v), kk, g, dirs


def rwkv_scan(r, decay, k, v, a_vec, b_vec, s0, reverse):
    def step(s, inp):
        r_t, w_t, k_t, v_t, a_t, b_t = inp
        sa = jnp.einsum('bhvk,bhk->bhv', s, a_t)
        s = s * w_t[:, :, None, :] + sa[..., None] * b_t[:, :, None, :] + v_t[..., None] * k_t[:, :, None, :]
        return s, jnp.einsum('bhvk,bhk->bhv', s, r_t)

    xs = tuple(jnp.moveaxis(t, 1, 0) for t in (r, decay, k, v, a_vec, b_vec))
    s, ys = lax.scan(step, s0, xs, reverse=reverse)
    return jnp.moveaxis(ys, 0, 1), s


def rwkv_out(y, r, v, g, dirs, p, dtype):
    b, n = y.shape[:2]
    mu = jnp.mean(y, -1, keepdims=True)
    var = jnp.mean(jnp.square(y - mu), -1, keepdims=True)
    yn = ((y - mu) * lax.rsqrt(var + RW_GN_EPS)).reshape(b, n, RW_WIDTH) * p['rw_ln_g'] + p['rw_ln_b']
    bonus = sum(jnp.sum(r * k_d * p['rw_rk'], -1, keepdims=True) * v for _, k_d, _ in dirs)
    return ((yn + bonus.reshape(b, n, RW_WIDTH)) * g).astype(dtype)


def rwkv_branch(uc, ul, p, need_ctx):
    rc, vc, kkc, gc, dc = rwkv_prep(uc, p)
    rl, vl, kkl, gl, dl = rwkv_prep(ul, p)
    s0 = jnp.zeros((ul.shape[0], RW_HEADS, RW_HEAD_DIM, RW_HEAD_DIM), F32)
    yc = yl = 0.0
    for d, rev in enumerate((False, True)):
        wc_, kc_, bc_ = dc[d]
        yc_d, sc = rwkv_scan(rc, wc_, kc_, vc, -kkc, bc_, s0, rev)
        wl_, kl_, bl_ = dl[d]
        yl_d, _ = rwkv_scan(rl, wl_, kl_, vl, -kkl, bl_, sc, rev)
        yc = yc + yc_d
        yl = yl + yl_d
    out_c = rwkv_out(yc, rc, vc, gc, dc, p, uc.dtype) if need_ctx else None
    return out_c, rwkv_out(yl, rl, vl, gl, dl, p, ul.dtype)


def merge_branches(branches, gate_logits, p):
    b, n, _ = gate_logits.shape
    gates = jax.nn.sigmoid(gate_logits.reshape(b, n, N_BRANCH, D_MODEL))
    merged = sum(gates[:, :, i] * (y @ p['w_branch'][i]) for i, y in enumerate(branches))
    return merged @ p['w_out']


def hybrid_mixer(hc, hl, p, rope, need_ctx):
    cuts = np.cumsum([MLA_COLS, SSM_COLS, HY_COLS, RW_COLS]).tolist()
    pc = jnp.split(hc @ p['w_in'], cuts, -1)
    pl = jnp.split(hl @ p['w_in'], cuts, -1)
    branches = (mla_branch(pc[0], pl[0], rope, p, need_ctx),
                mamba_branch(pc[1], pl[1], p, need_ctx),
                hyena_branch(pc[2], pl[2], p, need_ctx),
                rwkv_branch(pc[3], pl[3], p, need_ctx))
    yl = merge_branches([br[1] for br in branches], pl[4], p)
    yc = merge_branches([br[0] for br in branches], pc[4], p) if need_ctx else None
    return yc, yl


def expert_choice_ffn(h, p):
    b, n, _ = h.shape
    cap = EC_CAPACITY * n // N_EXPERTS
    aff = jax.nn.softmax((h @ p['w_router']).astype(F32), -1)
    gate, idx = lax.top_k(jnp.swapaxes(aff, 1, 2), cap)
    bidx = jnp.arange(b)[:, None, None]
    xs = h[bidx, idx]
    hid = jax.nn.silu(jnp.einsum('becd,edf->becf', xs, p['w_gate_e'])) * jnp.einsum('becd,edf->becf', xs, p['w_up_e'])
    ye = jnp.einsum('becf,efd->becd', hid, p['w_down_e']) * gate[..., None].astype(h.dtype)
    return jnp.zeros_like(h).at[bidx, idx].add(ye.astype(h.dtype))


def trunk_layer(xc, xl, mod_c, mod_l, p, rope, need_ctx):
    sh1c, sc1c, g1c, sh2c, sc2c, g2c = jnp.split(mod_c, 6, -1)
    sh1, sc1, g1, sh2, sc2, g2 = (m[:, None] for m in jnp.split(mod_l, 6, -1))
    yc, yl = hybrid_mixer(modulate(xc, sh1c, sc1c), modulate(xl, sh1, sc1), p, rope, need_ctx)
    xl = post_norm(xl, g1 * yl, p['ln1_g'], p['ln1_b'])
    xl = post_norm(xl, g2 * expert_choice_ffn(modulate(xl, sh2, sc2), p), p['ln2_g'], p['ln2_b'])
    if need_ctx:
        xc = post_norm(xc, g1c * yc, p['ln1_g'], p['ln1_b'])
        xc = post_norm(xc, g2c * expert_choice_ffn(modulate(xc, sh2c, sc2c), p), p['ln2_g'], p['ln2_b'])
    return xc, xl


def setup_inputs(seed: int = 0) -> dict:
    key = jax.random.key(seed)
    keys = iter(jax.random.split(key, 64))
    L = DEPTH

    def normal(shape, scale=1.0):
        return scale * jax.random.normal(next(keys), shape, F32)

    def gain(shape):
        return 1.0 + 0.05 * jax.random.normal(next(keys), shape, F32)

    def uniform(shape, lo, hi):
        return jax.random.uniform(next(keys), shape, F32, lo, hi)

    dt0 = jnp.exp(uniform((L, 2, SSM_HEADS), math.log(1e-3), math.log(1e-1)))
    return {
        'x': normal((BATCH, SEQ, D_MODEL)),
        'c': normal((BATCH, D_MODEL)),
        'ctx': normal((BATCH, CTX_LEN, D_MODEL)),
        'c_ctx': normal((D_MODEL,)),
        'w_ada': normal((L, D_MODEL, 6 * D_MODEL), D_MODEL ** -0.5),
        'b_ada': normal((L, 6 * D_MODEL), 0.01),
        'w_in': normal((L, D_MODEL, N_IN), D_MODEL ** -0.5),
        'mla_q_norm': gain((L, MLA_Q_LORA)),
        'mla_w_q_up': normal((L, MLA_Q_LORA, MLA_HEADS * (QK_NOPE + QK_ROPE)), MLA_Q_LORA ** -0.5),
        'mla_kv_norm': gain((L, MLA_KV_LORA)),
        'mla_w_kv_up': normal((L, MLA_KV_LORA, MLA_HEADS * (QK_NOPE + V_HEAD)), MLA_KV_LORA ** -0.5),
        'ssm_conv_w': normal((L, 3, SSM_XBC), 3 ** -0.5),
        'ssm_conv_b': normal((L, SSM_XBC), 0.01),
        'ssm_dt_bias': dt0 + jnp.log(-jnp.expm1(-dt0)),
        'ssm_a_log': jnp.log(uniform((L, 2, SSM_HEADS), 1.0, 16.0)),
        'ssm_d': gain((L, SSM_HEADS)),
        'ssm_norm': gain((L, SSM_INNER)),
        'hy_conv_w': normal((L, 3, HY_COLS), 3 ** -0.5),
        'hy_conv_b': normal((L, HY_COLS), 0.01),
        'hy_w1': normal((L, HY_EMB, HY_FILTER_HIDDEN), HY_EMB ** -0.5),
        'hy_b1': normal((L, HY_FILTER_HIDDEN), 0.1),
        'hy_w2': normal((L, HY_FILTER_HIDDEN, HY_FILTER_HIDDEN), HY_FILTER_HIDDEN ** -0.5),
        'hy_b2': normal((L, HY_FILTER_HIDDEN), 0.1),
        'hy_w3': normal((L, HY_FILTER_HIDDEN, 2 * HY_WIDTH), 0.02),
        'hy_freq': gain((L, HY_FILTER_HIDDEN)),
        'hy_d': normal((L, HY_WIDTH), 0.1),
        'rw_mu': uniform((L, 2, RW_COLS), 0.0, 0.5),
        'rw_w0': uniform((L, 2, RW_WIDTH), -5.0, -0.5),
        'rw_w_up': normal((L, 2, RW_DECAY_LORA, RW_WIDTH), 0.1),
        'rw_a0': normal((L, 2, RW_WIDTH), 0.1),
        'rw_a_up': normal((L, 2, RW_ICLR_LORA, RW_WIDTH), 0.1),
        'rw_g_up': normal((L, RW_GATE_LORA, RW_WIDTH), RW_GATE_LORA ** -0.5),
        'rw_kk': 0.85 + normal((L, RW_WIDTH), 0.05),
        'rw_ka': gain((L, RW_WIDTH)),
        'rw_rk': normal((L, RW_HEADS, RW_HEAD_DIM), 0.1),
        'rw_ln_g': gain((L, RW_WIDTH)),
        'rw_ln_b': normal((L, RW_WIDTH), 0.01),
        'w_branch': normal((L, N_BRANCH, BRANCH_WIDTH, D_MODEL), BRANCH_WIDTH ** -0.5),
        'w_out': normal((L, D_MODEL, D_MODEL), BETA * D_MODEL ** -0.5),
        'ln1_g': gain((L, D_MODEL)),
        'ln1_b': normal((L, D_MODEL), 0.01),
        'w_router': normal((L, D_MODEL, N_EXPERTS), D_MODEL ** -0.5),
        'w_gate_e': normal((L, N_EXPERTS, D_MODEL, EXPERT_FF), D_MODEL ** -0.5),
        'w_up_e': normal((L, N_EXPERTS, D_MODEL, EXPERT_FF), D_MODEL ** -0.5),
        'w_down_e': normal((L, N_EXPERTS, EXPERT_FF, D_MODEL), BETA * EXPERT_FF ** -0.5),
        'ln2_g': gain((L, D_MODEL)),
        'ln2_b': normal((L, D_MODEL), 0.01),
    }


def reference(x, c, ctx, c_ctx, w_ada, b_ada, w_in, mla_q_norm, mla_w_q_up, mla_kv_norm, mla_w_kv_up,
              ssm_conv_w, ssm_conv_b, ssm_dt_bias, ssm_a_log, ssm_d, ssm_norm,
              hy_conv_w, hy_conv_b, hy_w1, hy_b1, hy_w2, hy_b2, hy_w3, hy_freq, hy_d,
              rw_mu, rw_w0, rw_w_up, rw_a0, rw_a_up, rw_g_up, rw_kk, rw_ka, rw_rk, rw_ln_g, rw_ln_b,
              w_branch, w_out, ln1_g, ln1_b, w_router, w_gate_e, w_up_e, w_down_e, ln2_g, ln2_b):
    stacked = dict(
        w_in=w_in, mla_q_norm=mla_q_norm, mla_w_q_up=mla_w_q_up, mla_kv_norm=mla_kv_norm, mla_w_kv_up=mla_w_kv_up,
        ssm_conv_w=ssm_conv_w, ssm_conv_b=ssm_conv_b, ssm_dt_bias=ssm_dt_bias, ssm_a_log=ssm_a_log,
        ssm_d=ssm_d, ssm_norm=ssm_norm,
        hy_conv_w=hy_conv_w, hy_conv_b=hy_conv_b, hy_w1=hy_w1, hy_b1=hy_b1, hy_w2=hy_w2, hy_b2=hy_b2,
        hy_w3=hy_w3, hy_freq=hy_freq, hy_d=hy_d,
        rw_mu=rw_mu, rw_w0=rw_w0, rw_w_up=rw_w_up, rw_a0=rw_a0, rw_a_up=rw_a_up, rw_g_up=rw_g_up,
        rw_kk=rw_kk, rw_ka=rw_ka, rw_rk=rw_rk, rw_ln_g=rw_ln_g, rw_ln_b=rw_ln_b,
        w_branch=w_branch, w_out=w_out, ln1_g=ln1_g, ln1_b=ln1_b,
        w_router=w_router, w_gate_e=w_gate_e, w_up_e=w_up_e, w_down_e=w_down_e, ln2_g=ln2_g, ln2_b=ln2_b)
    rope = axial_rope_tables(x.shape[1])
    xc, xl = ctx, x
    for i in range(DEPTH):
        p = {name: arr[i] for name, arr in stacked.items()}
        mod_l = jax.nn.silu(c) @ w_ada[i] + b_ada[i]
        mod_c = jax.nn.silu(c_ctx) @ w_ada[i] + b_ada[i]
        xc, xl = trunk_layer(xc, xl, mod_c, mod_l, p, rope, i < DEPTH - 1)
    return xl
```

```python
import math
import numpy as np
from concourse.bass_utils import run_bass_kernel_spmd
import concourse.bass as bass
import concourse.mybir as mybir

F32 = mybir.dt.float32
BF16 = mybir.dt.bfloat16
I32 = mybir.dt.int32
AF = mybir.ActivationFunctionType
ALU = mybir.AluOpType
AX = mybir.AxisListType

COMPUTE = ("pe", "act", "dve", "pool")
NDSEM = 6


class Prog:
    def __init__(self, nc, same_engine_sync=True):
        self.nc = nc
        self.ops = {e: [] for e in ("pe", "act", "dve", "pool", "sp")}
        self.cnt = {e: 0 for e in COMPUTE}
        self.waited = {e: {} for e in self.ops}
        self.res = {}
        self.same = same_engine_sync
        self.stack = []
        self.sems = {}
        self.dma_slots = {}
        self.dma_rr = {}
        self._ctx = []

    def enter(self, cm):
        v = cm.__enter__()
        self._ctx.append(cm)
        return v

    def sb(self, name, shape, dt=F32):
        return self.enter(self.nc.sbuf_tensor(name, list(shape), dt))

    def ps(self, name, shape, dt=F32):
        return self.enter(self.nc.psum_tensor(name, list(shape), dt))

    def setup_sems(self, dma_queues=("sp", "act", "pool")):
        for e in COMPUTE:
            self.sems[e] = self.enter(self.nc.semaphore("s_" + e))
        for q in dma_queues:
            self.dma_slots[q] = [[self.enter(self.nc.semaphore("d_%s%d" % (q, i))), 0] for i in range(NDSEM)]
            self.dma_rr[q] = 0

    def _need(self, eng, tok, waits):
        if tok is None:
            return
        kind, key, val = tok
        if kind == "E":
            if key == eng and (eng == "pe" or not self.same):
                return
            sem = self.sems[key]
        else:
            sem = key
        sid = id(sem)
        if self.waited[eng].get(sid, 0) >= val:
            return
        cur = waits.get(sid)
        if cur is None or cur[1] < val:
            waits[sid] = (sem, val)

    def _deps(self, eng, reads, writes):
        waits = {}
        for r in reads:
            st = self.res.get(r)
            if st:
                self._need(eng, st["w"], waits)
        for w in writes:
            st = self.res.get(w)
            if st:
                self._need(eng, st["w"], waits)
                for t in st["r"].values():
                    self._need(eng, t, waits)
        for sid, (sem, val) in waits.items():
            self.waited[eng][sid] = val
        return list(waits.values())

    def _commit(self, tok, reads, writes):
        for r in reads:
            st = self.res.setdefault(r, {"w": None, "r": {}})
            k = (tok[0], tok[1] if tok[0] == "E" else id(tok[1]))
            old = st["r"].get(k)
            if old is None or old[2] < tok[2]:
                st["r"][k] = tok
        for w in writes:
            self.res[w] = {"w": tok, "r": {}}

    def op(self, eng, fn, reads=(), writes=()):
        waits = self._deps(eng, reads, writes)
        self.cnt[eng] += 1
        n = self.cnt[eng]
        sem = self.sems[eng]
        self.ops[eng].append((waits, fn, sem, 1))
        self._commit(("E", eng, n), reads, writes)

    def dma(self, q, fn, reads=(), writes=()):
        waits = self._deps(q, reads, writes)
        slots = self.dma_slots[q]
        i = self.dma_rr[q]
        self.dma_rr[q] = (i + 1) % len(slots)
        sem, c = slots[i]
        if c > 0 and self.waited[q].get(id(sem), 0) < c:
            waits.append((sem, c))
            self.waited[q][id(sem)] = c
        slots[i][1] = c + 16
        self.ops[q].append((waits, fn, sem, 16))
        if q in COMPUTE:
            pass
        self._commit(("D", sem, c + 16), reads, writes)

    def finish_wait(self, eng, resources):
        waits = {}
        for r in resources:
            st = self.res.get(r)
            if st:
                self._need(eng, st["w"], waits)
        self.ops[eng].append((list(waits.values()), None, None, 0))

    def emit(self):
        nc = self.nc
        engmap = {"pe": "tensor", "act": "scalar", "dve": "vector", "pool": "gpsimd", "sp": "sync"}
        with nc.Block() as block:
            for e, name in engmap.items():
                ops = self.ops[e]

                def body(engine, ops=ops):
                    for waits, fn, sem, inc in ops:
                        for s, v in waits:
                            engine.wait_ge(s, v)
                        if fn is not None:
                            ins = fn(engine)
                            ins.then_inc(sem, inc)

                getattr(block, name)(body)
        for cm in reversed(self._ctx):
            cm.__exit__(None, None, None)
        self._ctx = []


D = 2048
KC = D // 128
EPS_LN = 1e-6


def new_nc():
    return bass.Bass("TRN2", target_bir_lowering=False)


def consts(P):
    nc = P.nc
    ones = P.sb("c_ones", [128, 128])
    P.op("pool", lambda e: e.memset(ones[:], 1.0), writes=["c_ones"])
    eps = P.sb("c_eps", [128, 4])
    P.op("pool", lambda e: e.memset(eps[:, 0:1], EPS_LN), writes=["c_eps0"])
    P.op("pool", lambda e: e.memset(eps[:, 1:2], 1e-5), writes=["c_eps1"])
    P.op("pool", lambda e: e.memset(eps[:, 2:3], 64e-5), writes=["c_eps2"])
    P.op("pool", lambda e: e.memset(eps[:, 3:4], 1e-24), writes=["c_eps3"])
    return {"ones": ones, "eps_ln": (eps[:, 0:1], "c_eps0"), "eps_1e5": (eps[:, 1:2], "c_eps1"), "eps_gn": (eps[:, 2:3], "c_eps2"), "eps_tiny": (eps[:, 3:4], "c_eps3")}


def rsqrt_op(P, C, out_ap, out_name, in_ap, in_name, epskey, scale=1.0):
    eps_ap, eps_name = C[epskey]
    np_ = out_ap.shape[0]
    P.op("act", lambda e: e.activation(out=out_ap, in_=in_ap, func=AF.Sqrt, bias=eps_ap[:np_], scale=scale), reads=[in_name, eps_name], writes=[out_name])
    P.op("dve", lambda e: e.reciprocal(out=out_ap, in_=out_ap), reads=[out_name], writes=[out_name])


def build_stageA(ncols):
    nc = new_nc()
    cT = nc.dram_tensor("cT", [D, 3], F32, kind="ExternalInput").ap()
    wA = nc.dram_tensor("wA", [D, ncols], F32, kind="ExternalInput").ap()
    bA = nc.dram_tensor("bA", [128, ncols // 128], F32, kind="ExternalInput").ap()
    out = nc.dram_tensor("modT", [128, ncols // 128, 3], F32, kind="ExternalOutput").ap()
    P = Prog(nc)
    P.setup_sems()
    NT = ncols // 128
    cs = P.sb("cs", [128, KC, 3])
    ss = P.sb("ss", [128, KC, 3])
    bs = P.sb("bs", [128, NT])
    res = P.sb("res", [128, NT, 3])
    wt = [P.sb("wt%d" % i, [128, KC, 128]) for i in range(2)]
    ps = [P.ps("ps%d" % i, [128, 3]) for i in range(2)]
    P.dma("sp", lambda e: e.dma_start(out=cs[:], in_=cT.rearrange("(c p) n -> p c n", p=128)), writes=["cs"])
    P.dma("sp", lambda e: e.dma_start(out=bs[:], in_=bA), writes=["bs"])
    P.op("act", lambda e: e.activation(out=ss[:], in_=cs[:], func=AF.Silu), reads=["cs"], writes=["ss"])
    wv = wA.rearrange("(c p) n -> p c n", p=128)
    for j in range(NT):
        b = j % 2
        P.dma("sp" if b == 0 else "act", lambda e, j=j, b=b: e.dma_start(out=wt[b][:], in_=wv[:, :, j * 128:(j + 1) * 128]), writes=["wt%d" % b])
        for c in range(KC):
            P.op("pe", lambda e, c=c, b=b: e.matmul(ps[b][:], lhsT=wt[b][:, c, :], rhs=ss[:, c, :], start=(c == 0), stop=(c == KC - 1)),
                 reads=["wt%d" % b, "ss"], writes=["ps%d" % b])
        P.op("dve", lambda e, j=j, b=b: e.tensor_scalar(out=res[:, j, :], in0=ps[b][:], scalar1=bs[:, j:j + 1], scalar2=None, op0=ALU.add),
             reads=["ps%d" % b, "bs"], writes=[("res", j)])
    P.dma("sp", lambda e: e.dma_start(out=out, in_=res[:]), reads=[("res", j) for j in range(NT)], writes=["out"])
    P.finish_wait("sp", ["out"])
    P.emit()
    return nc


def ln_modulate_tile(P, C, xs, xname, hs, hname, Tt, modsb, seg, tmp, pst, alpha_res=None):
    ones = C["ones"]
    sq, mean, var, rstd = tmp["sq"], tmp["mean"], tmp["var"], tmp["rstd"]
    P.op("act", lambda e: e.activation(out=sq[:, :, :Tt], in_=xs[:, :, :Tt], func=AF.Square), reads=[xname], writes=["sq"])
    for c in range(KC):
        P.op("pe", lambda e, c=c: e.matmul(pst[0][:, :Tt], lhsT=ones[:], rhs=xs[:, c, :Tt], start=(c == 0), stop=(c == KC - 1)),
             reads=[xname, "c_ones"], writes=["pst0"])
    for c in range(KC):
        P.op("pe", lambda e, c=c: e.matmul(pst[1][:, :Tt], lhsT=ones[:], rhs=sq[:, c, :Tt], start=(c == 0), stop=(c == KC - 1)),
             reads=["sq", "c_ones"], writes=["pst1"])
    P.op("act", lambda e: e.mul(out=mean[:, :Tt], in_=pst[0][:, :Tt], mul=1.0 / D), reads=["pst0"], writes=["mean"])
    P.op("dve", lambda e: e.tensor_tensor(out=var[:, :Tt], in0=mean[:, :Tt], in1=mean[:, :Tt], op=ALU.mult), reads=["mean"], writes=["var"])
    P.op("dve", lambda e: e.scalar_tensor_tensor(out=var[:, :Tt], in0=pst[1][:, :Tt], scalar=1.0 / D, in1=var[:, :Tt], op0=ALU.mult, op1=ALU.subtract),
         reads=["pst1", "var"], writes=["var"])
    rsqrt_op(P, C, rstd[:, :Tt], "rstd", var[:, :Tt], "var", "eps_ln")
    for c in range(KC):
        P.op("dve", lambda e, c=c: e.tensor_tensor(out=hs[:, c, :Tt], in0=xs[:, c, :Tt], in1=mean[:, :Tt], op=ALU.subtract),
             reads=[xname, "mean"], writes=[(hname, c)])
        P.op("pool", lambda e, c=c: e.tensor_tensor(out=hs[:, c, :Tt], in0=hs[:, c, :Tt], in1=rstd[:, :Tt], op=ALU.mult),
             reads=[(hname, c), "rstd"], writes=[(hname, c)])
        if modsb is not None:
            P.op("act", lambda e, c=c: e.activation(out=hs[:, c, :Tt], in_=hs[:, c, :Tt], func=AF.Identity,
                                                    bias=modsb[:, c, seg, 0:1], scale=modsb[:, c, seg, 1:2]),
                 reads=[(hname, c), "modsb"], writes=[(hname, c)])


def ln_tmp(P):
    tmp = {"sq": P.sb("sq", [128, KC, 512]), "mean": P.sb("mean", [128, 512]), "var": P.sb("var", [128, 512]), "rstd": P.sb("rstd", [128, 512])}
    pst = [P.ps("pst0", [128, 512]), P.ps("pst1", [128, 512])]
    return tmp, pst


def token_tiles(segs, tmax=512):
    out = []
    for (s, n, sid) in segs:
        o = 0
        while o < n:
            tt = min(tmax, n - o)
            out.append((s + o, tt, sid))
            o += tt
    return out


def build_stageB(Tc, segs, NB):
    nc = new_nc()
    nseg = len(segs)
    xT = nc.dram_tensor("xT", [D, Tc], F32, kind="ExternalInput").ap()
    modB = nc.dram_tensor("modB", [128, KC, nseg, 2], F32, kind="ExternalInput").ap()
    Wb = nc.dram_tensor("Wb", [D, NB], F32, kind="ExternalInput").ap()
    uT = nc.dram_tensor("uT", [NB, Tc], F32, kind="ExternalOutput").ap()
    P = Prog(nc)
    P.setup_sems()
    C = consts(P)
    tmp, pst = ln_tmp(P)
    modsb = P.sb("modsb", [128, KC, nseg, 2])
    xs = P.sb("xs", [128, KC, 512])
    hs = P.sb("hs", [128, KC, 512])
    wt = [P.sb("wt%d" % i, [128, KC, 128]) for i in range(3)]
    ys = [P.sb("ys%d" % i, [128, 512]) for i in range(2)]
    psg = [P.ps("psg%d" % i, [128, 512]) for i in range(2)]
    P.dma("sp", lambda e: e.dma_start(out=modsb[:], in_=modB), writes=["modsb"])
    P.op("dve", lambda e: e.tensor_scalar(out=modsb[:, :, :, 1:2], in0=modsb[:, :, :, 1:2], scalar1=1.0, scalar2=None, op0=ALU.add), reads=["modsb"], writes=["modsb"])
    xv = xT.rearrange("(c p) t -> p c t", p=128)
    wv = Wb.rearrange("(c p) n -> p c n", p=128)
    outs = []
    it = 0
    for (t0, Tt, sid) in token_tiles(segs):
        P.dma("sp", lambda e, t0=t0, Tt=Tt: e.dma_start(out=xs[:, :, :Tt], in_=xv[:, :, t0:t0 + Tt]), writes=["xs"])
        ln_modulate_tile(P, C, xs, "xs", hs, "hs", Tt, modsb, sid, tmp, pst)
        hreads = [("hs", c) for c in range(KC)]
        for j in range(NB // 128):
            b3 = it % 3
            b = it % 2
            it += 1
            P.dma("act" if it % 2 else "pool", lambda e, j=j, b3=b3: e.dma_start(out=wt[b3][:], in_=wv[:, :, j * 128:(j + 1) * 128]), writes=["wt%d" % b3])
            for c in range(KC):
                P.op("pe", lambda e, c=c, b=b, b3=b3, Tt=Tt: e.matmul(psg[b][:, :Tt], lhsT=wt[b3][:, c, :], rhs=hs[:, c, :Tt], start=(c == 0), stop=(c == KC - 1)),
                     reads=["wt%d" % b3, ("hs", c)], writes=["psg%d" % b])
            if b == 0:
                P.op("dve", lambda e, b=b, Tt=Tt: e.tensor_copy(out=ys[b][:, :Tt], in_=psg[b][:, :Tt]), reads=["psg%d" % b], writes=["ys%d" % b])
            else:
                P.op("act", lambda e, b=b, Tt=Tt: e.copy(out=ys[b][:, :Tt], in_=psg[b][:, :Tt]), reads=["psg%d" % b], writes=["ys%d" % b])
            key = ("uT", j, t0)
            outs.append(key)
            P.dma("sp", lambda e, j=j, b=b, t0=t0, Tt=Tt: e.dma_start(out=uT[j * 128:(j + 1) * 128, t0:t0 + Tt], in_=ys[b][:, :Tt]), reads=["ys%d" % b], writes=[key])
    P.finish_wait("sp", outs)
    P.emit()
    return nc


def build_mla(S, need_ctx):
    nc = new_nc()
    TOT = 2 * S + 512
    dt_in = lambda name, shp: nc.dram_tensor(name, shp, F32, kind="ExternalInput").ap()
    cqT = dt_in("cqT", [512, TOT]); ckvT = dt_in("ckvT", [512, TOT]); kpeT = dt_in("kpeT", [64, TOT]); kpeswT = dt_in("kpeswT", [64, TOT])
    wq = dt_in("wq", [512, 256]); wkv = dt_in("wkv", [512, 256]); gq = dt_in("gq", [128, 4]); gkv = dt_in("gkv", [128, 4])
    cosT = dt_in("cosT", [64, S]); sinT = dt_in("sinT", [64, S])
    o = nc.dram_tensor("o", [TOT, 128], F32, kind="ExternalOutput").ap()
    QT = nc.dram_tensor("QT", [192, TOT], F32, kind="Internal").ap()
    KT = nc.dram_tensor("KT", [192, TOT], F32, kind="Internal").ap()
    V = nc.dram_tensor("V", [TOT, 128], F32, kind="Internal").ap()
    P = Prog(nc)
    P.setup_sems()
    C = consts(P)
    ones = C["ones"]
    wqs = P.sb("wqs", [128, 4, 256]); wkvs = P.sb("wkvs", [128, 4, 256]); gqs = P.sb("gqs", [128, 4]); gkvs = P.sb("gkvs", [128, 4])
    P.dma("sp", lambda e: e.dma_start(out=wqs[:], in_=wq.rearrange("(c p) n -> p c n", p=128)), writes=["wqs"])
    P.dma("sp", lambda e: e.dma_start(out=wkvs[:], in_=wkv.rearrange("(c p) n -> p c n", p=128)), writes=["wkvs"])
    P.dma("sp", lambda e: e.dma_start(out=gqs[:], in_=gq), writes=["gqs"])
    P.dma("sp", lambda e: e.dma_start(out=gkvs[:], in_=gkv), writes=["gkvs"])
    cs = P.sb("cs", [128, 4, 512]); sq = P.sb("sq", [128, 4, 512]); rstd = P.sb("rstd", [128, 512])
    pes = P.sb("pes", [64, 2, 512]); tabs = P.sb("tabs", [64, 2, 512]); rt = P.sb("rt", [64, 2, 512])
    ev = P.sb("ev", [128, 512])
    pA = P.ps("pA", [128, 512]); pB = P.ps("pB", [128, 512])
    segs = [(0, S, 0), (S, S, 0), (2 * S, 256, 1), (2 * S + 256, 256, 1)]

    def rms_tile(src, gsb, gname, t0, Tt):
        P.dma("sp", lambda e: e.dma_start(out=cs[:, :, :Tt], in_=src.rearrange("(c p) t -> p c t", p=128)[:, :, t0:t0 + Tt]), writes=["cs"])
        P.op("act", lambda e: e.activation(out=sq[:, :, :Tt], in_=cs[:, :, :Tt], func=AF.Square), reads=["cs"], writes=["sq"])
        for c in range(4):
            P.op("pe", lambda e, c=c: e.matmul(pA[:, :Tt], lhsT=ones[:], rhs=sq[:, c, :Tt], start=(c == 0), stop=(c == 3)), reads=["sq", "c_ones"], writes=["pA"])
        rsqrt_op(P, C, rstd[:, :Tt], "rstd", pA[:, :Tt], "pA", "eps_ln", scale=1.0 / 512)
        for c in range(4):
            P.op("dve", lambda e, c=c: e.scalar_tensor_tensor(out=cs[:, c, :Tt], in0=cs[:, c, :Tt], scalar=gsb[:, c:c + 1], in1=rstd[:, :Tt], op0=ALU.mult, op1=ALU.mult),
                 reads=["cs", "rstd", gname], writes=["cs"])

    def proj_fm(ws, wname, c0, ncol, dst, dstrow, t0, Tt, rope_src=None):
        for c in range(4):
            P.op("pe", lambda e, c=c: e.matmul(pB[:ncol, :Tt], lhsT=ws[:, c, c0:c0 + ncol], rhs=cs[:, c, :Tt], start=(c == 0), stop=(c == 3)), reads=[wname, "cs"], writes=["pB"])
        P.op("act", lambda e: e.copy(out=ev[:ncol, :Tt], in_=pB[:ncol, :Tt]), reads=["pB"], writes=["ev"])
        P.dma("sp", lambda e: e.dma_start(out=dst[dstrow:dstrow + ncol, t0:t0 + Tt], in_=ev[:ncol, :Tt]), reads=["ev"], writes=[("scr", dstrow, t0)])

    def rope_store(dst, t0, Tt, sid, tl):
        if sid == 0:
            P.dma("act", lambda e: e.dma_start(out=tabs[:, 0, :Tt], in_=cosT[:, tl:tl + Tt]), writes=["tabs0"])
            P.dma("act", lambda e: e.dma_start(out=tabs[:, 1, :Tt], in_=sinT[:, tl:tl + Tt]), writes=["tabs1"])
            P.op("dve", lambda e: e.tensor_tensor(out=rt[:, 0, :Tt], in0=pes[:, 0, :Tt], in1=tabs[:, 0, :Tt], op=ALU.mult), reads=["pes", "tabs0"], writes=["rt0"])
            P.op("pool", lambda e: e.tensor_tensor(out=rt[:, 1, :Tt], in0=pes[:, 1, :Tt], in1=tabs[:, 1, :Tt], op=ALU.mult), reads=["pes", "tabs1"], writes=["rt1"])
            P.op("dve", lambda e: e.tensor_tensor(out=rt[:, 0, :Tt], in0=rt[:, 0, :Tt], in1=rt[:, 1, :Tt], op=ALU.add), reads=["rt0", "rt1"], writes=["rt0"])
            P.dma("sp", lambda e: e.dma_start(out=dst[128:192, t0:t0 + Tt], in_=rt[:, 0, :Tt]), reads=["rt0"], writes=[("scr", 128, t0)])
        else:
            P.dma("sp", lambda e: e.dma_start(out=dst[128:192, t0:t0 + Tt], in_=pes[:, 0, :Tt]), reads=["pes"], writes=[("scr", 128, t0)])

    def tile_body(t0, Tt, sid):
        tl = t0 % S if sid == 0 else 0
        rms_tile(cqT, gqs, "gqs", t0, Tt)
        proj_fm(wqs, "wqs", 0, 128, QT, 0, t0, Tt)
        for half in range(2):
            for c in range(4):
                P.op("pe", lambda e, c=c, half=half: e.matmul(pB[:64, :Tt], lhsT=wqs[:, c, 128 + 64 * half:192 + 64 * half], rhs=cs[:, c, :Tt], start=(c == 0), stop=(c == 3)),
                     reads=["wqs", "cs"], writes=["pB"])
            P.op("act", lambda e, half=half: e.copy(out=pes[:, half, :Tt], in_=pB[:64, :Tt]), reads=["pB"], writes=["pes"])
        rope_store(QT, t0, Tt, sid, tl)
        rms_tile(ckvT, gkvs, "gkvs", t0, Tt)
        proj_fm(wkvs, "wkvs", 0, 128, KT, 0, t0, Tt)
        for s0 in range(0, Tt, 128):
            for c in range(4):
                P.op("pe", lambda e, c=c, s0=s0: e.matmul(pB[:, :128], lhsT=cs[:, c, s0:s0 + 128], rhs=wkvs[:, c, 128:256], start=(c == 0), stop=(c == 3)), reads=["wkvs", "cs"], writes=["pB"])
            P.op("act", lambda e: e.copy(out=ev[:, :128], in_=pB[:, :128]), reads=["pB"], writes=["ev"])
            P.dma("sp", lambda e, s0=s0: e.dma_start(out=V[t0 + s0:t0 + s0 + 128, :], in_=ev[:, :128]), reads=["ev"], writes=[("scrV", t0 + s0)])
        P.dma("act", lambda e: e.dma_start(out=pes[:, 0, :Tt], in_=kpeT[:, t0:t0 + Tt]), writes=["pes"])
        P.dma("act", lambda e: e.dma_start(out=pes[:, 1, :Tt], in_=kpeswT[:, t0:t0 + Tt]), writes=["pes"])
        rope_store(KT, t0, Tt, sid, tl)

    for (t0_, Tt_, sid_) in token_tiles(segs):
        tile_body(t0_, Tt_, sid_)

    Sb = S + 256
    NKT = Sb // 128
    kn = P.sb("kn", [128, Sb]); kp = P.sb("kp", [64, Sb]); v1 = P.sb("v1", [128, NKT, 129])
    qn = P.sb("qn", [128, 512]); qp = P.sb("qp", [64, 512])
    pT = [P.sb("pT%d" % i, [128, 512]) for i in range(2)]
    pS = [pA, pB]
    pO = [P.ps("pO%d" % j, [128, 129]) for j in range(4)]
    osb = P.sb("osb", [128, 129]); rinv = P.sb("rinv", [128, 1])
    scale = 192 ** -0.5
    allscr = [k for k in P.res.keys() if isinstance(k, tuple) and k[0] in ("scr", "scrV")]
    outs = []
    def batch_body(b):
        cbase = 2 * S + 256 * b
        lbase = S * b
        P.op("pool", lambda e: e.memset(v1[:, :, 128:129], 1.0), reads=[], writes=["v1"])
        P.dma("sp", lambda e: e.dma_start(out=kn[:, 0:256], in_=KT[0:128, cbase:cbase + 256]), reads=allscr, writes=["kn"])
        P.dma("sp", lambda e: e.dma_start(out=kn[:, 256:Sb], in_=KT[0:128, lbase:lbase + S]), reads=allscr, writes=["kn"])
        P.dma("act", lambda e: e.dma_start(out=kp[:, 0:256], in_=KT[128:192, cbase:cbase + 256]), reads=allscr, writes=["kp"])
        P.dma("act", lambda e: e.dma_start(out=kp[:, 256:Sb], in_=KT[128:192, lbase:lbase + S]), reads=allscr, writes=["kp"])
        P.dma("pool", lambda e: e.dma_start(out=v1[:, 0:2, 0:128], in_=V[cbase:cbase + 256, :].rearrange("(j p) d -> p j d", p=128)), reads=allscr, writes=["v1"])
        P.dma("pool", lambda e: e.dma_start(out=v1[:, 2:NKT, 0:128], in_=V[lbase:lbase + S, :].rearrange("(j p) d -> p j d", p=128)), reads=allscr, writes=["v1"])
        qsets = [(lbase, S, NKT)]
        if need_ctx:
            qsets.append((cbase, 256, 2))
        for (q0, qn_tot, nkt) in qsets:
            for qq in range(0, qn_tot, 512):
                Tq = min(512, qn_tot - qq)
                P.dma("sp", lambda e, qq=qq, Tq=Tq, q0=q0: e.dma_start(out=qn[:, :Tq], in_=QT[0:128, q0 + qq:q0 + qq + Tq]), reads=allscr, writes=["qn"])
                P.dma("act", lambda e, qq=qq, Tq=Tq, q0=q0: e.dma_start(out=qp[:, :Tq], in_=QT[128:192, q0 + qq:q0 + qq + Tq]), reads=allscr, writes=["qp"])
                for kt in range(nkt):
                    bb = kt % 2
                    P.op("pe", lambda e, kt=kt, bb=bb, Tq=Tq: e.matmul(pS[bb][:, :Tq], lhsT=kn[:, kt * 128:(kt + 1) * 128], rhs=qn[:, :Tq], start=True, stop=False), reads=["kn", "qn"], writes=["pS%d" % bb])
                    P.op("pe", lambda e, kt=kt, bb=bb, Tq=Tq: e.matmul(pS[bb][:, :Tq], lhsT=kp[:, kt * 128:(kt + 1) * 128], rhs=qp[:, :Tq], start=False, stop=True), reads=["kp", "qp"], writes=["pS%d" % bb])
                    P.op("act", lambda e, bb=bb, Tq=Tq: e.activation(out=pT[bb][:, :Tq], in_=pS[bb][:, :Tq], func=AF.Exp, scale=scale), reads=["pS%d" % bb], writes=["pT%d" % bb])
                    for j in range(Tq // 128):
                        P.op("pe", lambda e, kt=kt, bb=bb, j=j, nkt=nkt: e.matmul(pO[j][:], lhsT=pT[bb][:, j * 128:(j + 1) * 128], rhs=v1[:, kt, :], start=(kt == 0), stop=(kt == nkt - 1)),
                             reads=["pT%d" % bb, "v1"], writes=["pO%d" % j])
                for j in range(Tq // 128):
                    P.op("dve", lambda e, j=j: e.reciprocal(out=rinv[:], in_=pO[j][:, 128:129]), reads=["pO%d" % j], writes=["rinv"])
                    P.op("dve", lambda e, j=j: e.tensor_scalar(out=osb[:, :128], in0=pO[j][:, :128], scalar1=rinv[:, 0:1], scalar2=None, op0=ALU.mult), reads=["pO%d" % j, "rinv"], writes=["osb"])
                    key = ("o", q0 + qq + j * 128)
                    outs.append(key)
                    P.dma("sp", lambda e, j=j, q0=q0, qq=qq: e.dma_start(out=o[q0 + qq + j * 128:q0 + qq + (j + 1) * 128, :], in_=osb[:, :128]), reads=["osb"], writes=[key])
    for b_ in range(2):
        batch_body(b_)
    P.finish_wait("sp", outs)
    P.emit()
    return nc


def build_rwkv(S, TC=8):
    nc = new_nc()
    TOT = 2 * S + 512
    Sb = S + 256
    din = lambda name, shp: nc.dram_tensor(name, shp, F32, kind="ExternalInput").ap()
    uR = din("uR", [800, TOT]); mu = din("mu", [128, 7, 2]); pc = din("pc", [128, 16])
    wup = din("wup", [128, 128]); aup = din("aup", [128, 128]); gup = din("gup", [160, 128])
    ident_d = din("ident", [128, 128]); blk_d = din("blk", [128, 128])
    yT = nc.dram_tensor("yT", [128, TOT], F32, kind="ExternalOutput").ap()
    scr = lambda name, shp: nc.dram_tensor(name, shp, F32, kind="Internal").ap()
    fm = {n: scr("fm_" + n, [128, TOT]) for n in ("w0", "w1", "r", "v", "g", "ks")}
    tm = {n: scr("tm_" + n, [TOT, 128]) for n in ("a", "b0", "b1", "k0", "k1", "v")}
    yd = [scr("yd%d" % d, [128, TOT]) for d in range(2)]
    P = Prog(nc)
    P.setup_sems()
    C = consts(P)
    ident = P.sb("ident_s", [128, 128]); blk = P.sb("blk_s", [128, 128])
    mus = P.sb("mus", [128, 7, 3]); pcs = P.sb("pcs", [128, 16])
    wups = P.sb("wups", [128, 128]); aups = P.sb("aups", [128, 128]); gups0 = P.sb("gups0", [128, 128]); gups1 = P.sb("gups1", [32, 128])
    P.dma("sp", lambda e: e.dma_start(out=ident[:], in_=ident_d), writes=["ident"])
    P.dma("sp", lambda e: e.dma_start(out=blk[:], in_=blk_d), writes=["blk"])
    P.dma("sp", lambda e: e.dma_start(out=mus[:, :, 0:2], in_=mu), writes=["mus"])
    P.dma("sp", lambda e: e.dma_start(out=pcs[:], in_=pc), writes=["pcs"])
    P.dma("act", lambda e: e.dma_start(out=wups[:], in_=wup), writes=["wups"])
    P.dma("act", lambda e: e.dma_start(out=aups[:], in_=aup), writes=["aups"])
    P.dma("act", lambda e: e.dma_start(out=gups0[:], in_=gup[0:128, :]), writes=["gups0"])
    P.dma("act", lambda e: e.dma_start(out=gups1[:], in_=gup[128:160, :]), writes=["gups1"])
    P.op("dve", lambda e: e.tensor_tensor(out=mus[:, :, 2:3], in0=mus[:, :, 0:1], in1=mus[:, :, 1:2], op=ALU.add), reads=["mus"], writes=["mus"])
    P.op("dve", lambda e: e.tensor_scalar(out=mus[:, :, 2:3], in0=mus[:, :, 2:3], scalar1=-1.0, scalar2=1.0, op0=ALU.mult, op1=ALU.add), reads=["mus"], writes=["mus"])
    P.op("dve", lambda e: e.tensor_scalar(out=pcs[:, 9:10], in0=pcs[:, 1:2], scalar1=-1.0, scalar2=1.0, op0=ALU.mult, op1=ALU.add), reads=["pcs"], writes=["pcs"])
    KKW, KA, RK, LNG, LNB, W0, A0, OMKA = 0, 1, 2, 3, 4, 5, 7, 9

    TT = 512
    raw = P.sb("raw", [128, TT + 2]); us = [P.sb("us%d" % j, [128, TT]) for j in range(7)]
    t1 = P.sb("t1", [128, TT]); t2 = P.sb("t2", [128, TT]); t3 = P.sb("t3", [128, TT]); kkn = P.sb("kkn", [128, TT]); ksum = P.sb("ksum", [128, TT])
    tr = P.sb("tr", [128, 128])
    pA = P.ps("pA", [128, 512]); pB = P.ps("pB", [128, 512]); pTr = P.ps("pTr", [128, 128])
    segs = [(0, S, 0), (S, S, 0), (2 * S, 256, 1), (2 * S + 256, 256, 1)]
    scr_keys = []

    def store_fm(name, src, sname, t0, Tt):
        key = ("fm", name, t0); scr_keys.append(key)
        P.dma("sp", lambda e: e.dma_start(out=fm[name][:, t0:t0 + Tt], in_=src[:, :Tt]), reads=[sname], writes=[key])

    def store_tm(name, src, sname, t0, Tt):
        for s0 in range(0, Tt, 128):
            P.op("pe", lambda e, s0=s0: e.transpose(out=pTr[:], in_=src[:, s0:s0 + 128], identity=ident[:]), reads=[sname, "ident"], writes=["pTr"])
            P.op("act", lambda e: e.copy(out=tr[:], in_=pTr[:]), reads=["pTr"], writes=["tr"])
            key = ("tm", name, t0 + s0); scr_keys.append(key)
            P.dma("sp", lambda e, s0=s0: e.dma_start(out=tm[name][t0 + s0:t0 + s0 + 128, :], in_=tr[:]), reads=["tr"], writes=[key])

    def prep_tile(seg0, segn, t0, Tt):
        for j in range(7):
            rows = 128 if j < 6 else 32
            r0 = j * 128
            lo = max(seg0, t0 - 1); hi = min(seg0 + segn, t0 + Tt + 1)
            P.op("pool", lambda e: e.memset(raw[:, :], 0.0), writes=["raw"])
            P.dma("sp" if j % 2 else "act", lambda e, r0=r0, rows=rows, lo=lo, hi=hi: e.dma_start(out=raw[:rows, lo - (t0 - 1):hi - (t0 - 1)], in_=uR[r0:r0 + rows, lo:hi]), writes=["raw"])
            un = "us%d" % j
            P.op("dve", lambda e, j=j, rows=rows: e.tensor_scalar(out=us[j][:rows, :Tt], in0=raw[:rows, 1:Tt + 1], scalar1=mus[:rows, j, 2:3], scalar2=None, op0=ALU.mult), reads=["raw", "mus"], writes=[un])
            P.op("dve", lambda e, j=j, rows=rows: e.scalar_tensor_tensor(out=us[j][:rows, :Tt], in0=raw[:rows, 0:Tt], scalar=mus[:rows, j, 0:1], in1=us[j][:rows, :Tt], op0=ALU.mult, op1=ALU.add), reads=["raw", "mus", un], writes=[un])
            P.op("dve", lambda e, j=j, rows=rows: e.scalar_tensor_tensor(out=us[j][:rows, :Tt], in0=raw[:rows, 2:Tt + 2], scalar=mus[:rows, j, 1:2], in1=us[j][:rows, :Tt], op0=ALU.mult, op1=ALU.add), reads=["raw", "mus", un], writes=[un])
        r_, k_, v_, wd_, ad_, g0_, g1_ = us
        store_fm("r", r_, "us0", t0, Tt); store_fm("v", v_, "us2", t0, Tt); store_tm("v", v_, "us2", t0, Tt)
        P.op("act", lambda e: e.activation(out=g0_[:, :Tt], in_=g0_[:, :Tt], func=AF.Sigmoid), reads=["us5"], writes=["us5"])
        P.op("act", lambda e: e.activation(out=g1_[:32, :Tt], in_=g1_[:32, :Tt], func=AF.Sigmoid), reads=["us6"], writes=["us6"])
        P.op("pe", lambda e: e.matmul(pA[:, :Tt], lhsT=gups0[:], rhs=g0_[:, :Tt], start=True, stop=False), reads=["gups0", "us5"], writes=["pA"])
        P.op("pe", lambda e: e.matmul(pA[:, :Tt], lhsT=gups1[:], rhs=g1_[:32, :Tt], start=False, stop=True), reads=["gups1", "us6"], writes=["pA"])
        P.op("act", lambda e: e.copy(out=t1[:, :Tt], in_=pA[:, :Tt]), reads=["pA"], writes=["t1"])
        store_fm("g", t1, "t1", t0, Tt)
        P.op("dve", lambda e: e.tensor_scalar(out=kkn[:, :Tt], in0=k_[:, :Tt], scalar1=pcs[:, KKW:KKW + 1], scalar2=None, op0=ALU.mult), reads=["us1", "pcs"], writes=["kkn"])
        P.op("act", lambda e: e.activation(out=t2[:, :Tt], in_=kkn[:, :Tt], func=AF.Square), reads=["kkn"], writes=["t2"])
        P.op("pe", lambda e: e.matmul(pB[:, :Tt], lhsT=blk[:], rhs=t2[:, :Tt], start=True, stop=True), reads=["blk", "t2"], writes=["pB"])
        rsqrt_op(P, C, t2[:, :Tt], "t2", pB[:, :Tt], "pB", "eps_tiny")
        P.op("dve", lambda e: e.tensor_tensor(out=kkn[:, :Tt], in0=kkn[:, :Tt], in1=t2[:, :Tt], op=ALU.mult), reads=["kkn", "t2"], writes=["kkn"])
        P.op("pool", lambda e: e.tensor_scalar(out=t3[:, :Tt], in0=kkn[:, :Tt], scalar1=-1.0, scalar2=None, op0=ALU.mult), reads=["kkn"], writes=["t3"])
        store_tm("a", t3, "t3", t0, Tt)
        P.op("act", lambda e: e.activation(out=wd_[:, :Tt], in_=wd_[:, :Tt], func=AF.Tanh), reads=["us3"], writes=["us3"])
        for d in range(2):
            P.op("pe", lambda e, d=d: e.matmul(pA[:, :Tt], lhsT=wups[64 * d:64 * d + 64, :], rhs=wd_[64 * d:64 * d + 64, :Tt], start=True, stop=True), reads=["wups", "us3"], writes=["pA"])
            P.op("act", lambda e, d=d: e.activation(out=t1[:, :Tt], in_=pA[:, :Tt], func=AF.Sigmoid, bias=pcs[:, W0 + d:W0 + d + 1]), reads=["pA", "pcs"], writes=["t1"])
            P.op("act", lambda e: e.activation(out=t1[:, :Tt], in_=t1[:, :Tt], func=AF.Exp, scale=-float(np.exp(-0.5))), reads=["t1"], writes=["t1"])
            store_fm("w%d" % d, t1, "t1", t0, Tt)
            P.op("pe", lambda e, d=d: e.matmul(pB[:, :Tt], lhsT=aups[64 * d:64 * d + 64, :], rhs=ad_[64 * d:64 * d + 64, :Tt], start=True, stop=True), reads=["aups", "us4"], writes=["pB"])
            P.op("act", lambda e, d=d: e.activation(out=t2[:, :Tt], in_=pB[:, :Tt], func=AF.Sigmoid, bias=pcs[:, A0 + d:A0 + d + 1]), reads=["pB", "pcs"], writes=["t2"])
            P.op("dve", lambda e: e.tensor_tensor(out=t3[:, :Tt], in0=kkn[:, :Tt], in1=t2[:, :Tt], op=ALU.mult), reads=["kkn", "t2"], writes=["t3"])
            store_tm("b%d" % d, t3, "t3", t0, Tt)
            P.op("dve", lambda e: e.tensor_scalar(out=t2[:, :Tt], in0=t2[:, :Tt], scalar1=pcs[:, KA:KA + 1], scalar2=pcs[:, OMKA:OMKA + 1], op0=ALU.mult, op1=ALU.add), reads=["t2", "pcs"], writes=["t2"])
            P.op("dve", lambda e: e.tensor_tensor(out=t2[:, :Tt], in0=t2[:, :Tt], in1=k_[:, :Tt], op=ALU.mult), reads=["t2", "us1"], writes=["t2"])
            store_tm("k%d" % d, t2, "t2", t0, Tt)
            if d == 0:
                P.op("pool", lambda e: e.tensor_copy(out=ksum[:, :Tt], in_=t2[:, :Tt]), reads=["t2"], writes=["ksum"])
            else:
                P.op("pool", lambda e: e.tensor_tensor(out=ksum[:, :Tt], in0=ksum[:, :Tt], in1=t2[:, :Tt], op=ALU.add), reads=["t2", "ksum"], writes=["ksum"])
        store_fm("ks", ksum, "ksum", t0, Tt)

    for (s0_, sn_, sid_) in segs:
        for (t0_, Tt_, _) in token_tiles([(s0_, sn_, sid_)], TT):
            prep_tile(s0_, sn_, t0_, Tt_)

    NCH = 4
    rows = {}
    for n in ("a", "b", "k", "v"):
        for bf in range(2):
            for pr in range(2):
                t = P.sb("row_%s_%d_%d" % (n, bf, pr), [128, TC, 128])
                P.op("pool", lambda e, t=t: e.memset(t[:], 0.0), writes=["row_%s%d_%d" % (n, ci, bf) for ci in (2 * pr, 2 * pr + 1)])
                for q in range(2):
                    rows[(n, 2 * pr + q, bf)] = t[64 * q:64 * q + 2]
    wcol = {(ci, bf): P.sb("wcol%d_%d" % (ci, bf), [128, TC]) for ci in range(NCH) for bf in range(2)}
    rcol = {(ci, bf): P.sb("rcol%d_%d" % (ci, bf), [128, TC]) for ci in range(NCH) for bf in range(2)}
    ST = {(ci, bf): P.sb("ST%d_%d" % (ci, bf), [128, 128]) for ci in range(NCH) for bf in range(2)}
    MT = {(ci, bf): P.sb("MT%d_%d" % (ci, bf), [128, 128]) for ci in range(NCH) for bf in range(2)}
    ysb = {(ci, bf): P.sb("ysb%d_%d" % (ci, bf), [128, TC]) for ci in range(NCH) for bf in range(2)}
    pM = [P.ps("pM%d" % i, [128, 128]) for i in range(2)]
    pSt = [P.ps("pSt%d" % i, [128, 128]) for i in range(2)]
    pY = [pA, pB]
    for ci in range(NCH):
        P.op("pool", lambda e, ci=ci: e.memset(ST[(ci, 0)][:], 0.0), writes=["ST%d_0" % ci])
    ydkeys = []

    def chunk_list(b, d):
        cb = 2 * S + 256 * b; lb = S * b
        ctx = [(cb + i * TC) for i in range(256 // TC)]
        lat = [(lb + i * TC) for i in range(S // TC)]
        return ctx + lat if d == 0 else ctx[::-1] + lat[::-1]

    chains = [(b, d) for b in range(2) for d in range(2)]
    clists = [chunk_list(b, d) for (b, d) in chains]
    nchunks = len(clists[0])
    step = 0
    for cidx in range(nchunks):
        bf = cidx % 2
        for ci, (b, d) in enumerate(chains):
            t0 = clists[ci][cidx]

            def load(ci=ci, d=d, t0=t0, bf=bf):
                for n, src in (("a", "a"), ("b", "b%d" % d), ("k", "k%d" % d), ("v", "v")):
                    for h in range(2):
                        rn = "row_%s%d_%d" % (n, ci, bf)
                        P.dma("sp" if h == 0 else "act", lambda e, n=n, src=src, h=h: e.dma_start(out=rows[(n, ci, bf)][h:h + 1, :, h * 64:(h + 1) * 64],
                                                                                                     in_=tm[src][t0:t0 + TC, h * 64:(h + 1) * 64].rearrange("(o t) k -> o t k", o=1)),
                              reads=scr_keys, writes=[rn])
                P.dma("pool", lambda e: e.dma_start(out=wcol[(ci, bf)][:], in_=fm["w%d" % d][:, t0:t0 + TC]), reads=scr_keys, writes=["wcol%d_%d" % (ci, bf)])
                P.dma("pool", lambda e: e.dma_start(out=rcol[(ci, bf)][:], in_=fm["r"][:, t0:t0 + TC]), reads=scr_keys, writes=["rcol%d_%d" % (ci, bf)])
            load()
        for s in range(TC):
            for ci, (b, d) in enumerate(chains):
                tt = s if d == 0 else TC - 1 - s
                cur = step % 2; nxt = (step + 1) % 2

                def one(ci=ci, tt=tt, bf=bf, cur=cur, nxt=nxt):
                    ar = rows[("a", ci, bf)]; br = rows[("b", ci, bf)]; kr = rows[("k", ci, bf)]; vr = rows[("v", ci, bf)]
                    pm = pM[ci % 2]; pst = pSt[ci % 2]
                    P.op("pe", lambda e: e.matmul(pm[:], lhsT=ar[:, tt, :], rhs=br[:, tt, :], start=True, stop=True),
                         reads=["row_a%d_%d" % (ci, bf), "row_b%d_%d" % (ci, bf)], writes=["pM%d" % (ci % 2)])
                    P.op("dve", lambda e: e.scalar_tensor_tensor(out=MT[(ci, cur)][:], in0=ident[:], scalar=wcol[(ci, bf)][:, tt:tt + 1], in1=pm[:], op0=ALU.mult, op1=ALU.add),
                         reads=["ident", "wcol%d_%d" % (ci, bf), "pM%d" % (ci % 2)], writes=["MT%d_%d" % (ci, cur)])
                    P.op("pe", lambda e: e.matmul(pst[:], lhsT=MT[(ci, cur)][:], rhs=ST[(ci, cur)][:], start=True, stop=False),
                         reads=["MT%d_%d" % (ci, cur), "ST%d_%d" % (ci, cur)], writes=["pSt%d" % (ci % 2)])
                    P.op("pe", lambda e: e.matmul(pst[:], lhsT=kr[:, tt, :], rhs=vr[:, tt, :], start=False, stop=True),
                         reads=["row_k%d_%d" % (ci, bf), "row_v%d_%d" % (ci, bf)], writes=["pSt%d" % (ci % 2)])
                    P.op("act", lambda e: e.copy(out=ST[(ci, nxt)][:], in_=pst[:]), reads=["pSt%d" % (ci % 2)], writes=["ST%d_%d" % (ci, nxt)])
                    P.op("pe", lambda e: e.matmul(pY[bf][:, ci * TC + tt:ci * TC + tt + 1], lhsT=ST[(ci, nxt)][:], rhs=rcol[(ci, bf)][:, tt:tt + 1], start=True, stop=True),
                         reads=["ST%d_%d" % (ci, nxt), "rcol%d_%d" % (ci, bf)], writes=[("pY", bf, ci)])
                one()
            step += 1
        for ci, (b, d) in enumerate(chains):
            t0 = clists[ci][cidx]

            def fin(ci=ci, d=d, t0=t0, bf=bf):
                P.op("dve", lambda e: e.tensor_copy(out=ysb[(ci, bf)][:], in_=pY[bf][:, ci * TC:(ci + 1) * TC]), reads=[("pY", bf, ci)], writes=["ysb%d_%d" % (ci, bf)])
                key = ("yd", d, t0); ydkeys.append(key)
                P.dma("sp", lambda e: e.dma_start(out=yd[d][:, t0:t0 + TC], in_=ysb[(ci, bf)][:]), reads=["ysb%d_%d" % (ci, bf)], writes=[key])
            fin()

    y0 = P.sb("py0", [128, TT]); y1 = P.sb("py1", [128, TT])
    outs = []

    def post_tile(t0, Tt):
        P.dma("sp", lambda e: e.dma_start(out=y0[:, :Tt], in_=yd[0][:, t0:t0 + Tt]), reads=ydkeys, writes=["py0"])
        P.dma("act", lambda e: e.dma_start(out=y1[:, :Tt], in_=yd[1][:, t0:t0 + Tt]), reads=ydkeys, writes=["py1"])
        P.dma("sp", lambda e: e.dma_start(out=us[0][:, :Tt], in_=fm["r"][:, t0:t0 + Tt]), reads=scr_keys, writes=["us0"])
        P.dma("act", lambda e: e.dma_start(out=us[1][:, :Tt], in_=fm["ks"][:, t0:t0 + Tt]), reads=scr_keys, writes=["us1"])
        P.dma("sp", lambda e: e.dma_start(out=us[2][:, :Tt], in_=fm["v"][:, t0:t0 + Tt]), reads=scr_keys, writes=["us2"])
        P.dma("act", lambda e: e.dma_start(out=us[3][:, :Tt], in_=fm["g"][:, t0:t0 + Tt]), reads=scr_keys, writes=["us3"])
        P.op("dve", lambda e: e.tensor_tensor(out=y0[:, :Tt], in0=y0[:, :Tt], in1=y1[:, :Tt], op=ALU.add), reads=["py0", "py1"], writes=["py0"])
        P.op("act", lambda e: e.activation(out=t1[:, :Tt], in_=y0[:, :Tt], func=AF.Square), reads=["py0"], writes=["t1"])
        P.op("pe", lambda e: e.matmul(pA[:, :Tt], lhsT=blk[:], rhs=y0[:, :Tt], start=True, stop=True), reads=["blk", "py0"], writes=["pA"])
        P.op("pe", lambda e: e.matmul(pB[:, :Tt], lhsT=blk[:], rhs=t1[:, :Tt], start=True, stop=True), reads=["blk", "t1"], writes=["pB"])
        P.op("act", lambda e: e.mul(out=t2[:, :Tt], in_=pA[:, :Tt], mul=1.0 / 64), reads=["pA"], writes=["t2"])
        P.op("dve", lambda e: e.tensor_tensor(out=t3[:, :Tt], in0=t2[:, :Tt], in1=t2[:, :Tt], op=ALU.mult), reads=["t2"], writes=["t3"])
        P.op("dve", lambda e: e.scalar_tensor_tensor(out=t3[:, :Tt], in0=pB[:, :Tt], scalar=1.0 / 64, in1=t3[:, :Tt], op0=ALU.mult, op1=ALU.subtract), reads=["pB", "t3"], writes=["t3"])
        rsqrt_op(P, C, t3[:, :Tt], "t3", t3[:, :Tt], "t3", "eps_gn")
        P.op("dve", lambda e: e.tensor_tensor(out=y0[:, :Tt], in0=y0[:, :Tt], in1=t2[:, :Tt], op=ALU.subtract), reads=["py0", "t2"], writes=["py0"])
        P.op("dve", lambda e: e.tensor_tensor(out=y0[:, :Tt], in0=y0[:, :Tt], in1=t3[:, :Tt], op=ALU.mult), reads=["py0", "t3"], writes=["py0"])
        P.op("act", lambda e: e.activation(out=y0[:, :Tt], in_=y0[:, :Tt], func=AF.Identity, bias=pcs[:, LNB:LNB + 1], scale=pcs[:, LNG:LNG + 1]), reads=["py0", "pcs"], writes=["py0"])
        P.op("dve", lambda e: e.scalar_tensor_tensor(out=t1[:, :Tt], in0=us[0][:, :Tt], scalar=pcs[:, RK:RK + 1], in1=us[1][:, :Tt], op0=ALU.mult, op1=ALU.mult), reads=["us0", "us1", "pcs"], writes=["t1"])
        P.op("pe", lambda e: e.matmul(pA[:, :Tt], lhsT=blk[:], rhs=t1[:, :Tt], start=True, stop=True), reads=["blk", "t1"], writes=["pA"])
        P.op("dve", lambda e: e.tensor_tensor(out=t1[:, :Tt], in0=pA[:, :Tt], in1=us[2][:, :Tt], op=ALU.mult), reads=["pA", "us2"], writes=["t1"])
        P.op("dve", lambda e: e.tensor_tensor(out=y0[:, :Tt], in0=y0[:, :Tt], in1=t1[:, :Tt], op=ALU.add), reads=["py0", "t1"], writes=["py0"])
        P.op("dve", lambda e: e.tensor_tensor(out=y0[:, :Tt], in0=y0[:, :Tt], in1=us[3][:, :Tt], op=ALU.mult), reads=["py0", "us3"], writes=["py0"])
        key = ("out", t0); outs.append(key)
        P.dma("sp", lambda e: e.dma_start(out=yT[:, t0:t0 + Tt], in_=y0[:, :Tt]), reads=["py0"], writes=[key])

    for (t0_, Tt_, _) in token_tiles(segs, TT):
        post_tile(t0_, Tt_)
    P.finish_wait("sp", outs)
    P.emit()
    return nc


def conv3_tile(P, raw, rawname, dst, dname, cws, cwname, j, rows, Tt, silu):
    P.op("dve", lambda e: e.tensor_scalar(out=dst[:rows, :Tt], in0=raw[:rows, 1:Tt + 1], scalar1=cws[:rows, j, 1:2], scalar2=cws[:rows, j, 3:4], op0=ALU.mult, op1=ALU.add), reads=[rawname, cwname], writes=[dname])
    P.op("dve", lambda e: e.scalar_tensor_tensor(out=dst[:rows, :Tt], in0=raw[:rows, 0:Tt], scalar=cws[:rows, j, 0:1], in1=dst[:rows, :Tt], op0=ALU.mult, op1=ALU.add), reads=[rawname, cwname, dname], writes=[dname])
    P.op("dve", lambda e: e.scalar_tensor_tensor(out=dst[:rows, :Tt], in0=raw[:rows, 2:Tt + 2], scalar=cws[:rows, j, 2:3], in1=dst[:rows, :Tt], op0=ALU.mult, op1=ALU.add), reads=[rawname, cwname, dname], writes=[dname])
    if silu:
        P.op("act", lambda e: e.activation(out=dst[:rows, :Tt], in_=dst[:rows, :Tt], func=AF.Silu), reads=[dname], writes=[dname])


def load_halo(P, q, raw, rawname, src, r0, rows, seg0, segn, t0, Tt):
    lo = max(seg0, t0 - 1); hi = min(seg0 + segn, t0 + Tt + 1)
    P.op("pool", lambda e: e.memset(raw[:, :], 0.0), writes=[rawname])
    P.dma(q, lambda e: e.dma_start(out=raw[:rows, lo - (t0 - 1):hi - (t0 - 1)], in_=src[r0:r0 + rows, lo:hi]), writes=[rawname])


def build_ssd(S):
    nc = new_nc()
    TOT = 2 * S + 512
    din = lambda name, shp: nc.dram_tensor(name, shp, F32, kind="ExternalInput").ap()
    zT = din("zT", [128, TOT]); xbcT = din("xbcT", [384, TOT]); cw = din("cw", [128, 3, 4]); dttok = din("dttok", [TOT, 4])
    dtb = din("dtb", [128, 4]); alog = din("alog", [128, 4]); Dp = din("Dp", [128, 1]); ident_d = din("ident", [128, 128]); triU_d = din("triU", [128, 128])
    yT = nc.dram_tensor("yT", [128, TOT], F32, kind="ExternalOutput").ap()
    scr = lambda name, shp: nc.dram_tensor(name, shp, F32, kind="Internal").ap()
    fm = {n: scr("fm_" + n, [128, TOT]) for n in ("xs", "B", "C")}
    tm = {n: scr("tm_" + n, [TOT, 128]) for n in ("X", "B")}
    yd = [scr("yd%d" % d, [128, TOT]) for d in range(2)]
    P = Prog(nc)
    P.setup_sems()
    C = consts(P)
    ones = C["ones"]
    ident = P.sb("ident_s", [128, 128]); U = P.sb("U_s", [128, 128]); L = P.sb("L_s", [128, 128])
    cws = P.sb("cws", [128, 3, 4]); dtbs = P.sb("dtbs", [128, 4]); als = P.sb("als", [128, 4]); Dps = P.sb("Dps", [128, 1])
    pTr = P.ps("pTr", [128, 128])
    P.dma("sp", lambda e: e.dma_start(out=ident[:], in_=ident_d), writes=["ident"])
    P.dma("sp", lambda e: e.dma_start(out=U[:], in_=triU_d), writes=["U"])
    P.dma("sp", lambda e: e.dma_start(out=cws[:], in_=cw), writes=["cws"])
    P.dma("act", lambda e: e.dma_start(out=dtbs[:], in_=dtb), writes=["dtbs"])
    P.dma("act", lambda e: e.dma_start(out=als[:], in_=alog), writes=["als"])
    P.dma("act", lambda e: e.dma_start(out=Dps[:], in_=Dp), writes=["Dps"])
    P.op("pe", lambda e: e.transpose(out=pTr[:], in_=U[:], identity=ident[:]), reads=["U", "ident"], writes=["pTr"])
    P.op("act", lambda e: e.copy(out=L[:], in_=pTr[:]), reads=["pTr"], writes=["L"])
    P.op("act", lambda e: e.activation(out=als[:], in_=als[:], func=AF.Exp), reads=["als"], writes=["als"])
    P.op("dve", lambda e: e.tensor_scalar(out=als[:], in0=als[:], scalar1=-1.0, scalar2=None, op0=ALU.mult), reads=["als"], writes=["als"])
    NCK = TOT // 128
    dts = P.sb("dts", [128, NCK, 4]); dAs = P.sb("dAs", [128, NCK, 4])
    P.dma("sp", lambda e: e.dma_start(out=dts[:], in_=dttok.rearrange("(c p) k -> p c k", p=128)), writes=["dts"])
    for cg in range(NCK):
        P.op("dve", lambda e, cg=cg: e.tensor_tensor(out=dts[:, cg, :], in0=dts[:, cg, :], in1=dtbs[:], op=ALU.add), reads=["dts", "dtbs"], writes=["dts"])
    P.op("act", lambda e: e.activation(out=dts[:], in_=dts[:], func=AF.Exp), reads=["dts"], writes=["dts"])
    P.op("act", lambda e: e.activation(out=dts[:], in_=dts[:], func=AF.Ln, bias=1.0), reads=["dts"], writes=["dts"])
    for cg in range(NCK):
        P.op("dve", lambda e, cg=cg: e.tensor_tensor(out=dAs[:, cg, :], in0=dts[:, cg, :], in1=als[:], op=ALU.mult), reads=["dts", "als"], writes=["dAs"])

    TT = 512
    raw = P.sb("raw", [128, TT + 2]); cv = P.sb("cv", [128, TT]); tr = P.sb("tr", [128, 128])
    segs = [(0, S, 0), (S, S, 0), (2 * S, 256, 1), (2 * S + 256, 256, 1)]
    scr_keys = []

    def prep_tile(seg0, segn, t0, Tt):
        for j, name in enumerate(("xs", "B", "C")):
            load_halo(P, "sp" if j % 2 else "act", raw, "raw", xbcT, j * 128, 128, seg0, segn, t0, Tt)
            conv3_tile(P, raw, "raw", cv, "cv", cws, "cws", j, 128, Tt, True)
            key = ("fm", name, t0); scr_keys.append(key)
            P.dma("sp", lambda e, name=name: e.dma_start(out=fm[name][:, t0:t0 + Tt], in_=cv[:, :Tt]), reads=["cv"], writes=[key])
            if name != "C":
                tn = "X" if name == "xs" else "B"
                for s0 in range(0, Tt, 128):
                    P.op("pe", lambda e, s0=s0: e.transpose(out=pTr[:], in_=cv[:, s0:s0 + 128], identity=ident[:]), reads=["cv", "ident"], writes=["pTr"])
                    P.op("act", lambda e: e.copy(out=tr[:], in_=pTr[:]), reads=["pTr"], writes=["tr"])
                    key = ("tm", tn, t0 + s0); scr_keys.append(key)
                    P.dma("sp", lambda e, s0=s0, tn=tn: e.dma_start(out=tm[tn][t0 + s0:t0 + s0 + 128, :], in_=tr[:]), reads=["tr"], writes=[key])

    for (s0_, sn_, sid_) in segs:
        for (t0_, Tt_, _) in token_tiles([(s0_, sn_, sid_)], TT):
            prep_tile(s0_, sn_, t0_, Tt_)

    chains = [(b, d) for b in range(2) for d in range(2)]

    def chunk_list(b, d):
        cb = 2 * S + 256 * b; lb = S * b
        ctx = [cb, cb + 128]; lat = [lb + i * 128 for i in range(S // 128)]
        return ctx + lat if d == 0 else ctx[::-1] + lat[::-1]
    clists = [chunk_list(b, d) for (b, d) in chains]
    bufs = {}
    for ci in range(4):
        for bf in range(2):
            for n in ("BT", "CT", "Bk", "Xk"):
                bufs[(n, ci, bf)] = P.sb("%s%d_%d" % (n, ci, bf), [128, 128])
    STp = {}; Xp = {}
    for ci in range(4):
        for h in range(2):
            STp[(ci, h)] = P.sb("STp%d_%d" % (ci, h), [128, 128]); Xp[(ci, h)] = P.sb("Xp%d_%d" % (ci, h), [128, 128])
            P.op("pool", lambda e, t=STp[(ci, h)]: e.memset(t[:], 0.0), writes=["STp%d_%d" % (ci, h)])
            P.op("pool", lambda e, t=Xp[(ci, h)]: e.memset(t[:], 0.0), writes=["Xp%d_%d" % (ci, h)])
    dAb = P.sb("dAb", [128, 128]); Lm = P.sb("Lm", [128, 128]); MTt = P.sb("MTt", [128, 128]); erow = P.sb("erow", [128, 128]); CTs = P.sb("CTs", [128, 128])
    colsb = P.sb("colsb", [128, 1]); dcol = P.sb("dcol", [128, 1]); Xd = P.sb("Xd", [128, 64]); ysb = P.sb("ysb", [128, 128])
    pG = P.ps("pG", [128, 128]); pR = P.ps("pR", [128, 128]); pC = P.ps("pC", [128, 1]); pY = P.ps("pY", [128, 128]); pS = P.ps("pS", [128, 64])
    ydkeys = []

    def chunk_step(ci, d, t0, bf):
        BT, CT, Bk, Xk = (bufs[(n, ci, bf)] for n in ("BT", "CT", "Bk", "Xk"))
        nm = lambda n: "%s%d_%d" % (n, ci, bf)
        P.dma("sp", lambda e: e.dma_start(out=BT[:], in_=fm["B"][:, t0:t0 + 128]), reads=scr_keys, writes=[nm("BT")])
        P.dma("act", lambda e: e.dma_start(out=CT[:], in_=fm["C"][:, t0:t0 + 128]), reads=scr_keys, writes=[nm("CT")])
        P.dma("sp", lambda e: e.dma_start(out=Bk[:], in_=tm["B"][t0:t0 + 128, :]), reads=scr_keys, writes=[nm("Bk")])
        P.dma("act", lambda e: e.dma_start(out=Xk[:], in_=tm["X"][t0:t0 + 128, :]), reads=scr_keys, writes=[nm("Xk")])
        cg = t0 // 128
        Ud, Ud_n = (U, "U") if d == 0 else (L, "L")
        last = 127 if d == 0 else 0
        P.op("pe", lambda e: e.matmul(pG[:], lhsT=BT[:], rhs=CT[:], start=True, stop=True), reads=[nm("BT"), nm("CT")], writes=["pG"])
        for h in range(2):
            dh = d * 2 + h
            hc = slice(h * 64, (h + 1) * 64)
            stn = "STp%d_%d" % (ci, h); xpn = "Xp%d_%d" % (ci, h)
            P.op("dve", lambda e, dh=dh: e.tensor_scalar(out=dAb[:], in0=ones[:], scalar1=dAs[:, cg, dh:dh + 1], scalar2=None, op0=ALU.mult), reads=["c_ones", "dAs"], writes=["dAb"])
            P.op("pe", lambda e: e.matmul(pR[:], lhsT=dAb[:], rhs=Ud[:], start=True, stop=True), reads=["dAb", Ud_n], writes=["pR"])
            P.op("pe", lambda e, dh=dh: e.matmul(pC[:], lhsT=Ud[:], rhs=dAs[:, cg, dh:dh + 1], start=True, stop=True), reads=["dAs", Ud_n], writes=["pC"])
            P.op("act", lambda e: e.copy(out=colsb[:], in_=pC[:]), reads=["pC"], writes=["colsb"])
            P.op("dve", lambda e: e.tensor_scalar(out=Lm[:], in0=pR[:], scalar1=colsb[:, 0:1], scalar2=0.0, op0=ALU.subtract, op1=ALU.min), reads=["pR", "colsb"], writes=["Lm"])
            P.op("act", lambda e: e.activation(out=Lm[:], in_=Lm[:], func=AF.Exp), reads=["Lm"], writes=["Lm"])
            P.op("pool", lambda e: e.tensor_tensor(out=Lm[:], in0=Lm[:], in1=Ud[:], op=ALU.mult), reads=["Lm", Ud_n], writes=["Lm"])
            P.op("dve", lambda e: e.tensor_tensor(out=MTt[:], in0=Lm[:], in1=pG[:], op=ALU.mult), reads=["Lm", "pG"], writes=["MTt"])
            P.op("act", lambda e: e.activation(out=erow[:], in_=pR[:], func=AF.Exp), reads=["pR"], writes=["erow"])
            P.op("pool", lambda e: e.tensor_tensor(out=CTs[:], in0=CT[:], in1=erow[:], op=ALU.mult), reads=[nm("CT"), "erow"], writes=["CTs"])
            P.op("dve", lambda e, dh=dh, h=h, hc=hc: e.tensor_scalar(out=Xp[(ci, h)][:, hc], in0=Xk[:, hc], scalar1=dts[:, cg, dh:dh + 1], scalar2=None, op0=ALU.mult), reads=[nm("Xk"), "dts"], writes=[xpn])
            P.op("pe", lambda e, h=h: e.matmul(pY[:], lhsT=Xp[(ci, h)][:], rhs=MTt[:], start=(h == 0), stop=False), reads=[xpn, "MTt"], writes=["pY"])
            P.op("pe", lambda e, h=h: e.matmul(pY[:], lhsT=STp[(ci, h)][:], rhs=CTs[:], start=False, stop=(h == 1)), reads=[stn, "CTs"], writes=["pY"])
            P.op("dve", lambda e: e.tensor_tensor(out=dcol[:], in0=pR[:, last:last + 1], in1=colsb[:], op=ALU.subtract), reads=["pR", "colsb"], writes=["dcol"])
            P.op("act", lambda e: e.activation(out=dcol[:], in_=dcol[:], func=AF.Exp), reads=["dcol"], writes=["dcol"])
            P.op("dve", lambda e, h=h, hc=hc: e.tensor_scalar(out=Xd[:], in0=Xp[(ci, h)][:, hc], scalar1=dcol[:, 0:1], scalar2=None, op0=ALU.mult), reads=[xpn, "dcol"], writes=["Xd"])
            P.op("pe", lambda e: e.matmul(pS[:], lhsT=Bk[:], rhs=Xd[:], start=True, stop=True), reads=[nm("Bk"), "Xd"], writes=["pS"])
            P.op("dve", lambda e, h=h, hc=hc: e.scalar_tensor_tensor(out=STp[(ci, h)][:, hc], in0=STp[(ci, h)][:, hc], scalar=erow[:, last:last + 1], in1=pS[:], op0=ALU.mult, op1=ALU.add),
                 reads=[stn, "erow", "pS"], writes=[stn])
        P.op("act", lambda e: e.copy(out=ysb[:], in_=pY[:]), reads=["pY"], writes=["ysb"])
        key = ("yd", d, t0); ydkeys.append(key)
        P.dma("sp", lambda e: e.dma_start(out=yd[d][:, t0:t0 + 128], in_=ysb[:]), reads=["ysb"], writes=[key])

    for cidx in range(len(clists[0])):
        for ci, (b, d) in enumerate(chains):
            chunk_step(ci, d, clists[ci][cidx], cidx % 2)

    y0 = P.sb("py0", [128, TT]); y1 = P.sb("py1", [128, TT]); xs_ = P.sb("pxs", [128, TT]); z_ = P.sb("pz", [128, TT])
    outs = []

    def post_tile(t0, Tt):
        P.dma("sp", lambda e: e.dma_start(out=y0[:, :Tt], in_=yd[0][:, t0:t0 + Tt]), reads=ydkeys, writes=["py0"])
        P.dma("act", lambda e: e.dma_start(out=y1[:, :Tt], in_=yd[1][:, t0:t0 + Tt]), reads=ydkeys, writes=["py1"])
        P.dma("sp", lambda e: e.dma_start(out=xs_[:, :Tt], in_=fm["xs"][:, t0:t0 + Tt]), reads=scr_keys, writes=["pxs"])
        P.dma("act", lambda e: e.dma_start(out=z_[:, :Tt], in_=zT[:, t0:t0 + Tt]), writes=["pz"])
        P.op("dve", lambda e: e.tensor_tensor(out=y0[:, :Tt], in0=y0[:, :Tt], in1=y1[:, :Tt], op=ALU.add), reads=["py0", "py1"], writes=["py0"])
        P.op("dve", lambda e: e.scalar_tensor_tensor(out=y0[:, :Tt], in0=xs_[:, :Tt], scalar=Dps[:, 0:1], in1=y0[:, :Tt], op0=ALU.mult, op1=ALU.add), reads=["py0", "pxs", "Dps"], writes=["py0"])
        P.op("act", lambda e: e.activation(out=z_[:, :Tt], in_=z_[:, :Tt], func=AF.Silu), reads=["pz"], writes=["pz"])
        P.op("dve", lambda e: e.tensor_tensor(out=y0[:, :Tt], in0=y0[:, :Tt], in1=z_[:, :Tt], op=ALU.mult), reads=["py0", "pz"], writes=["py0"])
        key = ("out", t0); outs.append(key)
        P.dma("sp", lambda e: e.dma_start(out=yT[:, t0:t0 + Tt], in_=y0[:, :Tt]), reads=["py0"], writes=[key])

    for (t0_, Tt_, _) in token_tiles(segs, TT):
        post_tile(t0_, Tt_)
    P.finish_wait("sp", outs)
    P.emit()
    return nc


def build_hyena(S, need_ctx=True):
    nc = new_nc()
    TOT = 2 * S + 512
    din = lambda name, shp: nc.dram_tensor(name, shp, F32, kind="ExternalInput").ap()
    uH = din("uH", [384, TOT]); cw = din("cw", [128, 3, 4]); hd = din("hd", [128, 1]); zL = din("zL", [33, S]); zC = din("zC", [33, 256])
    w1 = din("w1", [33, 64]); w2 = din("w2", [64, 64]); w3 = din("w3", [64, 256]); fp = din("fp", [64, 4]); winL = din("winL", [128, S]); winC = din("winC", [128, 256])
    yT = nc.dram_tensor("yT", [128, TOT], F32, kind="ExternalOutput").ap()
    P = Prog(nc)
    P.setup_sems()
    cws = P.sb("cws", [128, 3, 4]); hds = P.sb("hds", [128, 1]); w1s = P.sb("w1s", [33, 64]); w2s = P.sb("w2s", [64, 64]); w3s = P.sb("w3s", [64, 256]); fps = P.sb("fps", [64, 6])
    for (t, src, n) in ((cws, cw, "cws"), (hds, hd, "hds"), (w1s, w1, "w1s"), (w2s, w2, "w2s"), (w3s, w3, "w3s")):
        P.dma("sp", lambda e, t=t, src=src: e.dma_start(out=t[:], in_=src), writes=[n])
    P.dma("sp", lambda e: e.dma_start(out=fps[:, 0:4], in_=fp), writes=["fps"])
    P.op("dve", lambda e: e.tensor_tensor(out=fps[:, 3:4], in0=fps[:, 0:1], in1=fps[:, 2:3], op=ALU.mult), reads=["fps"], writes=["fps"])
    P.op("dve", lambda e: e.tensor_tensor(out=fps[:, 4:5], in0=fps[:, 1:2], in1=fps[:, 2:3], op=ALU.mult), reads=["fps"], writes=["fps"])
    P.op("pool", lambda e: e.memset(fps[:, 5:6], -float(np.pi)), reads=["fps"], writes=["fps"])
    hf = P.sb("hf", [128, S]); hb = P.sb("hb", [128, S]); vv = P.sb("vv", [128, S]); yA = P.sb("yA", [128, S]); yB = P.sb("yB", [128, S])
    zt = P.sb("zt", [33, 512]); h1 = P.sb("h1", [64, 512]); h2 = P.sb("h2", [64, 512]); wn = P.sb("wn", [128, 512])
    raw = P.sb("raw", [128, 514]); x0 = P.sb("x0", [128, 512]); x1 = P.sb("x1", [128, 512])
    rr = P.sb("rr", [64, 512]); ri = P.sb("ri", [64, 512], I32)
    pA = P.ps("pA", [128, 512]); pB = P.ps("pB", [128, 512])
    TWO_PI = float(2 * np.pi)

    def sin_layer(ws, wname, src, sname, krows, dst, dname, fcol, Tt):
        P.op("pe", lambda e: e.matmul(pA[:64, :Tt], lhsT=ws[:krows, :], rhs=src[:krows, :Tt], start=True, stop=True), reads=[wname, sname], writes=["pA"])
        P.op("dve", lambda e: e.tensor_scalar(out=dst[:, :Tt], in0=pA[:64, :Tt], scalar1=fps[:, 2:3], scalar2=fps[:, fcol:fcol + 1], op0=ALU.mult, op1=ALU.add), reads=["pA", "fps"], writes=[dname])
        P.op("dve", lambda e: e.tensor_scalar(out=rr[:, :Tt], in0=dst[:, :Tt], scalar1=float(1.0 / TWO_PI), scalar2=16.5, op0=ALU.mult, op1=ALU.add), reads=[dname], writes=["rr"])
        P.op("dve", lambda e: e.tensor_copy(out=ri[:, :Tt], in_=rr[:, :Tt]), reads=["rr"], writes=["ri"])
        P.op("dve", lambda e: e.tensor_copy(out=rr[:, :Tt], in_=ri[:, :Tt]), reads=["ri"], writes=["rr"])
        P.op("dve", lambda e: e.tensor_scalar(out=rr[:, :Tt], in0=rr[:, :Tt], scalar1=-16.0, scalar2=-TWO_PI, op0=ALU.add, op1=ALU.mult), reads=["rr"], writes=["rr"])
        P.op("dve", lambda e: e.tensor_tensor(out=dst[:, :Tt], in0=dst[:, :Tt], in1=rr[:, :Tt], op=ALU.add), reads=[dname, "rr"], writes=[dname])
        P.op("dve", lambda e: e.tensor_single_scalar(out=rr[:, :Tt], in_=dst[:, :Tt], scalar=float(np.pi), op=ALU.is_gt), reads=[dname], writes=["rr"])
        P.op("dve", lambda e: e.scalar_tensor_tensor(out=dst[:, :Tt], in0=rr[:, :Tt], scalar=-TWO_PI, in1=dst[:, :Tt], op0=ALU.mult, op1=ALU.add), reads=[dname, "rr"], writes=[dname])
        P.op("dve", lambda e: e.tensor_single_scalar(out=rr[:, :Tt], in_=dst[:, :Tt], scalar=-float(np.pi), op=ALU.is_lt), reads=[dname], writes=["rr"])
        P.op("dve", lambda e: e.scalar_tensor_tensor(out=dst[:, :Tt], in0=rr[:, :Tt], scalar=TWO_PI, in1=dst[:, :Tt], op0=ALU.mult, op1=ALU.add), reads=[dname, "rr"], writes=[dname])
        P.op("act", lambda e: e.activation(out=dst[:, :Tt], in_=dst[:, :Tt], func=AF.Sin), reads=[dname], writes=[dname])

    def filters(zsrc, win, n):
        for t0 in range(0, n, 512):
            Tt = min(512, n - t0)

            def body(t0=t0, Tt=Tt):
                P.dma("sp", lambda e: e.dma_start(out=zt[:, :Tt], in_=zsrc[:, t0:t0 + Tt]), writes=["zt"])
                P.dma("act", lambda e: e.dma_start(out=wn[:, :Tt], in_=win[:, t0:t0 + Tt]), writes=["wn"])
                sin_layer(w1s, "w1s", zt, "zt", 33, h1, "h1", 3, Tt)
                sin_layer(w2s, "w2s", h1, "h1", 64, h2, "h2", 4, Tt)
                for side, (dst, dn) in enumerate(((hf, "hf"), (hb, "hb"))):
                    P.op("pe", lambda e, side=side: e.matmul(pB[:, :Tt], lhsT=w3s[:, side * 128:(side + 1) * 128], rhs=h2[:, :Tt], start=True, stop=True), reads=["w3s", "h2"], writes=["pB"])
                    P.op("dve", lambda e, dst=dst: e.tensor_tensor(out=dst[:, t0:t0 + Tt], in0=pB[:, :Tt], in1=wn[:, :Tt], op=ALU.mult), reads=["pB", "wn"], writes=[dn])
            body()

    outs = []

    def run_seq(seg0, n):
        for t0 in range(0, n, 512):
            Tt = min(512, n - t0)

            def body(t0=t0, Tt=Tt):
                g0 = seg0 + t0
                load_halo(P, "sp", raw, "raw", uH, 256, 128, seg0, n, g0, Tt)
                conv3_tile(P, raw, "raw", x0, "x0", cws, "cws", 2, 128, Tt, False)
                load_halo(P, "act", raw, "raw", uH, 128, 128, seg0, n, g0, Tt)
                conv3_tile(P, raw, "raw", x1, "x1", cws, "cws", 1, 128, Tt, False)
                P.op("dve", lambda e: e.tensor_tensor(out=vv[:, t0:t0 + Tt], in0=x0[:, :Tt], in1=x1[:, :Tt], op=ALU.mult), reads=["x0", "x1"], writes=["vv"])
            body()
        P.op("dve", lambda e: e.tensor_scalar(out=yA[:, :n], in0=vv[:, :n], scalar1=hds[:, 0:1], scalar2=None, op0=ALU.mult), reads=["vv", "hds"], writes=["yA"])
        P.op("pool", lambda e: e.memset(yB[:, :n], 0.0), writes=["yB"])
        k = 0
        for m in range(0, n):
            for side in range(2):
                if side == 1 and m == 0:
                    continue
                eng, acc, an = ("dve", yA, "yA")
                k += 1
                if side == 0:
                    P.op(eng, lambda e, m=m, acc=acc: e.scalar_tensor_tensor(out=acc[:, m:n], in0=vv[:, 0:n - m], scalar=hf[:, m:m + 1], in1=acc[:, m:n], op0=ALU.mult, op1=ALU.add),
                         reads=["vv", "hf", an], writes=[an])
                else:
                    P.op(eng, lambda e, m=m, acc=acc: e.scalar_tensor_tensor(out=acc[:, 0:n - m], in0=vv[:, m:n], scalar=hb[:, m:m + 1], in1=acc[:, 0:n - m], op0=ALU.mult, op1=ALU.add),
                         reads=["vv", "hb", an], writes=[an])
        P.op("dve", lambda e: e.tensor_tensor(out=yA[:, :n], in0=yA[:, :n], in1=yB[:, :n], op=ALU.add), reads=["yA", "yB"], writes=["yA"])
        for t0 in range(0, n, 512):
            Tt = min(512, n - t0)

            def body2(t0=t0, Tt=Tt):
                g0 = seg0 + t0
                load_halo(P, "sp", raw, "raw", uH, 0, 128, seg0, n, g0, Tt)
                conv3_tile(P, raw, "raw", x0, "x0", cws, "cws", 0, 128, Tt, False)
                P.op("dve", lambda e: e.tensor_tensor(out=x0[:, :Tt], in0=x0[:, :Tt], in1=yA[:, t0:t0 + Tt], op=ALU.mult), reads=["x0", "yA"], writes=["x0"])
                key = ("out", g0); outs.append(key)
                P.dma("sp", lambda e: e.dma_start(out=yT[:, g0:g0 + Tt], in_=x0[:, :Tt]), reads=["x0"], writes=[key])
            body2()

    filters(zL, winL, S)
    run_seq(0, S)
    run_seq(S, S)
    if need_ctx:
        filters(zC, winC, 256)
        run_seq(2 * S, 256)
        run_seq(2 * S + 256, 256)
    P.finish_wait("sp", outs)
    P.emit()
    return nc


ALPHA_DN = float((2 * 2) ** 0.25)


def ln_affine(P, C, src, skeys, dst, dkeys, Tt, bias_fn, scale_fn, pkeys, tmp, pst):
    ones = C["ones"]
    sq, mean, var, rstd = tmp["sq"], tmp["mean"], tmp["var"], tmp["rstd"]
    P.op("act", lambda e: e.activation(out=sq[:, :, :Tt], in_=src[:, :, :Tt], func=AF.Square), reads=skeys, writes=["sq"])
    for c in range(KC):
        P.op("pe", lambda e, c=c: e.matmul(pst[0][:, :Tt], lhsT=ones[:], rhs=src[:, c, :Tt], start=(c == 0), stop=(c == KC - 1)), reads=[skeys[c], "c_ones"], writes=["pst0"])
    for c in range(KC):
        P.op("pe", lambda e, c=c: e.matmul(pst[1][:, :Tt], lhsT=ones[:], rhs=sq[:, c, :Tt], start=(c == 0), stop=(c == KC - 1)), reads=["sq", "c_ones"], writes=["pst1"])
    P.op("act", lambda e: e.mul(out=mean[:, :Tt], in_=pst[0][:, :Tt], mul=1.0 / D), reads=["pst0"], writes=["mean"])
    P.op("dve", lambda e: e.tensor_tensor(out=var[:, :Tt], in0=mean[:, :Tt], in1=mean[:, :Tt], op=ALU.mult), reads=["mean"], writes=["var"])
    P.op("dve", lambda e: e.scalar_tensor_tensor(out=var[:, :Tt], in0=pst[1][:, :Tt], scalar=1.0 / D, in1=var[:, :Tt], op0=ALU.mult, op1=ALU.subtract), reads=["pst1", "var"], writes=["var"])
    rsqrt_op(P, C, rstd[:, :Tt], "rstd", var[:, :Tt], "var", "eps_ln")
    for c in range(KC):
        P.op("dve", lambda e, c=c: e.tensor_tensor(out=dst[:, c, :Tt], in0=src[:, c, :Tt], in1=mean[:, :Tt], op=ALU.subtract), reads=[skeys[c], "mean"], writes=[dkeys[c]])
        P.op("pool", lambda e, c=c: e.tensor_tensor(out=dst[:, c, :Tt], in0=dst[:, c, :Tt], in1=rstd[:, :Tt], op=ALU.mult), reads=[dkeys[c], "rstd"], writes=[dkeys[c]])
        P.op("act", lambda e, c=c: e.activation(out=dst[:, c, :Tt], in_=dst[:, c, :Tt], func=AF.Identity, bias=bias_fn(c), scale=scale_fn(c)), reads=[dkeys[c]] + pkeys, writes=[dkeys[c]])


def build_stageD(Tc, segs, TT=256):
    nc = new_nc()
    nseg = len(segs)
    din = lambda name, shp: nc.dram_tensor(name, shp, F32, kind="ExternalInput").ap()
    xT = din("xT", [D, Tc]); ybT = din("ybT", [4, 1024, Tc]); modD = din("modD", [128, KC, nseg, 5]); lnp = din("lnp", [128, KC, 2]); ssg = din("ssg", [128, 8])
    Wg = din("Wg", [D, 8192]); Wbr = din("Wbr", [4, 1024, D]); Wo = din("Wo", [D, D]); Wr = din("Wr", [D, 16])
    xl1T = nc.dram_tensor("xl1T", [D, Tc], F32, kind="ExternalOutput").ap()
    affT = nc.dram_tensor("affT", [16, Tc], F32, kind="ExternalOutput").ap()
    P = Prog(nc)
    P.setup_sems()
    C = consts(P)
    ones = C["ones"]
    tmp = {"sq": P.sb("sq", [128, KC, TT]), "mean": P.sb("mean", [128, TT]), "var": P.sb("var", [128, TT]), "rstd": P.sb("rstd", [128, TT])}
    pst = [P.ps("pst0", [128, TT]), P.ps("pst1", [128, TT])]
    mods = P.sb("mods", [128, KC, nseg, 5]); lns = P.sb("lns", [128, KC, 2]); ssgs = P.sb("ssgs", [128, 8]); wrs = P.sb("wrs", [128, KC, 16])
    P.dma("sp", lambda e: e.dma_start(out=mods[:], in_=modD), writes=["mods"])
    P.dma("sp", lambda e: e.dma_start(out=lns[:], in_=lnp), writes=["lns"])
    P.dma("sp", lambda e: e.dma_start(out=ssgs[:], in_=ssg), writes=["ssgs"])
    P.dma("sp", lambda e: e.dma_start(out=wrs[:], in_=Wr.rearrange("(c p) n -> p c n", p=128)), writes=["wrs"])
    for col in (1, 4):
        P.op("dve", lambda e, col=col: e.tensor_scalar(out=mods[:, :, :, col:col + 1], in0=mods[:, :, :, col:col + 1], scalar1=1.0, scalar2=None, op0=ALU.add), reads=["mods"], writes=["mods"])
    xs = P.sb("xs", [128, KC, TT]); hs = P.sb("hs", [128, KC, TT]); mg = P.sb("mg", [128, KC, TT])
    yb = [P.sb("yb%d" % i, [128, 8, TT]) for i in range(2)]
    wgt = [P.sb("wgt%d" % i, [128, KC, 128]) for i in range(2)]
    wbt = [P.sb("wbt%d" % i, [128, 8, 128]) for i in range(2)]
    gt = P.sb("gt", [128, TT]); tm_ = P.sb("tm_", [128, TT]); ex = P.sb("ex", [16, TT])
    pg = [P.ps("pg%d" % i, [128, TT]) for i in range(2)]
    pb = [P.ps("pb%d" % i, [128, TT]) for i in range(2)]
    xk = [("xs", c) for c in range(KC)]; hk = [("hs", c) for c in range(KC)]; mk_ = [("mg", c) for c in range(KC)]
    xv = xT.rearrange("(c p) t -> p c t", p=128)
    wgv = Wg.rearrange("(c p) n -> p c n", p=128)
    wov = Wo.rearrange("(c p) n -> p c n", p=128)
    outs = []
    cnt = [0]

    def tile_body(t0, Tt, sid):
        P.dma("sp", lambda e: e.dma_start(out=xs[:, :, :Tt], in_=xv[:, :, t0:t0 + Tt]), writes=xk)
        ln_affine(P, C, xs, xk, hs, hk, Tt, lambda c: mods[:, c, sid, 0:1], lambda c: mods[:, c, sid, 1:2], ["mods"], tmp, pst)
        for i in range(4):
            ybi = yb[i % 2]; ybn = "yb%d" % (i % 2)
            P.dma("act", lambda e, i=i, ybi=ybi: e.dma_start(out=ybi[:, :, :Tt], in_=ybT[i].rearrange("(c p) t -> p c t", p=128)[:, :, t0:t0 + Tt]), writes=[ybn])
            if i == 1:
                sq = tmp["sq"]
                P.op("act", lambda e, ybi=ybi: e.activation(out=sq[:, 0:8, :Tt], in_=ybi[:, :, :Tt], func=AF.Square), reads=[ybn], writes=["sq"])
                for g in range(2):
                    for c in range(4):
                        P.op("pe", lambda e, g=g, c=c: e.matmul(pst[g][:, :Tt], lhsT=ones[:], rhs=sq[:, 4 * g + c, :Tt], start=(c == 0), stop=(c == 3)), reads=["sq", "c_ones"], writes=["pst%d" % g])
                    rs = tmp["mean"] if g == 0 else tmp["var"]
                    rsn = "mean" if g == 0 else "var"
                    rsqrt_op(P, C, rs[:, :Tt], rsn, pst[g][:, :Tt], "pst%d" % g, "eps_1e5", scale=1.0 / 512)
                    for c in range(4):
                        P.op("dve", lambda e, g=g, c=c, rs=rs, ybi=ybi: e.scalar_tensor_tensor(out=ybi[:, 4 * g + c, :Tt], in0=ybi[:, 4 * g + c, :Tt], scalar=ssgs[:, 4 * g + c:4 * g + c + 1], in1=rs[:, :Tt], op0=ALU.mult, op1=ALU.mult),
                             reads=[ybn, rsn, "ssgs"], writes=[ybn])
            for dt in range(KC):
                b = cnt[0] % 2
                cnt[0] += 1
                col = i * D + dt * 128
                P.dma("sp", lambda e, b=b, col=col: e.dma_start(out=wgt[b][:], in_=wgv[:, :, col:col + 128]), writes=["wgt%d" % b])
                P.dma("pool", lambda e, b=b, i=i, dt=dt: e.dma_start(out=wbt[b][:], in_=Wbr[i].rearrange("(c p) n -> p c n", p=128)[:, :, dt * 128:(dt + 1) * 128]), writes=["wbt%d" % b])
                for c in range(KC):
                    P.op("pe", lambda e, b=b, c=c: e.matmul(pg[b][:, :Tt], lhsT=wgt[b][:, c, :], rhs=hs[:, c, :Tt], start=(c == 0), stop=(c == KC - 1)), reads=["wgt%d" % b, hk[c]], writes=["pg%d" % b])
                for c in range(8):
                    P.op("pe", lambda e, b=b, c=c, ybi=ybi: e.matmul(pb[b][:, :Tt], lhsT=wbt[b][:, c, :], rhs=ybi[:, c, :Tt], start=(c == 0), stop=(c == 7)), reads=["wbt%d" % b, ybn], writes=["pb%d" % b])
                P.op("act", lambda e, b=b: e.activation(out=gt[:, :Tt], in_=pg[b][:, :Tt], func=AF.Sigmoid), reads=["pg%d" % b], writes=["gt"])
                if i == 0:
                    P.op("dve", lambda e, b=b, dt=dt: e.tensor_tensor(out=mg[:, dt, :Tt], in0=gt[:, :Tt], in1=pb[b][:, :Tt], op=ALU.mult), reads=["gt", "pb%d" % b], writes=[mk_[dt]])
                else:
                    P.op("dve", lambda e, b=b: e.tensor_tensor(out=tm_[:, :Tt], in0=gt[:, :Tt], in1=pb[b][:, :Tt], op=ALU.mult), reads=["gt", "pb%d" % b], writes=["tm_"])
                    P.op("pool", lambda e, dt=dt: e.tensor_tensor(out=mg[:, dt, :Tt], in0=mg[:, dt, :Tt], in1=tm_[:, :Tt], op=ALU.add), reads=["tm_", mk_[dt]], writes=[mk_[dt]])
        for dt in range(KC):
            b = cnt[0] % 2
            cnt[0] += 1
            P.dma("sp", lambda e, b=b, dt=dt: e.dma_start(out=wgt[b][:], in_=wov[:, :, dt * 128:(dt + 1) * 128]), writes=["wgt%d" % b])
            for c in range(KC):
                P.op("pe", lambda e, b=b, c=c: e.matmul(pg[b][:, :Tt], lhsT=wgt[b][:, c, :], rhs=mg[:, c, :Tt], start=(c == 0), stop=(c == KC - 1)), reads=["wgt%d" % b, mk_[c]], writes=["pg%d" % b])
            P.op("dve", lambda e, b=b, dt=dt: e.tensor_scalar(out=tm_[:, :Tt], in0=pg[b][:, :Tt], scalar1=mods[:, dt, sid, 2:3], scalar2=None, op0=ALU.mult), reads=["pg%d" % b, "mods"], writes=["tm_"])
            P.op("dve", lambda e, dt=dt: e.scalar_tensor_tensor(out=hs[:, dt, :Tt], in0=xs[:, dt, :Tt], scalar=ALPHA_DN, in1=tm_[:, :Tt], op0=ALU.mult, op1=ALU.add), reads=["tm_", xk[dt]], writes=[hk[dt]])
        ln_affine(P, C, hs, hk, xs, xk, Tt, lambda c: lns[:, c, 0:1], lambda c: lns[:, c, 1:2], ["lns"], tmp, pst)
        key = ("xl1", t0); outs.append(key)
        P.dma("sp", lambda e: e.dma_start(out=xl1T.rearrange("(c p) t -> p c t", p=128)[:, :, t0:t0 + Tt], in_=xs[:, :, :Tt]), reads=xk, writes=[key])
        ln_affine(P, C, xs, xk, hs, hk, Tt, lambda c: mods[:, c, sid, 3:4], lambda c: mods[:, c, sid, 4:5], ["mods"], tmp, pst)
        for c in range(KC):
            P.op("pe", lambda e, c=c: e.matmul(pg[0][:16, :Tt], lhsT=wrs[:, c, :], rhs=hs[:, c, :Tt], start=(c == 0), stop=(c == KC - 1)), reads=["wrs", hk[c]], writes=["pg0"])
        P.op("act", lambda e: e.activation(out=ex[:, :Tt], in_=pg[0][:16, :Tt], func=AF.Exp), reads=["pg0"], writes=["ex"])
        P.op("pe", lambda e: e.matmul(pb[0][:16, :Tt], lhsT=ones[:16, :16], rhs=ex[:, :Tt], start=True, stop=True), reads=["ex", "c_ones"], writes=["pb0"])
        P.op("dve", lambda e: e.reciprocal(out=gt[:16, :Tt], in_=pb[0][:16, :Tt]), reads=["pb0"], writes=["gt"])
        P.op("dve", lambda e: e.tensor_tensor(out=ex[:, :Tt], in0=ex[:, :Tt], in1=gt[:16, :Tt], op=ALU.mult), reads=["ex", "gt"], writes=["ex"])
        key = ("aff", t0); outs.append(key)
        P.dma("sp", lambda e: e.dma_start(out=affT[:, t0:t0 + Tt], in_=ex[:, :Tt]), reads=["ex"], writes=[key])

    for (t0_, Tt_, sid_) in token_tiles(segs, TT):
        tile_body(t0_, Tt_, sid_)
    P.finish_wait("sp", outs)
    P.emit()
    return nc


def build_stageE(Tc, segs, S, NE=16, TT=256):
    nc = new_nc()
    nseg = len(segs)
    FF = 1408
    FC = FF // 128
    din = lambda name, shp: nc.dram_tensor(name, shp, F32, kind="ExternalInput").ap()
    xl1T = din("xl1T", [D, Tc]); modE = din("modE", [128, KC, nseg, 3]); lnp = din("lnp", [128, KC, 2])
    affL = din("affL", [16, S]); affC = din("affC", [16, 256]); affown = din("affown", [16, Tc]); sel = din("sel", [16, 16, 128])
    Wge = din("Wge", [NE, D, FF]); Wue = din("Wue", [NE, D, FF]); Wde = din("Wde", [NE, FF, D])
    xoT = nc.dram_tensor("xoT", [D, Tc], F32, kind="ExternalOutput").ap()
    P = Prog(nc)
    P.setup_sems()
    C = consts(P)
    tmp = {"sq": P.sb("sq", [128, KC, TT]), "mean": P.sb("mean", [128, TT]), "var": P.sb("var", [128, TT]), "rstd": P.sb("rstd", [128, TT])}
    pst = [P.ps("pst0", [128, TT]), P.ps("pst1", [128, TT])]
    mods = P.sb("mods", [128, KC, nseg, 3]); lns = P.sb("lns", [128, KC, 2]); sels = P.sb("sels", [16, 16, 128])
    P.dma("sp", lambda e: e.dma_start(out=mods[:], in_=modE), writes=["mods"])
    P.dma("sp", lambda e: e.dma_start(out=lns[:], in_=lnp), writes=["lns"])
    P.dma("sp", lambda e: e.dma_start(out=sels[:], in_=sel), writes=["sels"])
    P.op("dve", lambda e: e.tensor_scalar(out=mods[:, :, :, 1:2], in0=mods[:, :, :, 1:2], scalar1=1.0, scalar2=None, op0=ALU.add), reads=["mods"], writes=["mods"])
    work = P.sb("work", [16, S]); mx = P.sb("mx", [16, 8]); thr = P.sb("thr", [16, 2]); wown = P.sb("wown", [16, Tc]); msk = P.sb("msk", [16, Tc])

    def threshold(src, n, col):
        cap = n // 8
        P.dma("sp", lambda e: e.dma_start(out=work[:, :n], in_=src), writes=["work"])
        for it in range(cap // 8):
            P.op("dve", lambda e: e.max(out=mx[:], in_=work[:, :n]), reads=["work"], writes=["mx"])
            if it < cap // 8 - 1:
                P.op("dve", lambda e: e.match_replace(out=work[:, :n], in_to_replace=mx[:], in_values=work[:, :n], imm_value=-1.0), reads=["mx", "work"], writes=["work"])
        P.op("dve", lambda e: e.tensor_reduce(out=thr[:, col:col + 1], in_=mx[:], axis=AX.X, op=ALU.min), reads=["mx"], writes=["thr"])

    threshold(affL, S, 0)
    threshold(affC, 256, 1)
    P.dma("sp", lambda e: e.dma_start(out=wown[:], in_=affown), writes=["wown"])
    for (s0, sn, sid) in segs:
        P.op("dve", lambda e, s0=s0, sn=sn, sid=sid: e.tensor_scalar(out=msk[:, s0:s0 + sn], in0=wown[:, s0:s0 + sn], scalar1=thr[:, sid:sid + 1], scalar2=None, op0=ALU.is_ge), reads=["wown", "thr"], writes=["msk"])
    P.op("dve", lambda e: e.tensor_tensor(out=wown[:], in0=wown[:], in1=msk[:], op=ALU.mult), reads=["wown", "msk"], writes=["wown"])

    xs = P.sb("xs", [128, KC, TT]); hs = P.sb("hs", [128, KC, TT]); acc = P.sb("acc", [128, KC, TT]); hid = P.sb("hid", [128, FC, TT])
    wg = [P.sb("wg%d" % i, [128, KC, 128]) for i in range(2)]; wu = [P.sb("wu%d" % i, [128, KC, 128]) for i in range(2)]; wd = [P.sb("wd%d" % i, [128, FC, 128]) for i in range(2)]
    wbc = P.sb("wbc", [128, TT]); sg = P.sb("sg", [128, TT]); tm_ = P.sb("tm_", [128, TT])
    pg = P.ps("pg", [128, TT]); pu = P.ps("pu", [128, TT]); pd = [P.ps("pd%d" % i, [128, TT]) for i in range(2)]; pw = P.ps("pw", [128, TT])
    xk = [("xs", c) for c in range(KC)]; hk = [("hs", c) for c in range(KC)]; ak = [("acc", c) for c in range(KC)]; hidk = [("hid", f) for f in range(FC)]
    xv = xl1T.rearrange("(c p) t -> p c t", p=128)
    outs = []
    cnt = [0, 0]

    def tile_body(t0, Tt, sid):
        P.dma("sp", lambda e: e.dma_start(out=xs[:, :, :Tt], in_=xv[:, :, t0:t0 + Tt]), writes=xk)
        ln_affine(P, C, xs, xk, hs, hk, Tt, lambda c: mods[:, c, sid, 0:1], lambda c: mods[:, c, sid, 1:2], ["mods"], tmp, pst)
        for ex in range(NE):
            P.op("pe", lambda e, ex=ex: e.matmul(pw[:, :Tt], lhsT=sels[:, ex, :], rhs=wown[:, t0:t0 + Tt], start=True, stop=True), reads=["sels", "wown"], writes=["pw"])
            P.op("act", lambda e: e.copy(out=wbc[:, :Tt], in_=pw[:, :Tt]), reads=["pw"], writes=["wbc"])
            wgv = Wge[ex].rearrange("(c p) f -> p c f", p=128); wuv = Wue[ex].rearrange("(c p) f -> p c f", p=128); wdv = Wde[ex].rearrange("(c p) d -> p c d", p=128)
            for f in range(FC):
                b = cnt[0] % 2
                cnt[0] += 1
                P.dma("sp", lambda e, b=b, f=f, wgv=wgv: e.dma_start(out=wg[b][:], in_=wgv[:, :, f * 128:(f + 1) * 128]), writes=["wg%d" % b])
                P.dma("act", lambda e, b=b, f=f, wuv=wuv: e.dma_start(out=wu[b][:], in_=wuv[:, :, f * 128:(f + 1) * 128]), writes=["wu%d" % b])
                for c in range(KC):
                    P.op("pe", lambda e, b=b, c=c: e.matmul(pg[:, :Tt], lhsT=wg[b][:, c, :], rhs=hs[:, c, :Tt], start=(c == 0), stop=(c == KC - 1)), reads=["wg%d" % b, hk[c]], writes=["pg"])
                for c in range(KC):
                    P.op("pe", lambda e, b=b, c=c: e.matmul(pu[:, :Tt], lhsT=wu[b][:, c, :], rhs=hs[:, c, :Tt], start=(c == 0), stop=(c == KC - 1)), reads=["wu%d" % b, hk[c]], writes=["pu"])
                P.op("act", lambda e: e.activation(out=sg[:, :Tt], in_=pg[:, :Tt], func=AF.Silu), reads=["pg"], writes=["sg"])
                P.op("dve", lambda e: e.tensor_tensor(out=sg[:, :Tt], in0=sg[:, :Tt], in1=pu[:, :Tt], op=ALU.mult), reads=["sg", "pu"], writes=["sg"])
                P.op("pool", lambda e, f=f: e.tensor_tensor(out=hid[:, f, :Tt], in0=sg[:, :Tt], in1=wbc[:, :Tt], op=ALU.mult), reads=["sg", "wbc"], writes=[hidk[f]])
            for dt in range(KC):
                b = cnt[1] % 2
                cnt[1] += 1
                P.dma("pool", lambda e, b=b, dt=dt, wdv=wdv: e.dma_start(out=wd[b][:], in_=wdv[:, :, dt * 128:(dt + 1) * 128]), writes=["wd%d" % b])
                for f in range(FC):
                    P.op("pe", lambda e, b=b, f=f: e.matmul(pd[b][:, :Tt], lhsT=wd[b][:, f, :], rhs=hid[:, f, :Tt], start=(f == 0), stop=(f == FC - 1)), reads=["wd%d" % b, hidk[f]], writes=["pd%d" % b])
                if ex == 0:
                    P.op("dve", lambda e, b=b, dt=dt: e.tensor_copy(out=acc[:, dt, :Tt], in_=pd[b][:, :Tt]), reads=["pd%d" % b], writes=[ak[dt]])
                else:
                    P.op("dve", lambda e, b=b, dt=dt: e.tensor_tensor(out=acc[:, dt, :Tt], in0=acc[:, dt, :Tt], in1=pd[b][:, :Tt], op=ALU.add), reads=["pd%d" % b, ak[dt]], writes=[ak[dt]])
        for dt in range(KC):
            P.op("dve", lambda e, dt=dt: e.tensor_scalar(out=tm_[:, :Tt], in0=acc[:, dt, :Tt], scalar1=mods[:, dt, sid, 2:3], scalar2=None, op0=ALU.mult), reads=[ak[dt], "mods"], writes=["tm_"])
            P.op("dve", lambda e, dt=dt: e.scalar_tensor_tensor(out=hs[:, dt, :Tt], in0=xs[:, dt, :Tt], scalar=ALPHA_DN, in1=tm_[:, :Tt], op0=ALU.mult, op1=ALU.add), reads=["tm_", xk[dt]], writes=[hk[dt]])
        ln_affine(P, C, hs, hk, xs, xk, Tt, lambda c: lns[:, c, 0:1], lambda c: lns[:, c, 1:2], ["lns"], tmp, pst)
        key = ("xo", t0); outs.append(key)
        P.dma("sp", lambda e: e.dma_start(out=xoT.rearrange("(c p) t -> p c t", p=128)[:, :, t0:t0 + Tt], in_=xs[:, :, :Tt]), reads=xk, writes=[key])

    for (t0_, Tt_, sid_) in token_tiles(segs, TT):
        tile_body(t0_, Tt_, sid_)
    P.finish_wait("sp", outs)
    P.emit()
    return nc


SEQ = 8192
CTX = 256
MLA_COLS, SSM_COLS, HY_COLS, RW_COLS = 1088, 2592, 3072, 3488
N_CORES = 8


def _c(a):
    return np.ascontiguousarray(a, dtype=np.float32)


def _run(nc, in_maps):
    return run_bass_kernel_spmd(nc, in_maps, core_ids=list(range(N_CORES))).results


def _stageA(c, c_ctx, w_ada, b_ada):
    ncols = 2 * 12288 // N_CORES
    cT = _c(np.stack([c[0], c[1], c_ctx], 1))
    maps = []
    for k in range(N_CORES):
        sl = slice(k * 1536, (k + 1) * 1536)
        wA = np.concatenate([w_ada[0][:, sl], w_ada[1][:, sl]], 1)
        bA = np.concatenate([b_ada[0][sl], b_ada[1][sl]]).reshape(ncols // 128, 128).T
        maps.append({"cT": cT, "wA": _c(wA), "bA": _c(bA)})
    res = _run(build_stageA(ncols), maps)
    mod = np.zeros((2, 12288, 3), np.float32)
    for k in range(N_CORES):
        m = res[k]["modT"].transpose(1, 0, 2).reshape(ncols, 3)
        mod[0, k * 1536:(k + 1) * 1536] = m[:1536]
        mod[1, k * 1536:(k + 1) * 1536] = m[1536:]
    return mod


_SWAP = np.arange(64).reshape(2, 2, 16)[:, ::-1, :].reshape(64)


def _stageB(x, ctx, mod, w_in_l, S):
    Tl = S // 4
    Tc = Tl + 64
    cols = np.concatenate([np.arange(MLA_COLS), 1024 + _SWAP, np.arange(MLA_COLS, MLA_COLS + SSM_COLS + HY_COLS + RW_COLS)])
    NB = ((len(cols) + 127) // 128) * 128
    Wb = np.zeros((D, NB), np.float32)
    Wb[:, :len(cols)] = w_in_l[:, cols]
    maps = []
    for k in range(N_CORES):
        b, q = k // 4, k % 4
        xT = np.concatenate([x[b, q * Tl:(q + 1) * Tl], ctx[b, q * 64:(q + 1) * 64]], 0).T
        md = np.stack([np.stack([mod[:D, b], mod[D:2 * D, b]], -1), np.stack([mod[:D, 2], mod[D:2 * D, 2]], -1)], 1)
        md = md.reshape(KC, 128, 2, 2).transpose(1, 0, 2, 3)
        maps.append({"xT": _c(xT), "modB": _c(md), "Wb": Wb})
    res = _run(build_stageB(Tc, [(0, Tl, 0), (Tl, 64, 1)], NB), maps)
    TOT = 2 * S + 512
    u = np.zeros((TOT, len(cols)), np.float32)
    for k in range(N_CORES):
        b, q = k // 4, k % 4
        uk = res[k]["uT"].T[:, :len(cols)]
        u[b * S + q * Tl:b * S + (q + 1) * Tl] = uk[:Tl]
        u[2 * S + b * 256 + q * 64:2 * S + b * 256 + (q + 1) * 64] = uk[Tl:]
    return u


def _rope_tables(S):
    rows = S // 64
    row = np.repeat(np.arange(rows), 64).astype(np.float32)
    col = np.tile(np.arange(64), rows).astype(np.float32)
    inv = (10000.0 ** (-np.arange(0, 32, 2, dtype=np.float32) / 32)).astype(np.float32)
    ang = np.stack([row[:, None] * inv, col[:, None] * inv], 1)
    cos, sin = np.cos(ang).astype(np.float32), np.sin(ang).astype(np.float32)
    cosT = np.stack([cos, cos], 2).reshape(S, 64).T
    sinT = np.stack([-sin, sin], 2).reshape(S, 64).T
    return _c(cosT), _c(sinT)


def _mla(u, p, S, need_ctx):
    um = u[:, :MLA_COLS + 64]
    cosT, sinT = _rope_tables(S)
    wqu = p["mla_w_q_up"].reshape(512, 8, 192)
    wkvu = p["mla_w_kv_up"].reshape(512, 8, 256)
    shared = {"cqT": _c(um[:, :512].T), "ckvT": _c(um[:, 512:1024].T), "kpeT": _c(um[:, 1024:1088].T), "kpeswT": _c(um[:, 1088:1152].T),
              "gq": _c(p["mla_q_norm"].reshape(4, 128).T), "gkv": _c(p["mla_kv_norm"].reshape(4, 128).T), "cosT": cosT, "sinT": sinT}
    maps = []
    for h in range(8):
        wq = np.concatenate([wqu[:, h, :128], wqu[:, h, 128:], wqu[:, h, 128:][:, _SWAP]], 1)
        maps.append(dict(shared, wq=_c(wq), wkv=_c(wkvu[:, h])))
    res = _run(build_mla(S, need_ctx), maps)
    return np.concatenate([res[h]["o"] for h in range(8)], 1)


def _ssd(u, p, S):
    us = u[:, MLA_COLS + 64:MLA_COLS + 64 + SSM_COLS]
    maps = []
    for core in range(8):
        ch = np.arange(core * 128, (core + 1) * 128)
        g = core // 4
        allc = np.concatenate([1024 + ch, 2048 + g * 128 + np.arange(128), 2304 + g * 128 + np.arange(128)])
        cwf = np.concatenate([p["ssm_conv_w"], p["ssm_conv_b"][None]], 0)[:, allc - 1024]
        cw = cwf.T.reshape(3, 128, 4).transpose(1, 0, 2)
        hs = [2 * core, 2 * core + 1]
        dtc = 2560 + np.array([hs[0], hs[1], 16 + hs[0], 16 + hs[1]])
        sel = ([0, 0, 1, 1], [hs[0], hs[1], hs[0], hs[1]])
        maps.append({"zT": _c(us[:, ch].T), "xbcT": _c(us[:, allc].T), "cw": _c(cw), "dttok": _c(us[:, dtc]),
                     "dtb": _c(np.broadcast_to(p["ssm_dt_bias"][sel], (128, 4))), "alog": _c(np.broadcast_to(p["ssm_a_log"][sel], (128, 4))),
                     "Dp": _c(np.repeat(p["ssm_d"][hs], 64)[:, None]), "ident": np.eye(128, dtype=np.float32), "triU": _c(np.triu(np.ones((128, 128))))})
    res = _run(build_ssd(S), maps)
    return np.concatenate([res[k]["yT"].T for k in range(8)], 1)


def _hy_consts(n):
    t = np.linspace(0.0, 1.0, n, dtype=np.float32)[:, None]
    wpos = (2.0 * math.pi * np.arange(n, dtype=np.float32)[:, None] / n).astype(np.float32)
    f = np.linspace(1e-4, 15, 16, dtype=np.float32)[None]
    z = np.concatenate([t, np.cos(f * wpos), -np.sin(f * wpos)], -1).astype(np.float32)
    lo, hi = math.log(1e-2) / 1.5, math.log(1e-2) / 0.3
    deltas = np.abs(np.linspace(lo, hi, 1024, dtype=np.float32))
    return _c(z.T), np.exp(-t * deltas).astype(np.float32)


def _hyena(u, p, S, need_ctx):
    c0 = MLA_COLS + 64 + SSM_COLS
    uh = u[:, c0:c0 + HY_COLS]
    zL, winL = _hy_consts(S)
    zC, winC = _hy_consts(256)
    maps = []
    for core in range(8):
        ch = np.arange(core * 128, (core + 1) * 128)
        cols = np.concatenate([ch, 1024 + ch, 2048 + ch])
        cwf = np.concatenate([p["hy_conv_w"], p["hy_conv_b"][None]], 0)[:, cols]
        cw = cwf.T.reshape(3, 128, 4).transpose(1, 0, 2)
        w3c = np.concatenate([p["hy_w3"][:, ch], p["hy_w3"][:, 1024 + ch]], 1)
        fp = np.stack([p["hy_b1"], p["hy_b2"], p["hy_freq"], np.zeros(64, np.float32)], 1)
        maps.append({"uH": _c(uh[:, cols].T), "cw": _c(cw), "hd": _c(p["hy_d"][ch][:, None]), "zL": zL, "zC": zC, "w1": _c(p["hy_w1"]), "w2": _c(p["hy_w2"]),
                     "w3": _c(w3c), "fp": _c(fp), "winL": _c(winL[:, ch].T), "winC": _c(winC[:, ch].T)})
    res = _run(build_hyena(S, need_ctx), maps)
    return np.concatenate([res[k]["yT"].T for k in range(8)], 1)


def _rwkv(u, p, S):
    c0 = MLA_COLS + 64 + SSM_COLS + HY_COLS
    ur = u[:, c0:c0 + RW_COLS]
    blk = np.kron(np.eye(2, dtype=np.float32), np.ones((64, 64), np.float32))
    maps = []
    for core in range(8):
        ch = slice(core * 128, (core + 1) * 128)
        cols = np.concatenate([np.arange(1024)[ch], 1024 + np.arange(1024)[ch], 2048 + np.arange(1024)[ch], np.arange(3072, 3488)])
        mup = np.zeros((2, 896), np.float32)
        mup[:, :800] = p["rw_mu"][:, cols]
        pc = np.zeros((128, 16), np.float32)
        pc[:, 0] = p["rw_kk"][ch]; pc[:, 1] = p["rw_ka"][ch]; pc[:, 2] = p["rw_rk"].reshape(-1)[ch]
        pc[:, 3] = p["rw_ln_g"][ch]; pc[:, 4] = p["rw_ln_b"][ch]
        pc[:, 5] = p["rw_w0"][0, ch]; pc[:, 6] = p["rw_w0"][1, ch]; pc[:, 7] = p["rw_a0"][0, ch]; pc[:, 8] = p["rw_a0"][1, ch]
        maps.append({"uR": _c(ur[:, cols].T), "mu": _c(mup.reshape(2, 7, 128).transpose(2, 1, 0)), "pc": pc,
                     "wup": _c(p["rw_w_up"][:, :, ch].reshape(128, 128)), "aup": _c(p["rw_a_up"][:, :, ch].reshape(128, 128)),
                     "gup": _c(p["rw_g_up"][:, ch]), "ident": np.eye(128, dtype=np.float32), "blk": blk})
    res = _run(build_rwkv(S), maps)
    return np.concatenate([res[k]["yT"].T for k in range(8)], 1)


def _core_tokens(arr_lat, arr_ctx, k, S):
    Tl = S // 4
    b, q = k // 4, k % 4
    return np.concatenate([arr_lat[b, q * Tl:(q + 1) * Tl], arr_ctx[b, q * 64:(q + 1) * 64]], 0)


def _stageD(x, ctx, ys, mod, p, S):
    Tl = S // 4
    Tc = Tl + 64
    lnp = _c(np.stack([p["ln1_b"], p["ln1_g"]], -1).reshape(KC, 128, 2).transpose(1, 0, 2))
    shared = {"lnp": lnp, "ssg": _c(p["ssm_norm"].reshape(8, 128).T), "Wg": _c(p["w_in"][:, 10240:]), "Wbr": _c(p["w_branch"]), "Wo": _c(p["w_out"]), "Wr": _c(p["w_router"])}
    m6 = mod.reshape(6, D, 3)
    maps = []
    for k in range(N_CORES):
        b = k // 4
        xT = _core_tokens(x, ctx, k, S).T
        ybT = np.stack([_core_tokens(y[:2 * S].reshape(2, S, 1024), y[2 * S:].reshape(2, 256, 1024), k, S).T for y in ys], 0)
        md = np.stack([m6[[0, 1, 2, 3, 4], :, b].T, m6[[0, 1, 2, 3, 4], :, 2].T], 1).reshape(KC, 128, 2, 5).transpose(1, 0, 2, 3)
        maps.append(dict(shared, xT=_c(xT), ybT=_c(ybT), modD=_c(md)))
    res = _run(build_stageD(Tc, [(0, Tl, 0), (Tl, 64, 1)]), maps)
    return [res[k]["xl1T"] for k in range(N_CORES)], [res[k]["affT"] for k in range(N_CORES)]


def _stageE(xl1T, affT, mod, p, S):
    Tl = S // 4
    Tc = Tl + 64
    lnp = _c(np.stack([p["ln2_b"], p["ln2_g"]], -1).reshape(KC, 128, 2).transpose(1, 0, 2))
    sel = np.zeros((16, 16, 128), np.float32)
    for e in range(16):
        sel[e, e, :] = 1.0
    shared = {"lnp": lnp, "sel": sel, "Wge": _c(p["w_gate_e"]), "Wue": _c(p["w_up_e"]), "Wde": _c(p["w_down_e"])}
    affL = [np.concatenate([affT[4 * b + q][:, :Tl] for q in range(4)], 1) for b in range(2)]
    affC = [np.concatenate([affT[4 * b + q][:, Tl:] for q in range(4)], 1) for b in range(2)]
    m6 = mod.reshape(6, D, 3)
    maps = []
    for k in range(N_CORES):
        b = k // 4
        md = np.stack([m6[[3, 4, 5], :, b].T, m6[[3, 4, 5], :, 2].T], 1).reshape(KC, 128, 2, 3).transpose(1, 0, 2, 3)
        maps.append(dict(shared, xl1T=_c(xl1T[k]), modE=_c(md), affL=_c(affL[b]), affC=_c(affC[b]), affown=_c(affT[k])))
    res = _run(build_stageE(Tc, [(0, Tl, 0), (Tl, 64, 1)], S), maps)
    x_new = np.zeros((2, S, D), np.float32)
    c_new = np.zeros((2, 256, D), np.float32)
    for k in range(N_CORES):
        b, q = k // 4, k % 4
        o = res[k]["xoT"].T
        x_new[b, q * Tl:(q + 1) * Tl] = o[:Tl]
        c_new[b, q * 64:(q + 1) * 64] = o[Tl:]
    return x_new, c_new


def kernel(**inputs):
    inp = {k: np.asarray(v) for k, v in inputs.items()}
    x, c, ctx, c_ctx = inp["x"], inp["c"], inp["ctx"], inp["c_ctx"]
    S = x.shape[1]
    depth = inp["w_in"].shape[0]
    mod = _stageA(c, c_ctx, inp["w_ada"], inp["b_ada"])
    skip = ("x", "c", "ctx", "c_ctx", "w_ada", "b_ada")
    for layer in range(depth):
        p = {k: v[layer] for k, v in inp.items() if k not in skip}
        u = _stageB(x, ctx, mod[layer], p["w_in"], S)
        ys = [_mla(u, p, S, True), _ssd(u, p, S), _hyena(u, p, S, True), _rwkv(u, p, S)]
        del u
        xl1T, affT = _stageD(x, ctx, ys, mod[layer], p, S)
        del ys
        x, ctx = _stageE(xl1T, affT, mod[layer], p, S)
    return np.ascontiguousarray(x, dtype=np.float32)
```

```python
import math
import numpy as np
from concourse.bass_utils import run_bass_kernel_spmd
import concourse.bass as bass
import concourse.mybir as mybir

F32 = mybir.dt.float32
BF16 = mybir.dt.bfloat16
I32 = mybir.dt.int32
AF = mybir.ActivationFunctionType
ALU = mybir.AluOpType
AX = mybir.AxisListType

COMPUTE = ("pe", "act", "dve", "pool")
NDSEM = 6


_SHARED = {"nc": None, "P": None, "prefix": ""}


class _NcProxy:
    def __init__(self, nc, prefix):
        self._nc = nc
        self._prefix = prefix

    def dram_tensor(self, name, *a, **k):
        return self._nc.dram_tensor(self._prefix + name, *a, **k)

    def __getattr__(self, n):
        return getattr(self._nc, n)


def Prog(nc):
    if _SHARED["P"] is not None:
        P = _SHARED["P"]
        P.nc = nc
        P.prefix = _SHARED["prefix"]
        return P
    return _Prog(nc)


class _Prog:
    def __init__(self, nc, same_engine_sync=True):
        self.prefix = ""
        self._sctx = []
        self.nc = nc
        self.ops = {e: [] for e in ("pe", "act", "dve", "pool", "sp")}
        self.cnt = {e: 0 for e in COMPUTE}
        self.waited = {e: {} for e in self.ops}
        self.res = {}
        self.same = same_engine_sync
        self.stack = []
        self.sems = {}
        self.dma_slots = {}
        self.dma_rr = {}
        self._ctx = []

    def enter(self, cm):
        v = cm.__enter__()
        self._ctx.append(cm)
        return v

    def sb(self, name, shape, dt=F32):
        return self.enter(self.nc.sbuf_tensor(self.prefix + name, list(shape), dt))

    def ps(self, name, shape, dt=F32):
        return self.enter(self.nc.psum_tensor(self.prefix + name, list(shape), dt))

    def _sem(self, name):
        cm = self.nc.semaphore(name)
        v = cm.__enter__()
        self._sctx.append(cm)
        return v

    def setup_sems(self, dma_queues=("sp", "act", "pool")):
        if self.sems:
            return
        for e in COMPUTE:
            self.sems[e] = self._sem("s_" + e)
        for q in dma_queues:
            self.dma_slots[q] = [[self._sem("d_%s%d" % (q, i)), 0] for i in range(NDSEM)]
            self.dma_rr[q] = 0

    def close(self):
        for cm in reversed(self._sctx):
            cm.__exit__(None, None, None)
        self._sctx = []

    def _need(self, eng, tok, waits):
        if tok is None:
            return
        kind, key, val = tok
        if kind == "E":
            if key == eng and (eng == "pe" or not self.same):
                return
            sem = self.sems[key]
        else:
            sem = key
        sid = id(sem)
        if self.waited[eng].get(sid, 0) >= val:
            return
        cur = waits.get(sid)
        if cur is None or cur[1] < val:
            waits[sid] = (sem, val)

    def _deps(self, eng, reads, writes):
        waits = {}
        for r in reads:
            st = self.res.get(r)
            if st:
                self._need(eng, st["w"], waits)
        for w in writes:
            st = self.res.get(w)
            if st:
                self._need(eng, st["w"], waits)
                for t in st["r"].values():
                    self._need(eng, t, waits)
        for sid, (sem, val) in waits.items():
            self.waited[eng][sid] = val
        return list(waits.values())

    def _commit(self, tok, reads, writes):
        for r in reads:
            st = self.res.setdefault(r, {"w": None, "r": {}})
            k = (tok[0], tok[1] if tok[0] == "E" else id(tok[1]))
            old = st["r"].get(k)
            if old is None or old[2] < tok[2]:
                st["r"][k] = tok
        for w in writes:
            self.res[w] = {"w": tok, "r": {}}

    def op(self, eng, fn, reads=(), writes=()):
        waits = self._deps(eng, reads, writes)
        self.cnt[eng] += 1
        n = self.cnt[eng]
        sem = self.sems[eng]
        self.ops[eng].append((waits, fn, sem, 1))
        self._commit(("E", eng, n), reads, writes)

    def dma(self, q, fn, reads=(), writes=()):
        waits = self._deps(q, reads, writes)
        slots = self.dma_slots[q]
        i = self.dma_rr[q]
        self.dma_rr[q] = (i + 1) % len(slots)
        sem, c = slots[i]
        if c > 0 and self.waited[q].get(id(sem), 0) < c:
            waits.append((sem, c))
            self.waited[q][id(sem)] = c
        slots[i][1] = c + 16
        self.ops[q].append((waits, fn, sem, 16))
        if q in COMPUTE:
            pass
        self._commit(("D", sem, c + 16), reads, writes)

    def finish_wait(self, eng, resources):
        waits = {}
        for r in resources:
            st = self.res.get(r)
            if st:
                self._need(eng, st["w"], waits)
        self.ops[eng].append((list(waits.values()), None, None, 0))

    def emit(self):
        self._emit_block()
        for cm in reversed(self._ctx):
            cm.__exit__(None, None, None)
        self._ctx = []
        if _SHARED["P"] is self:
            self.ops = {e: [] for e in ("pe", "act", "dve", "pool", "sp")}
            allw = [(self.sems[e], self.cnt[e]) for e in COMPUTE if self.cnt[e] > 0]
            for q, slots in self.dma_slots.items():
                allw += [(sem, c) for sem, c in slots if c > 0]
            for e in self.ops:
                w = [(sm, v) for sm, v in allw if self.waited[e].get(id(sm), 0) < v]
                for sm, v in w:
                    self.waited[e][id(sm)] = v
                self.ops[e].append((w, None, None, 0))
            self.res = {}
        else:
            self.close()

    def _emit_block(self):
        nc = self.nc
        engmap = {"pe": "tensor", "act": "scalar", "dve": "vector", "pool": "gpsimd", "sp": "sync"}
        with nc.Block() as block:
            for e, name in engmap.items():
                ops = self.ops[e]

                def body(engine, ops=ops):
                    for waits, fn, sem, inc in ops:
                        for s, v in waits:
                            engine.wait_ge(s, v)
                        if fn is not None:
                            ins = fn(engine)
                            ins.then_inc(sem, inc)

                getattr(block, name)(body)


D = 2048
KC = D // 128
EPS_LN = 1e-6


def new_nc():
    if _SHARED["nc"] is not None:
        return _NcProxy(_SHARED["nc"], _SHARED["prefix"])
    return bass.Bass("TRN2", target_bir_lowering=False)


def build_mixers(S):
    nc = bass.Bass("TRN2", target_bir_lowering=False)
    P = _Prog(nc)
    _SHARED.update(nc=nc, P=P)
    try:
        for prefix, fn in (("mla_", lambda: build_mla(S, True)), ("ssd_", lambda: build_ssd(S)), ("hy_", lambda: build_hyena(S, True)), ("rw_", lambda: build_rwkv(S))):
            _SHARED["prefix"] = prefix
            P.prefix = prefix
            fn()
    finally:
        _SHARED.update(nc=None, P=None, prefix="")
    P.close()
    return nc


def consts(P):
    nc = P.nc
    ones = P.sb("c_ones", [128, 128])
    P.op("pool", lambda e: e.memset(ones[:], 1.0), writes=["c_ones"])
    eps = P.sb("c_eps", [128, 4])
    P.op("pool", lambda e: e.memset(eps[:, 0:1], EPS_LN), writes=["c_eps0"])
    P.op("pool", lambda e: e.memset(eps[:, 1:2], 1e-5), writes=["c_eps1"])
    P.op("pool", lambda e: e.memset(eps[:, 2:3], 64e-5), writes=["c_eps2"])
    P.op("pool", lambda e: e.memset(eps[:, 3:4], 1e-24), writes=["c_eps3"])
    return {"ones": ones, "eps_ln": (eps[:, 0:1], "c_eps0"), "eps_1e5": (eps[:, 1:2], "c_eps1"), "eps_gn": (eps[:, 2:3], "c_eps2"), "eps_tiny": (eps[:, 3:4], "c_eps3")}


def rsqrt_op(P, C, out_ap, out_name, in_ap, in_name, epskey, scale=1.0):
    eps_ap, eps_name = C[epskey]
    np_ = out_ap.shape[0]
    P.op("act", lambda e: e.activation(out=out_ap, in_=in_ap, func=AF.Sqrt, bias=eps_ap[:np_], scale=scale), reads=[in_name, eps_name], writes=[out_name])
    P.op("dve", lambda e: e.reciprocal(out=out_ap, in_=out_ap), reads=[out_name], writes=[out_name])


def build_stageA(ncols):
    nc = new_nc()
    cT = nc.dram_tensor("cT", [D, 3], F32, kind="ExternalInput").ap()
    wA = nc.dram_tensor("wA", [D, ncols], F32, kind="ExternalInput").ap()
    bA = nc.dram_tensor("bA", [128, ncols // 128], F32, kind="ExternalInput").ap()
    out = nc.dram_tensor("modT", [128, ncols // 128, 3], F32, kind="ExternalOutput").ap()
    P = Prog(nc)
    P.setup_sems()
    NT = ncols // 128
    cs = P.sb("cs", [128, KC, 3])
    ss = P.sb("ss", [128, KC, 3])
    bs = P.sb("bs", [128, NT])
    res = P.sb("res", [128, NT, 3])
    wt = [P.sb("wt%d" % i, [128, KC, 128]) for i in range(2)]
    ps = [P.ps("ps%d" % i, [128, 3]) for i in range(2)]
    P.dma("sp", lambda e: e.dma_start(out=cs[:], in_=cT.rearrange("(c p) n -> p c n", p=128)), writes=["cs"])
    P.dma("sp", lambda e: e.dma_start(out=bs[:], in_=bA), writes=["bs"])
    P.op("act", lambda e: e.activation(out=ss[:], in_=cs[:], func=AF.Silu), reads=["cs"], writes=["ss"])
    wv = wA.rearrange("(c p) n -> p c n", p=128)
    for j in range(NT):
        b = j % 2
        P.dma("sp" if b == 0 else "act", lambda e, j=j, b=b: e.dma_start(out=wt[b][:], in_=wv[:, :, j * 128:(j + 1) * 128]), writes=["wt%d" % b])
        for c in range(KC):
            P.op("pe", lambda e, c=c, b=b: e.matmul(ps[b][:], lhsT=wt[b][:, c, :], rhs=ss[:, c, :], start=(c == 0), stop=(c == KC - 1)),
                 reads=["wt%d" % b, "ss"], writes=["ps%d" % b])
        P.op("dve", lambda e, j=j, b=b: e.tensor_scalar(out=res[:, j, :], in0=ps[b][:], scalar1=bs[:, j:j + 1], scalar2=None, op0=ALU.add),
             reads=["ps%d" % b, "bs"], writes=[("res", j)])
    P.dma("sp", lambda e: e.dma_start(out=out, in_=res[:]), reads=[("res", j) for j in range(NT)], writes=["out"])
    P.finish_wait("sp", ["out"])
    P.emit()
    return nc


def ln_modulate_tile(P, C, xs, xname, hs, hname, Tt, modsb, seg, tmp, pst, alpha_res=None):
    ones = C["ones"]
    sq, mean, var, rstd = tmp["sq"], tmp["mean"], tmp["var"], tmp["rstd"]
    P.op("act", lambda e: e.activation(out=sq[:, :, :Tt], in_=xs[:, :, :Tt], func=AF.Square), reads=[xname], writes=["sq"])
    for c in range(KC):
        P.op("pe", lambda e, c=c: e.matmul(pst[0][:, :Tt], lhsT=ones[:], rhs=xs[:, c, :Tt], start=(c == 0), stop=(c == KC - 1)),
             reads=[xname, "c_ones"], writes=["pst0"])
    for c in range(KC):
        P.op("pe", lambda e, c=c: e.matmul(pst[1][:, :Tt], lhsT=ones[:], rhs=sq[:, c, :Tt], start=(c == 0), stop=(c == KC - 1)),
             reads=["sq", "c_ones"], writes=["pst1"])
    P.op("act", lambda e: e.mul(out=mean[:, :Tt], in_=pst[0][:, :Tt], mul=1.0 / D), reads=["pst0"], writes=["mean"])
    P.op("dve", lambda e: e.tensor_tensor(out=var[:, :Tt], in0=mean[:, :Tt], in1=mean[:, :Tt], op=ALU.mult), reads=["mean"], writes=["var"])
    P.op("dve", lambda e: e.scalar_tensor_tensor(out=var[:, :Tt], in0=pst[1][:, :Tt], scalar=1.0 / D, in1=var[:, :Tt], op0=ALU.mult, op1=ALU.subtract),
         reads=["pst1", "var"], writes=["var"])
    rsqrt_op(P, C, rstd[:, :Tt], "rstd", var[:, :Tt], "var", "eps_ln")
    for c in range(KC):
        P.op("dve", lambda e, c=c: e.tensor_tensor(out=hs[:, c, :Tt], in0=xs[:, c, :Tt], in1=mean[:, :Tt], op=ALU.subtract),
             reads=[xname, "mean"], writes=[(hname, c)])
        P.op("pool", lambda e, c=c: e.tensor_tensor(out=hs[:, c, :Tt], in0=hs[:, c, :Tt], in1=rstd[:, :Tt], op=ALU.mult),
             reads=[(hname, c), "rstd"], writes=[(hname, c)])
        if modsb is not None:
            P.op("act", lambda e, c=c: e.activation(out=hs[:, c, :Tt], in_=hs[:, c, :Tt], func=AF.Identity,
                                                    bias=modsb[:, c, seg, 0:1], scale=modsb[:, c, seg, 1:2]),
                 reads=[(hname, c), "modsb"], writes=[(hname, c)])


def ln_tmp(P):
    tmp = {"sq": P.sb("sq", [128, KC, 512]), "mean": P.sb("mean", [128, 512]), "var": P.sb("var", [128, 512]), "rstd": P.sb("rstd", [128, 512])}
    pst = [P.ps("pst0", [128, 512]), P.ps("pst1", [128, 512])]
    return tmp, pst


def token_tiles(segs, tmax=512):
    out = []
    for (s, n, sid) in segs:
        o = 0
        while o < n:
            tt = min(tmax, n - o)
            out.append((s + o, tt, sid))
            o += tt
    return out


def build_stageB(Tc, segs, NB):
    nc = new_nc()
    nseg = len(segs)
    xT = nc.dram_tensor("xT", [D, Tc], F32, kind="ExternalInput").ap()
    modB = nc.dram_tensor("modB", [128, KC, nseg, 2], F32, kind="ExternalInput").ap()
    Wb = nc.dram_tensor("Wb", [D, NB], F32, kind="ExternalInput").ap()
    uT = nc.dram_tensor("uT", [NB, Tc], F32, kind="ExternalOutput").ap()
    P = Prog(nc)
    P.setup_sems()
    C = consts(P)
    tmp, pst = ln_tmp(P)
    modsb = P.sb("modsb", [128, KC, nseg, 2])
    xs = P.sb("xs", [128, KC, 512])
    hs = P.sb("hs", [128, KC, 512])
    wt = [P.sb("wt%d" % i, [128, KC, 128]) for i in range(3)]
    ys = [P.sb("ys%d" % i, [128, 512]) for i in range(2)]
    psg = [P.ps("psg%d" % i, [128, 512]) for i in range(2)]
    P.dma("sp", lambda e: e.dma_start(out=modsb[:], in_=modB), writes=["modsb"])
    P.op("dve", lambda e: e.tensor_scalar(out=modsb[:, :, :, 1:2], in0=modsb[:, :, :, 1:2], scalar1=1.0, scalar2=None, op0=ALU.add), reads=["modsb"], writes=["modsb"])
    xv = xT.rearrange("(c p) t -> p c t", p=128)
    wv = Wb.rearrange("(c p) n -> p c n", p=128)
    outs = []
    it = 0
    for (t0, Tt, sid) in token_tiles(segs):
        P.dma("sp", lambda e, t0=t0, Tt=Tt: e.dma_start(out=xs[:, :, :Tt], in_=xv[:, :, t0:t0 + Tt]), writes=["xs"])
        ln_modulate_tile(P, C, xs, "xs", hs, "hs", Tt, modsb, sid, tmp, pst)
        hreads = [("hs", c) for c in range(KC)]
        for j in range(NB // 128):
            b3 = it % 3
            b = it % 2
            it += 1
            P.dma("act" if it % 2 else "pool", lambda e, j=j, b3=b3: e.dma_start(out=wt[b3][:], in_=wv[:, :, j * 128:(j + 1) * 128]), writes=["wt%d" % b3])
            for c in range(KC):
                P.op("pe", lambda e, c=c, b=b, b3=b3, Tt=Tt: e.matmul(psg[b][:, :Tt], lhsT=wt[b3][:, c, :], rhs=hs[:, c, :Tt], start=(c == 0), stop=(c == KC - 1)),
                     reads=["wt%d" % b3, ("hs", c)], writes=["psg%d" % b])
            if b == 0:
                P.op("dve", lambda e, b=b, Tt=Tt: e.tensor_copy(out=ys[b][:, :Tt], in_=psg[b][:, :Tt]), reads=["psg%d" % b], writes=["ys%d" % b])
            else:
                P.op("act", lambda e, b=b, Tt=Tt: e.copy(out=ys[b][:, :Tt], in_=psg[b][:, :Tt]), reads=["psg%d" % b], writes=["ys%d" % b])
            key = ("uT", j, t0)
            outs.append(key)
            P.dma("sp", lambda e, j=j, b=b, t0=t0, Tt=Tt: e.dma_start(out=uT[j * 128:(j + 1) * 128, t0:t0 + Tt], in_=ys[b][:, :Tt]), reads=["ys%d" % b], writes=[key])
    P.finish_wait("sp", outs)
    P.emit()
    return nc


def build_mla(S, need_ctx):
    nc = new_nc()
    TOT = 2 * S + 512
    dt_in = lambda name, shp: nc.dram_tensor(name, shp, F32, kind="ExternalInput").ap()
    cqT = dt_in("cqT", [512, TOT]); ckvT = dt_in("ckvT", [512, TOT]); kpeT = dt_in("kpeT", [64, TOT]); kpeswT = dt_in("kpeswT", [64, TOT])
    wq = dt_in("wq", [512, 256]); wkv = dt_in("wkv", [512, 256]); gq = dt_in("gq", [128, 4]); gkv = dt_in("gkv", [128, 4])
    cosT = dt_in("cosT", [64, S]); sinT = dt_in("sinT", [64, S])
    o = nc.dram_tensor("o", [TOT, 128], F32, kind="ExternalOutput").ap()
    QT = nc.dram_tensor("QT", [192, TOT], F32, kind="Internal").ap()
    KT = nc.dram_tensor("KT", [192, TOT], F32, kind="Internal").ap()
    V = nc.dram_tensor("V", [TOT, 128], F32, kind="Internal").ap()
    P = Prog(nc)
    P.setup_sems()
    C = consts(P)
    ones = C["ones"]
    wqs = P.sb("wqs", [128, 4, 256]); wkvs = P.sb("wkvs", [128, 4, 256]); gqs = P.sb("gqs", [128, 4]); gkvs = P.sb("gkvs", [128, 4])
    P.dma("sp", lambda e: e.dma_start(out=wqs[:], in_=wq.rearrange("(c p) n -> p c n", p=128)), writes=["wqs"])
    P.dma("sp", lambda e: e.dma_start(out=wkvs[:], in_=wkv.rearrange("(c p) n -> p c n", p=128)), writes=["wkvs"])
    P.dma("sp", lambda e: e.dma_start(out=gqs[:], in_=gq), writes=["gqs"])
    P.dma("sp", lambda e: e.dma_start(out=gkvs[:], in_=gkv), writes=["gkvs"])
    cs = P.sb("cs", [128, 4, 512]); sq = P.sb("sq", [128, 4, 512]); rstd = P.sb("rstd", [128, 512])
    pes = P.sb("pes", [64, 2, 512]); tabs = P.sb("tabs", [64, 2, 512]); rt = P.sb("rt", [64, 2, 512])
    ev = P.sb("ev", [128, 512])
    pA = P.ps("pA", [128, 512]); pB = P.ps("pB", [128, 512])
    segs = [(0, S, 0), (S, S, 0), (2 * S, 256, 1), (2 * S + 256, 256, 1)]

    def rms_tile(src, gsb, gname, t0, Tt):
        P.dma("sp", lambda e: e.dma_start(out=cs[:, :, :Tt], in_=src.rearrange("(c p) t -> p c t", p=128)[:, :, t0:t0 + Tt]), writes=["cs"])
        P.op("act", lambda e: e.activation(out=sq[:, :, :Tt], in_=cs[:, :, :Tt], func=AF.Square), reads=["cs"], writes=["sq"])
        for c in range(4):
            P.op("pe", lambda e, c=c: e.matmul(pA[:, :Tt], lhsT=ones[:], rhs=sq[:, c, :Tt], start=(c == 0), stop=(c == 3)), reads=["sq", "c_ones"], writes=["pA"])
        rsqrt_op(P, C, rstd[:, :Tt], "rstd", pA[:, :Tt], "pA", "eps_ln", scale=1.0 / 512)
        for c in range(4):
            P.op("dve", lambda e, c=c: e.scalar_tensor_tensor(out=cs[:, c, :Tt], in0=cs[:, c, :Tt], scalar=gsb[:, c:c + 1], in1=rstd[:, :Tt], op0=ALU.mult, op1=ALU.mult),
                 reads=["cs", "rstd", gname], writes=["cs"])

    def proj_fm(ws, wname, c0, ncol, dst, dstrow, t0, Tt, rope_src=None):
        for c in range(4):
            P.op("pe", lambda e, c=c: e.matmul(pB[:ncol, :Tt], lhsT=ws[:, c, c0:c0 + ncol], rhs=cs[:, c, :Tt], start=(c == 0), stop=(c == 3)), reads=[wname, "cs"], writes=["pB"])
        P.op("act", lambda e: e.copy(out=ev[:ncol, :Tt], in_=pB[:ncol, :Tt]), reads=["pB"], writes=["ev"])
        P.dma("sp", lambda e: e.dma_start(out=dst[dstrow:dstrow + ncol, t0:t0 + Tt], in_=ev[:ncol, :Tt]), reads=["ev"], writes=[("scr", dstrow, t0)])

    def rope_store(dst, t0, Tt, sid, tl):
        if sid == 0:
            P.dma("act", lambda e: e.dma_start(out=tabs[:, 0, :Tt], in_=cosT[:, tl:tl + Tt]), writes=["tabs0"])
            P.dma("act", lambda e: e.dma_start(out=tabs[:, 1, :Tt], in_=sinT[:, tl:tl + Tt]), writes=["tabs1"])
            P.op("dve", lambda e: e.tensor_tensor(out=rt[:, 0, :Tt], in0=pes[:, 0, :Tt], in1=tabs[:, 0, :Tt], op=ALU.mult), reads=["pes", "tabs0"], writes=["rt0"])
            P.op("pool", lambda e: e.tensor_tensor(out=rt[:, 1, :Tt], in0=pes[:, 1, :Tt], in1=tabs[:, 1, :Tt], op=ALU.mult), reads=["pes", "tabs1"], writes=["rt1"])
            P.op("dve", lambda e: e.tensor_tensor(out=rt[:, 0, :Tt], in0=rt[:, 0, :Tt], in1=rt[:, 1, :Tt], op=ALU.add), reads=["rt0", "rt1"], writes=["rt0"])
            P.dma("sp", lambda e: e.dma_start(out=dst[128:192, t0:t0 + Tt], in_=rt[:, 0, :Tt]), reads=["rt0"], writes=[("scr", 128, t0)])
        else:
            P.dma("sp", lambda e: e.dma_start(out=dst[128:192, t0:t0 + Tt], in_=pes[:, 0, :Tt]), reads=["pes"], writes=[("scr", 128, t0)])

    def tile_body(t0, Tt, sid):
        tl = t0 % S if sid == 0 else 0
        rms_tile(cqT, gqs, "gqs", t0, Tt)
        proj_fm(wqs, "wqs", 0, 128, QT, 0, t0, Tt)
        for half in range(2):
            for c in range(4):
                P.op("pe", lambda e, c=c, half=half: e.matmul(pB[:64, :Tt], lhsT=wqs[:, c, 128 + 64 * half:192 + 64 * half], rhs=cs[:, c, :Tt], start=(c == 0), stop=(c == 3)),
                     reads=["wqs", "cs"], writes=["pB"])
            P.op("act", lambda e, half=half: e.copy(out=pes[:, half, :Tt], in_=pB[:64, :Tt]), reads=["pB"], writes=["pes"])
        rope_store(QT, t0, Tt, sid, tl)
        rms_tile(ckvT, gkvs, "gkvs", t0, Tt)
        proj_fm(wkvs, "wkvs", 0, 128, KT, 0, t0, Tt)
        for s0 in range(0, Tt, 128):
            for c in range(4):
                P.op("pe", lambda e, c=c, s0=s0: e.matmul(pB[:, :128], lhsT=cs[:, c, s0:s0 + 128], rhs=wkvs[:, c, 128:256], start=(c == 0), stop=(c == 3)), reads=["wkvs", "cs"], writes=["pB"])
            P.op("act", lambda e: e.copy(out=ev[:, :128], in_=pB[:, :128]), reads=["pB"], writes=["ev"])
            P.dma("sp", lambda e, s0=s0: e.dma_start(out=V[t0 + s0:t0 + s0 + 128, :], in_=ev[:, :128]), reads=["ev"], writes=[("scrV", t0 + s0)])
        P.dma("act", lambda e: e.dma_start(out=pes[:, 0, :Tt], in_=kpeT[:, t0:t0 + Tt]), writes=["pes"])
        P.dma("act", lambda e: e.dma_start(out=pes[:, 1, :Tt], in_=kpeswT[:, t0:t0 + Tt]), writes=["pes"])
        rope_store(KT, t0, Tt, sid, tl)

    for (t0_, Tt_, sid_) in token_tiles(segs):
        tile_body(t0_, Tt_, sid_)

    Sb = S + 256
    NKT = Sb // 128
    kn = P.sb("kn", [128, Sb]); kp = P.sb("kp", [64, Sb]); v1 = P.sb("v1", [128, NKT, 129])
    qn = P.sb("qn", [128, 512]); qp = P.sb("qp", [64, 512])
    pT = [P.sb("pT%d" % i, [128, 512]) for i in range(2)]
    pS = [pA, pB]
    pO = [P.ps("pO%d" % j, [128, 129]) for j in range(4)]
    osb = P.sb("osb", [128, 129]); rinv = P.sb("rinv", [128, 1])
    scale = 192 ** -0.5
    allscr = [k for k in P.res.keys() if isinstance(k, tuple) and k[0] in ("scr", "scrV")]
    outs = []
    def batch_body(b):
        cbase = 2 * S + 256 * b
        lbase = S * b
        P.op("pool", lambda e: e.memset(v1[:, :, 128:129], 1.0), reads=[], writes=["v1"])
        P.dma("sp", lambda e: e.dma_start(out=kn[:, 0:256], in_=KT[0:128, cbase:cbase + 256]), reads=allscr, writes=["kn"])
        P.dma("sp", lambda e: e.dma_start(out=kn[:, 256:Sb], in_=KT[0:128, lbase:lbase + S]), reads=allscr, writes=["kn"])
        P.dma("act", lambda e: e.dma_start(out=kp[:, 0:256], in_=KT[128:192, cbase:cbase + 256]), reads=allscr, writes=["kp"])
        P.dma("act", lambda e: e.dma_start(out=kp[:, 256:Sb], in_=KT[128:192, lbase:lbase + S]), reads=allscr, writes=["kp"])
        P.dma("pool", lambda e: e.dma_start(out=v1[:, 0:2, 0:128], in_=V[cbase:cbase + 256, :].rearrange("(j p) d -> p j d", p=128)), reads=allscr, writes=["v1"])
        P.dma("pool", lambda e: e.dma_start(out=v1[:, 2:NKT, 0:128], in_=V[lbase:lbase + S, :].rearrange("(j p) d -> p j d", p=128)), reads=allscr, writes=["v1"])
        qsets = [(lbase, S, NKT)]
        if need_ctx:
            qsets.append((cbase, 256, 2))
        for (q0, qn_tot, nkt) in qsets:
            for qq in range(0, qn_tot, 512):
                Tq = min(512, qn_tot - qq)
                P.dma("sp", lambda e, qq=qq, Tq=Tq, q0=q0: e.dma_start(out=qn[:, :Tq], in_=QT[0:128, q0 + qq:q0 + qq + Tq]), reads=allscr, writes=["qn"])
                P.dma("act", lambda e, qq=qq, Tq=Tq, q0=q0: e.dma_start(out=qp[:, :Tq], in_=QT[128:192, q0 + qq:q0 + qq + Tq]), reads=allscr, writes=["qp"])
                for kt in range(nkt):
                    bb = kt % 2
                    P.op("pe", lambda e, kt=kt, bb=bb, Tq=Tq: e.matmul(pS[bb][:, :Tq], lhsT=kn[:, kt * 128:(kt + 1) * 128], rhs=qn[:, :Tq], start=True, stop=False), reads=["kn", "qn"], writes=["pS%d" % bb])
                    P.op("pe", lambda e, kt=kt, bb=bb, Tq=Tq: e.matmul(pS[bb][:, :Tq], lhsT=kp[:, kt * 128:(kt + 1) * 128], rhs=qp[:, :Tq], start=False, stop=True), reads=["kp", "qp"], writes=["pS%d" % bb])
                    P.op("act", lambda e, bb=bb, Tq=Tq: e.activation(out=pT[bb][:, :Tq], in_=pS[bb][:, :Tq], func=AF.Exp, scale=scale), reads=["pS%d" % bb], writes=["pT%d" % bb])
                    for j in range(Tq // 128):
                        P.op("pe", lambda e, kt=kt, bb=bb, j=j, nkt=nkt: e.matmul(pO[j][:], lhsT=pT[bb][:, j * 128:(j + 1) * 128], rhs=v1[:, kt, :], start=(kt == 0), stop=(kt == nkt - 1)),
                             reads=["pT%d" % bb, "v1"], writes=["pO%d" % j])
                for j in range(Tq // 128):
                    P.op("dve", lambda e, j=j: e.reciprocal(out=rinv[:], in_=pO[j][:, 128:129]), reads=["pO%d" % j], writes=["rinv"])
                    P.op("dve", lambda e, j=j: e.tensor_scalar(out=osb[:, :128], in0=pO[j][:, :128], scalar1=rinv[:, 0:1], scalar2=None, op0=ALU.mult), reads=["pO%d" % j, "rinv"], writes=["osb"])
                    key = ("o", q0 + qq + j * 128)
                    outs.append(key)
                    P.dma("sp", lambda e, j=j, q0=q0, qq=qq: e.dma_start(out=o[q0 + qq + j * 128:q0 + qq + (j + 1) * 128, :], in_=osb[:, :128]), reads=["osb"], writes=[key])
    for b_ in range(2):
        batch_body(b_)
    P.finish_wait("sp", outs)
    P.emit()
    return nc


def build_rwkv(S, TC=8):
    nc = new_nc()
    TOT = 2 * S + 512
    Sb = S + 256
    din = lambda name, shp: nc.dram_tensor(name, shp, F32, kind="ExternalInput").ap()
    uR = din("uR", [800, TOT]); mu = din("mu", [128, 7, 2]); pc = din("pc", [128, 16])
    wup = din("wup", [128, 128]); aup = din("aup", [128, 128]); gup = din("gup", [160, 128])
    ident_d = din("ident", [128, 128]); blk_d = din("blk", [128, 128])
    yT = nc.dram_tensor("yT", [128, TOT], F32, kind="ExternalOutput").ap()
    scr = lambda name, shp: nc.dram_tensor(name, shp, F32, kind="Internal").ap()
    fm = {n: scr("fm_" + n, [128, TOT]) for n in ("w0", "w1", "r", "v", "g", "ks")}
    tm = {n: scr("tm_" + n, [TOT, 128]) for n in ("a", "b0", "b1", "k0", "k1", "v")}
    yd = [scr("yd%d" % d, [128, TOT]) for d in range(2)]
    P = Prog(nc)
    P.setup_sems()
    C = consts(P)
    ident = P.sb("ident_s", [128, 128]); blk = P.sb("blk_s", [128, 128])
    mus = P.sb("mus", [128, 7, 3]); pcs = P.sb("pcs", [128, 16])
    wups = P.sb("wups", [128, 128]); aups = P.sb("aups", [128, 128]); gups0 = P.sb("gups0", [128, 128]); gups1 = P.sb("gups1", [32, 128])
    P.dma("sp", lambda e: e.dma_start(out=ident[:], in_=ident_d), writes=["ident"])
    P.dma("sp", lambda e: e.dma_start(out=blk[:], in_=blk_d), writes=["blk"])
    P.dma("sp", lambda e: e.dma_start(out=mus[:, :, 0:2], in_=mu), writes=["mus"])
    P.dma("sp", lambda e: e.dma_start(out=pcs[:], in_=pc), writes=["pcs"])
    P.dma("act", lambda e: e.dma_start(out=wups[:], in_=wup), writes=["wups"])
    P.dma("act", lambda e: e.dma_start(out=aups[:], in_=aup), writes=["aups"])
    P.dma("act", lambda e: e.dma_start(out=gups0[:], in_=gup[0:128, :]), writes=["gups0"])
    P.dma("act", lambda e: e.dma_start(out=gups1[:], in_=gup[128:160, :]), writes=["gups1"])
    P.op("dve", lambda e: e.tensor_tensor(out=mus[:, :, 2:3], in0=mus[:, :, 0:1], in1=mus[:, :, 1:2], op=ALU.add), reads=["mus"], writes=["mus"])
    P.op("dve", lambda e: e.tensor_scalar(out=mus[:, :, 2:3], in0=mus[:, :, 2:3], scalar1=-1.0, scalar2=1.0, op0=ALU.mult, op1=ALU.add), reads=["mus"], writes=["mus"])
    P.op("dve", lambda e: e.tensor_scalar(out=pcs[:, 9:10], in0=pcs[:, 1:2], scalar1=-1.0, scalar2=1.0, op0=ALU.mult, op1=ALU.add), reads=["pcs"], writes=["pcs"])
    KKW, KA, RK, LNG, LNB, W0, A0, OMKA = 0, 1, 2, 3, 4, 5, 7, 9

    TT = 512
    raw = P.sb("raw", [128, TT + 2]); us = [P.sb("us%d" % j, [128, TT]) for j in range(7)]
    t1 = P.sb("t1", [128, TT]); t2 = P.sb("t2", [128, TT]); t3 = P.sb("t3", [128, TT]); kkn = P.sb("kkn", [128, TT]); ksum = P.sb("ksum", [128, TT])
    tr = P.sb("tr", [128, 128])
    pA = P.ps("pA", [128, 512]); pB = P.ps("pB", [128, 512]); pTr = P.ps("pTr", [128, 128])
    segs = [(0, S, 0), (S, S, 0), (2 * S, 256, 1), (2 * S + 256, 256, 1)]
    scr_keys = []

    def store_fm(name, src, sname, t0, Tt):
        key = ("fm", name, t0); scr_keys.append(key)
        P.dma("sp", lambda e: e.dma_start(out=fm[name][:, t0:t0 + Tt], in_=src[:, :Tt]), reads=[sname], writes=[key])

    def store_tm(name, src, sname, t0, Tt):
        for s0 in range(0, Tt, 128):
            P.op("pe", lambda e, s0=s0: e.transpose(out=pTr[:], in_=src[:, s0:s0 + 128], identity=ident[:]), reads=[sname, "ident"], writes=["pTr"])
            P.op("act", lambda e: e.copy(out=tr[:], in_=pTr[:]), reads=["pTr"], writes=["tr"])
            key = ("tm", name, t0 + s0); scr_keys.append(key)
            P.dma("sp", lambda e, s0=s0: e.dma_start(out=tm[name][t0 + s0:t0 + s0 + 128, :], in_=tr[:]), reads=["tr"], writes=[key])

    def prep_tile(seg0, segn, t0, Tt):
        for j in range(7):
            rows = 128 if j < 6 else 32
            r0 = j * 128
            lo = max(seg0, t0 - 1); hi = min(seg0 + segn, t0 + Tt + 1)
            P.op("pool", lambda e: e.memset(raw[:, :], 0.0), writes=["raw"])
            P.dma("sp" if j % 2 else "act", lambda e, r0=r0, rows=rows, lo=lo, hi=hi: e.dma_start(out=raw[:rows, lo - (t0 - 1):hi - (t0 - 1)], in_=uR[r0:r0 + rows, lo:hi]), writes=["raw"])
            un = "us%d" % j
            P.op("dve", lambda e, j=j, rows=rows: e.tensor_scalar(out=us[j][:rows, :Tt], in0=raw[:rows, 1:Tt + 1], scalar1=mus[:rows, j, 2:3], scalar2=None, op0=ALU.mult), reads=["raw", "mus"], writes=[un])
            P.op("dve", lambda e, j=j, rows=rows: e.scalar_tensor_tensor(out=us[j][:rows, :Tt], in0=raw[:rows, 0:Tt], scalar=mus[:rows, j, 0:1], in1=us[j][:rows, :Tt], op0=ALU.mult, op1=ALU.add), reads=["raw", "mus", un], writes=[un])
            P.op("dve", lambda e, j=j, rows=rows: e.scalar_tensor_tensor(out=us[j][:rows, :Tt], in0=raw[:rows, 2:Tt + 2], scalar=mus[:rows, j, 1:2], in1=us[j][:rows, :Tt], op0=ALU.mult, op1=ALU.add), reads=["raw", "mus", un], writes=[un])
        r_, k_, v_, wd_, ad_, g0_, g1_ = us
        store_fm("r", r_, "us0", t0, Tt); store_fm("v", v_, "us2", t0, Tt); store_tm("v", v_, "us2", t0, Tt)
        P.op("act", lambda e: e.activation(out=g0_[:, :Tt], in_=g0_[:, :Tt], func=AF.Sigmoid), reads=["us5"], writes=["us5"])
        P.op("act", lambda e: e.activation(out=g1_[:32, :Tt], in_=g1_[:32, :Tt], func=AF.Sigmoid), reads=["us6"], writes=["us6"])
        P.op("pe", lambda e: e.matmul(pA[:, :Tt], lhsT=gups0[:], rhs=g0_[:, :Tt], start=True, stop=False), reads=["gups0", "us5"], writes=["pA"])
        P.op("pe", lambda e: e.matmul(pA[:, :Tt], lhsT=gups1[:], rhs=g1_[:32, :Tt], start=False, stop=True), reads=["gups1", "us6"], writes=["pA"])
        P.op("act", lambda e: e.copy(out=t1[:, :Tt], in_=pA[:, :Tt]), reads=["pA"], writes=["t1"])
        store_fm("g", t1, "t1", t0, Tt)
        P.op("dve", lambda e: e.tensor_scalar(out=kkn[:, :Tt], in0=k_[:, :Tt], scalar1=pcs[:, KKW:KKW + 1], scalar2=None, op0=ALU.mult), reads=["us1", "pcs"], writes=["kkn"])
        P.op("act", lambda e: e.activation(out=t2[:, :Tt], in_=kkn[:, :Tt], func=AF.Square), reads=["kkn"], writes=["t2"])
        P.op("pe", lambda e: e.matmul(pB[:, :Tt], lhsT=blk[:], rhs=t2[:, :Tt], start=True, stop=True), reads=["blk", "t2"], writes=["pB"])
        rsqrt_op(P, C, t2[:, :Tt], "t2", pB[:, :Tt], "pB", "eps_tiny")
        P.op("dve", lambda e: e.tensor_tensor(out=kkn[:, :Tt], in0=kkn[:, :Tt], in1=t2[:, :Tt], op=ALU.mult), reads=["kkn", "t2"], writes=["kkn"])
        P.op("pool", lambda e: e.tensor_scalar(out=t3[:, :Tt], in0=kkn[:, :Tt], scalar1=-1.0, scalar2=None, op0=ALU.mult), reads=["kkn"], writes=["t3"])
        store_tm("a", t3, "t3", t0, Tt)
        P.op("act", lambda e: e.activation(out=wd_[:, :Tt], in_=wd_[:, :Tt], func=AF.Tanh), reads=["us3"], writes=["us3"])
        for d in range(2):
            P.op("pe", lambda e, d=d: e.matmul(pA[:, :Tt], lhsT=wups[64 * d:64 * d + 64, :], rhs=wd_[64 * d:64 * d + 64, :Tt], start=True, stop=True), reads=["wups", "us3"], writes=["pA"])
            P.op("act", lambda e, d=d: e.activation(out=t1[:, :Tt], in_=pA[:, :Tt], func=AF.Sigmoid, bias=pcs[:, W0 + d:W0 + d + 1]), reads=["pA", "pcs"], writes=["t1"])
            P.op("act", lambda e: e.activation(out=t1[:, :Tt], in_=t1[:, :Tt], func=AF.Exp, scale=-float(np.exp(-0.5))), reads=["t1"], writes=["t1"])
            store_fm("w%d" % d, t1, "t1", t0, Tt)
            P.op("pe", lambda e, d=d: e.matmul(pB[:, :Tt], lhsT=aups[64 * d:64 * d + 64, :], rhs=ad_[64 * d:64 * d + 64, :Tt], start=True, stop=True), reads=["aups", "us4"], writes=["pB"])
            P.op("act", lambda e, d=d: e.activation(out=t2[:, :Tt], in_=pB[:, :Tt], func=AF.Sigmoid, bias=pcs[:, A0 + d:A0 + d + 1]), reads=["pB", "pcs"], writes=["t2"])
            P.op("dve", lambda e: e.tensor_tensor(out=t3[:, :Tt], in0=kkn[:, :Tt], in1=t2[:, :Tt], op=ALU.mult), reads=["kkn", "t2"], writes=["t3"])
            store_tm("b%d" % d, t3, "t3", t0, Tt)
            P.op("dve", lambda e: e.tensor_scalar(out=t2[:, :Tt], in0=t2[:, :Tt], scalar1=pcs[:, KA:KA + 1], scalar2=pcs[:, OMKA:OMKA + 1], op0=ALU.mult, op1=ALU.add), reads=["t2", "pcs"], writes=["t2"])
            P.op("dve", lambda e: e.tensor_tensor(out=t2[:, :Tt], in0=t2[:, :Tt], in1=k_[:, :Tt], op=ALU.mult), reads=["t2", "us1"], writes=["t2"])
            store_tm("k%d" % d, t2, "t2", t0, Tt)
            if d == 0:
                P.op("pool", lambda e: e.tensor_copy(out=ksum[:, :Tt], in_=t2[:, :Tt]), reads=["t2"], writes=["ksum"])
            else:
                P.op("pool", lambda e: e.tensor_tensor(out=ksum[:, :Tt], in0=ksum[:, :Tt], in1=t2[:, :Tt], op=ALU.add), reads=["t2", "ksum"], writes=["ksum"])
        store_fm("ks", ksum, "ksum", t0, Tt)

    for (s0_, sn_, sid_) in segs:
        for (t0_, Tt_, _) in token_tiles([(s0_, sn_, sid_)], TT):
            prep_tile(s0_, sn_, t0_, Tt_)

    NCH = 4
    rows = {}
    for n in ("a", "b", "k", "v"):
        for bf in range(2):
            for pr in range(2):
                t = P.sb("row_%s_%d_%d" % (n, bf, pr), [128, TC, 128])
                P.op("pool", lambda e, t=t: e.memset(t[:], 0.0), writes=["row_%s%d_%d" % (n, ci, bf) for ci in (2 * pr, 2 * pr + 1)])
                for q in range(2):
                    rows[(n, 2 * pr + q, bf)] = t[64 * q:64 * q + 2]
    wcol = {(ci, bf): P.sb("wcol%d_%d" % (ci, bf), [128, TC]) for ci in range(NCH) for bf in range(2)}
    rcol = {(ci, bf): P.sb("rcol%d_%d" % (ci, bf), [128, TC]) for ci in range(NCH) for bf in range(2)}
    ST = {(ci, bf): P.sb("ST%d_%d" % (ci, bf), [128, 128]) for ci in range(NCH) for bf in range(2)}
    MT = {(ci, bf): P.sb("MT%d_%d" % (ci, bf), [128, 128]) for ci in range(NCH) for bf in range(2)}
    ysb = {(ci, bf): P.sb("ysb%d_%d" % (ci, bf), [128, TC]) for ci in range(NCH) for bf in range(2)}
    pM = [P.ps("pM%d" % i, [128, 128]) for i in range(2)]
    pSt = [P.ps("pSt%d" % i, [128, 128]) for i in range(2)]
    pY = [pA, pB]
    for ci in range(NCH):
        P.op("pool", lambda e, ci=ci: e.memset(ST[(ci, 0)][:], 0.0), writes=["ST%d_0" % ci])
    ydkeys = []

    def chunk_list(b, d):
        cb = 2 * S + 256 * b; lb = S * b
        ctx = [(cb + i * TC) for i in range(256 // TC)]
        lat = [(lb + i * TC) for i in range(S // TC)]
        return ctx + lat if d == 0 else ctx[::-1] + lat[::-1]

    chains = [(b, d) for b in range(2) for d in range(2)]
    clists = [chunk_list(b, d) for (b, d) in chains]
    nchunks = len(clists[0])
    step = 0
    for cidx in range(nchunks):
        bf = cidx % 2
        for ci, (b, d) in enumerate(chains):
            t0 = clists[ci][cidx]

            def load(ci=ci, d=d, t0=t0, bf=bf):
                for n, src in (("a", "a"), ("b", "b%d" % d), ("k", "k%d" % d), ("v", "v")):
                    for h in range(2):
                        rn = "row_%s%d_%d" % (n, ci, bf)
                        P.dma("sp" if h == 0 else "act", lambda e, n=n, src=src, h=h: e.dma_start(out=rows[(n, ci, bf)][h:h + 1, :, h * 64:(h + 1) * 64],
                                                                                                     in_=tm[src][t0:t0 + TC, h * 64:(h + 1) * 64].rearrange("(o t) k -> o t k", o=1)),
                              reads=scr_keys, writes=[rn])
                P.dma("pool", lambda e: e.dma_start(out=wcol[(ci, bf)][:], in_=fm["w%d" % d][:, t0:t0 + TC]), reads=scr_keys, writes=["wcol%d_%d" % (ci, bf)])
                P.dma("pool", lambda e: e.dma_start(out=rcol[(ci, bf)][:], in_=fm["r"][:, t0:t0 + TC]), reads=scr_keys, writes=["rcol%d_%d" % (ci, bf)])
            load()
        for s in range(TC):
            for ci, (b, d) in enumerate(chains):
                tt = s if d == 0 else TC - 1 - s
                cur = step % 2; nxt = (step + 1) % 2

                def one(ci=ci, tt=tt, bf=bf, cur=cur, nxt=nxt):
                    ar = rows[("a", ci, bf)]; br = rows[("b", ci, bf)]; kr = rows[("k", ci, bf)]; vr = rows[("v", ci, bf)]
                    pm = pM[ci % 2]; pst = pSt[ci % 2]
                    P.op("pe", lambda e: e.matmul(pm[:], lhsT=ar[:, tt, :], rhs=br[:, tt, :], start=True, stop=True),
                         reads=["row_a%d_%d" % (ci, bf), "row_b%d_%d" % (ci, bf)], writes=["pM%d" % (ci % 2)])
                    P.op("dve", lambda e: e.scalar_tensor_tensor(out=MT[(ci, cur)][:], in0=ident[:], scalar=wcol[(ci, bf)][:, tt:tt + 1], in1=pm[:], op0=ALU.mult, op1=ALU.add),
                         reads=["ident", "wcol%d_%d" % (ci, bf), "pM%d" % (ci % 2)], writes=["MT%d_%d" % (ci, cur)])
                    P.op("pe", lambda e: e.matmul(pst[:], lhsT=MT[(ci, cur)][:], rhs=ST[(ci, cur)][:], start=True, stop=False),
                         reads=["MT%d_%d" % (ci, cur), "ST%d_%d" % (ci, cur)], writes=["pSt%d" % (ci % 2)])
                    P.op("pe", lambda e: e.matmul(pst[:], lhsT=kr[:, tt, :], rhs=vr[:, tt, :], start=False, stop=True),
                         reads=["row_k%d_%d" % (ci, bf), "row_v%d_%d" % (ci, bf)], writes=["pSt%d" % (ci % 2)])
                    P.op("act", lambda e: e.copy(out=ST[(ci, nxt)][:], in_=pst[:]), reads=["pSt%d" % (ci % 2)], writes=["ST%d_%d" % (ci, nxt)])
                    P.op("pe", lambda e: e.matmul(pY[bf][:, ci * TC + tt:ci * TC + tt + 1], lhsT=ST[(ci, nxt)][:], rhs=rcol[(ci, bf)][:, tt:tt + 1], start=True, stop=True),
                         reads=["ST%d_%d" % (ci, nxt), "rcol%d_%d" % (ci, bf)], writes=[("pY", bf, ci)])
                one()
            step += 1
        for ci, (b, d) in enumerate(chains):
            t0 = clists[ci][cidx]

            def fin(ci=ci, d=d, t0=t0, bf=bf):
                P.op("dve", lambda e: e.tensor_copy(out=ysb[(ci, bf)][:], in_=pY[bf][:, ci * TC:(ci + 1) * TC]), reads=[("pY", bf, ci)], writes=["ysb%d_%d" % (ci, bf)])
                key = ("yd", d, t0); ydkeys.append(key)
                P.dma("sp", lambda e: e.dma_start(out=yd[d][:, t0:t0 + TC], in_=ysb[(ci, bf)][:]), reads=["ysb%d_%d" % (ci, bf)], writes=[key])
            fin()

    y0 = P.sb("py0", [128, TT]); y1 = P.sb("py1", [128, TT])
    outs = []

    def post_tile(t0, Tt):
        P.dma("sp", lambda e: e.dma_start(out=y0[:, :Tt], in_=yd[0][:, t0:t0 + Tt]), reads=ydkeys, writes=["py0"])
        P.dma("act", lambda e: e.dma_start(out=y1[:, :Tt], in_=yd[1][:, t0:t0 + Tt]), reads=ydkeys, writes=["py1"])
        P.dma("sp", lambda e: e.dma_start(out=us[0][:, :Tt], in_=fm["r"][:, t0:t0 + Tt]), reads=scr_keys, writes=["us0"])
        P.dma("act", lambda e: e.dma_start(out=us[1][:, :Tt], in_=fm["ks"][:, t0:t0 + Tt]), reads=scr_keys, writes=["us1"])
        P.dma("sp", lambda e: e.dma_start(out=us[2][:, :Tt], in_=fm["v"][:, t0:t0 + Tt]), reads=scr_keys, writes=["us2"])
        P.dma("act", lambda e: e.dma_start(out=us[3][:, :Tt], in_=fm["g"][:, t0:t0 + Tt]), reads=scr_keys, writes=["us3"])
        P.op("dve", lambda e: e.tensor_tensor(out=y0[:, :Tt], in0=y0[:, :Tt], in1=y1[:, :Tt], op=ALU.add), reads=["py0", "py1"], writes=["py0"])
        P.op("act", lambda e: e.activation(out=t1[:, :Tt], in_=y0[:, :Tt], func=AF.Square), reads=["py0"], writes=["t1"])
        P.op("pe", lambda e: e.matmul(pA[:, :Tt], lhsT=blk[:], rhs=y0[:, :Tt], start=True, stop=True), reads=["blk", "py0"], writes=["pA"])
        P.op("pe", lambda e: e.matmul(pB[:, :Tt], lhsT=blk[:], rhs=t1[:, :Tt], start=True, stop=True), reads=["blk", "t1"], writes=["pB"])
        P.op("act", lambda e: e.mul(out=t2[:, :Tt], in_=pA[:, :Tt], mul=1.0 / 64), reads=["pA"], writes=["t2"])
        P.op("dve", lambda e: e.tensor_tensor(out=t3[:, :Tt], in0=t2[:, :Tt], in1=t2[:, :Tt], op=ALU.mult), reads=["t2"], writes=["t3"])
        P.op("dve", lambda e: e.scalar_tensor_tensor(out=t3[:, :Tt], in0=pB[:, :Tt], scalar=1.0 / 64, in1=t3[:, :Tt], op0=ALU.mult, op1=ALU.subtract), reads=["pB", "t3"], writes=["t3"])
        rsqrt_op(P, C, t3[:, :Tt], "t3", t3[:, :Tt], "t3", "eps_gn")
        P.op("dve", lambda e: e.tensor_tensor(out=y0[:, :Tt], in0=y0[:, :Tt], in1=t2[:, :Tt], op=ALU.subtract), reads=["py0", "t2"], writes=["py0"])
        P.op("dve", lambda e: e.tensor_tensor(out=y0[:, :Tt], in0=y0[:, :Tt], in1=t3[:, :Tt], op=ALU.mult), reads=["py0", "t3"], writes=["py0"])
        P.op("act", lambda e: e.activation(out=y0[:, :Tt], in_=y0[:, :Tt], func=AF.Identity, bias=pcs[:, LNB:LNB + 1], scale=pcs[:, LNG:LNG + 1]), reads=["py0", "pcs"], writes=["py0"])
        P.op("dve", lambda e: e.scalar_tensor_tensor(out=t1[:, :Tt], in0=us[0][:, :Tt], scalar=pcs[:, RK:RK + 1], in1=us[1][:, :Tt], op0=ALU.mult, op1=ALU.mult), reads=["us0", "us1", "pcs"], writes=["t1"])
        P.op("pe", lambda e: e.matmul(pA[:, :Tt], lhsT=blk[:], rhs=t1[:, :Tt], start=True, stop=True), reads=["blk", "t1"], writes=["pA"])
        P.op("dve", lambda e: e.tensor_tensor(out=t1[:, :Tt], in0=pA[:, :Tt], in1=us[2][:, :Tt], op=ALU.mult), reads=["pA", "us2"], writes=["t1"])
        P.op("dve", lambda e: e.tensor_tensor(out=y0[:, :Tt], in0=y0[:, :Tt], in1=t1[:, :Tt], op=ALU.add), reads=["py0", "t1"], writes=["py0"])
        P.op("dve", lambda e: e.tensor_tensor(out=y0[:, :Tt], in0=y0[:, :Tt], in1=us[3][:, :Tt], op=ALU.mult), reads=["py0", "us3"], writes=["py0"])
        key = ("out", t0); outs.append(key)
        P.dma("sp", lambda e: e.dma_start(out=yT[:, t0:t0 + Tt], in_=y0[:, :Tt]), reads=["py0"], writes=[key])

    for (t0_, Tt_, _) in token_tiles(segs, TT):
        post_tile(t0_, Tt_)
    P.finish_wait("sp", outs)
    P.emit()
    return nc


def conv3_tile(P, raw, rawname, dst, dname, cws, cwname, j, rows, Tt, silu):
    P.op("dve", lambda e: e.tensor_scalar(out=dst[:rows, :Tt], in0=raw[:rows, 1:Tt + 1], scalar1=cws[:rows, j, 1:2], scalar2=cws[:rows, j, 3:4], op0=ALU.mult, op1=ALU.add), reads=[rawname, cwname], writes=[dname])
    P.op("dve", lambda e: e.scalar_tensor_tensor(out=dst[:rows, :Tt], in0=raw[:rows, 0:Tt], scalar=cws[:rows, j, 0:1], in1=dst[:rows, :Tt], op0=ALU.mult, op1=ALU.add), reads=[rawname, cwname, dname], writes=[dname])
    P.op("dve", lambda e: e.scalar_tensor_tensor(out=dst[:rows, :Tt], in0=raw[:rows, 2:Tt + 2], scalar=cws[:rows, j, 2:3], in1=dst[:rows, :Tt], op0=ALU.mult, op1=ALU.add), reads=[rawname, cwname, dname], writes=[dname])
    if silu:
        P.op("act", lambda e: e.activation(out=dst[:rows, :Tt], in_=dst[:rows, :Tt], func=AF.Silu), reads=[dname], writes=[dname])


def load_halo(P, q, raw, rawname, src, r0, rows, seg0, segn, t0, Tt):
    lo = max(seg0, t0 - 1); hi = min(seg0 + segn, t0 + Tt + 1)
    P.op("pool", lambda e: e.memset(raw[:, :], 0.0), writes=[rawname])
    P.dma(q, lambda e: e.dma_start(out=raw[:rows, lo - (t0 - 1):hi - (t0 - 1)], in_=src[r0:r0 + rows, lo:hi]), writes=[rawname])


def build_ssd(S):
    nc = new_nc()
    TOT = 2 * S + 512
    din = lambda name, shp: nc.dram_tensor(name, shp, F32, kind="ExternalInput").ap()
    zT = din("zT", [128, TOT]); xbcT = din("xbcT", [384, TOT]); cw = din("cw", [128, 3, 4]); dttok = din("dttok", [TOT, 4])
    dtb = din("dtb", [128, 4]); alog = din("alog", [128, 4]); Dp = din("Dp", [128, 1]); ident_d = din("ident", [128, 128]); triU_d = din("triU", [128, 128])
    yT = nc.dram_tensor("yT", [128, TOT], F32, kind="ExternalOutput").ap()
    scr = lambda name, shp: nc.dram_tensor(name, shp, F32, kind="Internal").ap()
    fm = {n: scr("fm_" + n, [128, TOT]) for n in ("xs", "B", "C")}
    tm = {n: scr("tm_" + n, [TOT, 128]) for n in ("X", "B")}
    yd = [scr("yd%d" % d, [128, TOT]) for d in range(2)]
    P = Prog(nc)
    P.setup_sems()
    C = consts(P)
    ones = C["ones"]
    ident = P.sb("ident_s", [128, 128]); U = P.sb("U_s", [128, 128]); L = P.sb("L_s", [128, 128])
    cws = P.sb("cws", [128, 3, 4]); dtbs = P.sb("dtbs", [128, 4]); als = P.sb("als", [128, 4]); Dps = P.sb("Dps", [128, 1])
    pTr = P.ps("pTr", [128, 128])
    P.dma("sp", lambda e: e.dma_start(out=ident[:], in_=ident_d), writes=["ident"])
    P.dma("sp", lambda e: e.dma_start(out=U[:], in_=triU_d), writes=["U"])
    P.dma("sp", lambda e: e.dma_start(out=cws[:], in_=cw), writes=["cws"])
    P.dma("act", lambda e: e.dma_start(out=dtbs[:], in_=dtb), writes=["dtbs"])
    P.dma("act", lambda e: e.dma_start(out=als[:], in_=alog), writes=["als"])
    P.dma("act", lambda e: e.dma_start(out=Dps[:], in_=Dp), writes=["Dps"])
    P.op("pe", lambda e: e.transpose(out=pTr[:], in_=U[:], identity=ident[:]), reads=["U", "ident"], writes=["pTr"])
    P.op("act", lambda e: e.copy(out=L[:], in_=pTr[:]), reads=["pTr"], writes=["L"])
    P.op("act", lambda e: e.activation(out=als[:], in_=als[:], func=AF.Exp), reads=["als"], writes=["als"])
    P.op("dve", lambda e: e.tensor_scalar(out=als[:], in0=als[:], scalar1=-1.0, scalar2=None, op0=ALU.mult), reads=["als"], writes=["als"])
    NCK = TOT // 128
    dts = P.sb("dts", [128, NCK, 4]); dAs = P.sb("dAs", [128, NCK, 4])
    P.dma("sp", lambda e: e.dma_start(out=dts[:], in_=dttok.rearrange("(c p) k -> p c k", p=128)), writes=["dts"])
    for cg in range(NCK):
        P.op("dve", lambda e, cg=cg: e.tensor_tensor(out=dts[:, cg, :], in0=dts[:, cg, :], in1=dtbs[:], op=ALU.add), reads=["dts", "dtbs"], writes=["dts"])
    P.op("act", lambda e: e.activation(out=dts[:], in_=dts[:], func=AF.Exp), reads=["dts"], writes=["dts"])
    P.op("act", lambda e: e.activation(out=dts[:], in_=dts[:], func=AF.Ln, bias=1.0), reads=["dts"], writes=["dts"])
    for cg in range(NCK):
        P.op("dve", lambda e, cg=cg: e.tensor_tensor(out=dAs[:, cg, :], in0=dts[:, cg, :], in1=als[:], op=ALU.mult), reads=["dts", "als"], writes=["dAs"])

    TT = 512
    raw = P.sb("raw", [128, TT + 2]); cv = P.sb("cv", [128, TT]); tr = P.sb("tr", [128, 128])
    segs = [(0, S, 0), (S, S, 0), (2 * S, 256, 1), (2 * S + 256, 256, 1)]
    scr_keys = []

    def prep_tile(seg0, segn, t0, Tt):
        for j, name in enumerate(("xs", "B", "C")):
            load_halo(P, "sp" if j % 2 else "act", raw, "raw", xbcT, j * 128, 128, seg0, segn, t0, Tt)
            conv3_tile(P, raw, "raw", cv, "cv", cws, "cws", j, 128, Tt, True)
            key = ("fm", name, t0); scr_keys.append(key)
            P.dma("sp", lambda e, name=name: e.dma_start(out=fm[name][:, t0:t0 + Tt], in_=cv[:, :Tt]), reads=["cv"], writes=[key])
            if name != "C":
                tn = "X" if name == "xs" else "B"
                for s0 in range(0, Tt, 128):
                    P.op("pe", lambda e, s0=s0: e.transpose(out=pTr[:], in_=cv[:, s0:s0 + 128], identity=ident[:]), reads=["cv", "ident"], writes=["pTr"])
                    P.op("act", lambda e: e.copy(out=tr[:], in_=pTr[:]), reads=["pTr"], writes=["tr"])
                    key = ("tm", tn, t0 + s0); scr_keys.append(key)
                    P.dma("sp", lambda e, s0=s0, tn=tn: e.dma_start(out=tm[tn][t0 + s0:t0 + s0 + 128, :], in_=tr[:]), reads=["tr"], writes=[key])

    for (s0_, sn_, sid_) in segs:
        for (t0_, Tt_, _) in token_tiles([(s0_, sn_, sid_)], TT):
            prep_tile(s0_, sn_, t0_, Tt_)

    chains = [(b, d) for b in range(2) for d in range(2)]

    def chunk_list(b, d):
        cb = 2 * S + 256 * b; lb = S * b
        ctx = [cb, cb + 128]; lat = [lb + i * 128 for i in range(S // 128)]
        return ctx + lat if d == 0 else ctx[::-1] + lat[::-1]
    clists = [chunk_list(b, d) for (b, d) in chains]
    bufs = {}
    for ci in range(4):
        for bf in range(2):
            for n in ("BT", "CT", "Bk", "Xk"):
                bufs[(n, ci, bf)] = P.sb("%s%d_%d" % (n, ci, bf), [128, 128])
    STp = {}; Xp = {}
    for ci in range(4):
        for h in range(2):
            STp[(ci, h)] = P.sb("STp%d_%d" % (ci, h), [128, 128]); Xp[(ci, h)] = P.sb("Xp%d_%d" % (ci, h), [128, 128])
            P.op("pool", lambda e, t=STp[(ci, h)]: e.memset(t[:], 0.0), writes=["STp%d_%d" % (ci, h)])
            P.op("pool", lambda e, t=Xp[(ci, h)]: e.memset(t[:], 0.0), writes=["Xp%d_%d" % (ci, h)])
    dAb = P.sb("dAb", [128, 128]); Lm = P.sb("Lm", [128, 128]); MTt = P.sb("MTt", [128, 128]); erow = P.sb("erow", [128, 128]); CTs = P.sb("CTs", [128, 128])
    colsb = P.sb("colsb", [128, 1]); dcol = P.sb("dcol", [128, 1]); Xd = P.sb("Xd", [128, 64]); ysb = P.sb("ysb", [128, 128])
    pG = P.ps("pG", [128, 128]); pR = P.ps("pR", [128, 128]); pC = P.ps("pC", [128, 1]); pY = P.ps("pY", [128, 128]); pS = P.ps("pS", [128, 64])
    ydkeys = []

    def chunk_step(ci, d, t0, bf):
        BT, CT, Bk, Xk = (bufs[(n, ci, bf)] for n in ("BT", "CT", "Bk", "Xk"))
        nm = lambda n: "%s%d_%d" % (n, ci, bf)
        P.dma("sp", lambda e: e.dma_start(out=BT[:], in_=fm["B"][:, t0:t0 + 128]), reads=scr_keys, writes=[nm("BT")])
        P.dma("act", lambda e: e.dma_start(out=CT[:], in_=fm["C"][:, t0:t0 + 128]), reads=scr_keys, writes=[nm("CT")])
        P.dma("sp", lambda e: e.dma_start(out=Bk[:], in_=tm["B"][t0:t0 + 128, :]), reads=scr_keys, writes=[nm("Bk")])
        P.dma("act", lambda e: e.dma_start(out=Xk[:], in_=tm["X"][t0:t0 + 128, :]), reads=scr_keys, writes=[nm("Xk")])
        cg = t0 // 128
        Ud, Ud_n = (U, "U") if d == 0 else (L, "L")
        last = 127 if d == 0 else 0
        P.op("pe", lambda e: e.matmul(pG[:], lhsT=BT[:], rhs=CT[:], start=True, stop=True), reads=[nm("BT"), nm("CT")], writes=["pG"])
        for h in range(2):
            dh = d * 2 + h
            hc = slice(h * 64, (h + 1) * 64)
            stn = "STp%d_%d" % (ci, h); xpn = "Xp%d_%d" % (ci, h)
            P.op("dve", lambda e, dh=dh: e.tensor_scalar(out=dAb[:], in0=ones[:], scalar1=dAs[:, cg, dh:dh + 1], scalar2=None, op0=ALU.mult), reads=["c_ones", "dAs"], writes=["dAb"])
            P.op("pe", lambda e: e.matmul(pR[:], lhsT=dAb[:], rhs=Ud[:], start=True, stop=True), reads=["dAb", Ud_n], writes=["pR"])
            P.op("pe", lambda e, dh=dh: e.matmul(pC[:], lhsT=Ud[:], rhs=dAs[:, cg, dh:dh + 1], start=True, stop=True), reads=["dAs", Ud_n], writes=["pC"])
            P.op("act", lambda e: e.copy(out=colsb[:], in_=pC[:]), reads=["pC"], writes=["colsb"])
            P.op("dve", lambda e: e.tensor_scalar(out=Lm[:], in0=pR[:], scalar1=colsb[:, 0:1], scalar2=0.0, op0=ALU.subtract, op1=ALU.min), reads=["pR", "colsb"], writes=["Lm"])
            P.op("act", lambda e: e.activation(out=Lm[:], in_=Lm[:], func=AF.Exp), reads=["Lm"], writes=["Lm"])
            P.op("pool", lambda e: e.tensor_tensor(out=Lm[:], in0=Lm[:], in1=Ud[:], op=ALU.mult), reads=["Lm", Ud_n], writes=["Lm"])
            P.op("dve", lambda e: e.tensor_tensor(out=MTt[:], in0=Lm[:], in1=pG[:], op=ALU.mult), reads=["Lm", "pG"], writes=["MTt"])
            P.op("act", lambda e: e.activation(out=erow[:], in_=pR[:], func=AF.Exp), reads=["pR"], writes=["erow"])
            P.op("pool", lambda e: e.tensor_tensor(out=CTs[:], in0=CT[:], in1=erow[:], op=ALU.mult), reads=[nm("CT"), "erow"], writes=["CTs"])
            P.op("dve", lambda e, dh=dh, h=h, hc=hc: e.tensor_scalar(out=Xp[(ci, h)][:, hc], in0=Xk[:, hc], scalar1=dts[:, cg, dh:dh + 1], scalar2=None, op0=ALU.mult), reads=[nm("Xk"), "dts"], writes=[xpn])
            P.op("pe", lambda e, h=h: e.matmul(pY[:], lhsT=Xp[(ci, h)][:], rhs=MTt[:], start=(h == 0), stop=False), reads=[xpn, "MTt"], writes=["pY"])
            P.op("pe", lambda e, h=h: e.matmul(pY[:], lhsT=STp[(ci, h)][:], rhs=CTs[:], start=False, stop=(h == 1)), reads=[stn, "CTs"], writes=["pY"])
            P.op("dve", lambda e: e.tensor_tensor(out=dcol[:], in0=pR[:, last:last + 1], in1=colsb[:], op=ALU.subtract), reads=["pR", "colsb"], writes=["dcol"])
            P.op("act", lambda e: e.activation(out=dcol[:], in_=dcol[:], func=AF.Exp), reads=["dcol"], writes=["dcol"])
            P.op("dve", lambda e, h=h, hc=hc: e.tensor_scalar(out=Xd[:], in0=Xp[(ci, h)][:, hc], scalar1=dcol[:, 0:1], scalar2=None, op0=ALU.mult), reads=[xpn, "dcol"], writes=["Xd"])
            P.op("pe", lambda e: e.matmul(pS[:], lhsT=Bk[:], rhs=Xd[:], start=True, stop=True), reads=[nm("Bk"), "Xd"], writes=["pS"])
            P.op("dve", lambda e, h=h, hc=hc: e.scalar_tensor_tensor(out=STp[(ci, h)][:, hc], in0=STp[(ci, h)][:, hc], scalar=erow[:, last:last + 1], in1=pS[:], op0=ALU.mult, op1=ALU.add),
                 reads=[stn, "erow", "pS"], writes=[stn])
        P.op("act", lambda e: e.copy(out=ysb[:], in_=pY[:]), reads=["pY"], writes=["ysb"])
        key = ("yd", d, t0); ydkeys.append(key)
        P.dma("sp", lambda e: e.dma_start(out=yd[d][:, t0:t0 + 128], in_=ysb[:]), reads=["ysb"], writes=[key])

    for cidx in range(len(clists[0])):
        for ci, (b, d) in enumerate(chains):
            chunk_step(ci, d, clists[ci][cidx], cidx % 2)

    y0 = P.sb("py0", [128, TT]); y1 = P.sb("py1", [128, TT]); xs_ = P.sb("pxs", [128, TT]); z_ = P.sb("pz", [128, TT])
    outs = []

    def post_tile(t0, Tt):
        P.dma("sp", lambda e: e.dma_start(out=y0[:, :Tt], in_=yd[0][:, t0:t0 + Tt]), reads=ydkeys, writes=["py0"])
        P.dma("act", lambda e: e.dma_start(out=y1[:, :Tt], in_=yd[1][:, t0:t0 + Tt]), reads=ydkeys, writes=["py1"])
        P.dma("sp", lambda e: e.dma_start(out=xs_[:, :Tt], in_=fm["xs"][:, t0:t0 + Tt]), reads=scr_keys, writes=["pxs"])
        P.dma("act", lambda e: e.dma_start(out=z_[:, :Tt], in_=zT[:, t0:t0 + Tt]), writes=["pz"])
        P.op("dve", lambda e: e.tensor_tensor(out=y0[:, :Tt], in0=y0[:, :Tt], in1=y1[:, :Tt], op=ALU.add), reads=["py0", "py1"], writes=["py0"])
        P.op("dve", lambda e: e.scalar_tensor_tensor(out=y0[:, :Tt], in0=xs_[:, :Tt], scalar=Dps[:, 0:1], in1=y0[:, :Tt], op0=ALU.mult, op1=ALU.add), reads=["py0", "pxs", "Dps"], writes=["py0"])
        P.op("act", lambda e: e.activation(out=z_[:, :Tt], in_=z_[:, :Tt], func=AF.Silu), reads=["pz"], writes=["pz"])
        P.op("dve", lambda e: e.tensor_tensor(out=y0[:, :Tt], in0=y0[:, :Tt], in1=z_[:, :Tt], op=ALU.mult), reads=["py0", "pz"], writes=["py0"])
        key = ("out", t0); outs.append(key)
        P.dma("sp", lambda e: e.dma_start(out=yT[:, t0:t0 + Tt], in_=y0[:, :Tt]), reads=["py0"], writes=[key])

    for (t0_, Tt_, _) in token_tiles(segs, TT):
        post_tile(t0_, Tt_)
    P.finish_wait("sp", outs)
    P.emit()
    return nc


def build_hyena(S, need_ctx=True):
    nc = new_nc()
    TOT = 2 * S + 512
    din = lambda name, shp: nc.dram_tensor(name, shp, F32, kind="ExternalInput").ap()
    uH = din("uH", [384, TOT]); cw = din("cw", [128, 3, 4]); hd = din("hd", [128, 1]); zL = din("zL", [33, S]); zC = din("zC", [33, 256])
    w1 = din("w1", [33, 64]); w2 = din("w2", [64, 64]); w3 = din("w3", [64, 256]); fp = din("fp", [64, 4]); winL = din("winL", [128, S]); winC = din("winC", [128, 256])
    yT = nc.dram_tensor("yT", [128, TOT], F32, kind="ExternalOutput").ap()
    P = Prog(nc)
    P.setup_sems()
    cws = P.sb("cws", [128, 3, 4]); hds = P.sb("hds", [128, 1]); w1s = P.sb("w1s", [33, 64]); w2s = P.sb("w2s", [64, 64]); w3s = P.sb("w3s", [64, 256]); fps = P.sb("fps", [64, 6])
    for (t, src, n) in ((cws, cw, "cws"), (hds, hd, "hds"), (w1s, w1, "w1s"), (w2s, w2, "w2s"), (w3s, w3, "w3s")):
        P.dma("sp", lambda e, t=t, src=src: e.dma_start(out=t[:], in_=src), writes=[n])
    P.dma("sp", lambda e: e.dma_start(out=fps[:, 0:4], in_=fp), writes=["fps"])
    P.op("dve", lambda e: e.tensor_tensor(out=fps[:, 3:4], in0=fps[:, 0:1], in1=fps[:, 2:3], op=ALU.mult), reads=["fps"], writes=["fps"])
    P.op("dve", lambda e: e.tensor_tensor(out=fps[:, 4:5], in0=fps[:, 1:2], in1=fps[:, 2:3], op=ALU.mult), reads=["fps"], writes=["fps"])
    P.op("pool", lambda e: e.memset(fps[:, 5:6], -float(np.pi)), reads=["fps"], writes=["fps"])
    hf = P.sb("hf", [128, S]); hb = P.sb("hb", [128, S]); vv = P.sb("vv", [128, S]); yA = P.sb("yA", [128, S]); yB = P.sb("yB", [128, S])
    zt = P.sb("zt", [33, 512]); h1 = P.sb("h1", [64, 512]); h2 = P.sb("h2", [64, 512]); wn = P.sb("wn", [128, 512])
    raw = P.sb("raw", [128, 514]); x0 = P.sb("x0", [128, 512]); x1 = P.sb("x1", [128, 512])
    rr = P.sb("rr", [64, 512]); ri = P.sb("ri", [64, 512], I32)
    pA = P.ps("pA", [128, 512]); pB = P.ps("pB", [128, 512])
    TWO_PI = float(2 * np.pi)

    def sin_layer(ws, wname, src, sname, krows, dst, dname, fcol, Tt):
        P.op("pe", lambda e: e.matmul(pA[:64, :Tt], lhsT=ws[:krows, :], rhs=src[:krows, :Tt], start=True, stop=True), reads=[wname, sname], writes=["pA"])
        P.op("dve", lambda e: e.tensor_scalar(out=dst[:, :Tt], in0=pA[:64, :Tt], scalar1=fps[:, 2:3], scalar2=fps[:, fcol:fcol + 1], op0=ALU.mult, op1=ALU.add), reads=["pA", "fps"], writes=[dname])
        P.op("dve", lambda e: e.tensor_scalar(out=rr[:, :Tt], in0=dst[:, :Tt], scalar1=float(1.0 / TWO_PI), scalar2=16.5, op0=ALU.mult, op1=ALU.add), reads=[dname], writes=["rr"])
        P.op("dve", lambda e: e.tensor_copy(out=ri[:, :Tt], in_=rr[:, :Tt]), reads=["rr"], writes=["ri"])
        P.op("dve", lambda e: e.tensor_copy(out=rr[:, :Tt], in_=ri[:, :Tt]), reads=["ri"], writes=["rr"])
        P.op("dve", lambda e: e.tensor_scalar(out=rr[:, :Tt], in0=rr[:, :Tt], scalar1=-16.0, scalar2=-TWO_PI, op0=ALU.add, op1=ALU.mult), reads=["rr"], writes=["rr"])
        P.op("dve", lambda e: e.tensor_tensor(out=dst[:, :Tt], in0=dst[:, :Tt], in1=rr[:, :Tt], op=ALU.add), reads=[dname, "rr"], writes=[dname])
        P.op("dve", lambda e: e.tensor_single_scalar(out=rr[:, :Tt], in_=dst[:, :Tt], scalar=float(np.pi), op=ALU.is_gt), reads=[dname], writes=["rr"])
        P.op("dve", lambda e: e.scalar_tensor_tensor(out=dst[:, :Tt], in0=rr[:, :Tt], scalar=-TWO_PI, in1=dst[:, :Tt], op0=ALU.mult, op1=ALU.add), reads=[dname, "rr"], writes=[dname])
        P.op("dve", lambda e: e.tensor_single_scalar(out=rr[:, :Tt], in_=dst[:, :Tt], scalar=-float(np.pi), op=ALU.is_lt), reads=[dname], writes=["rr"])
        P.op("dve", lambda e: e.scalar_tensor_tensor(out=dst[:, :Tt], in0=rr[:, :Tt], scalar=TWO_PI, in1=dst[:, :Tt], op0=ALU.mult, op1=ALU.add), reads=[dname, "rr"], writes=[dname])
        P.op("act", lambda e: e.activation(out=dst[:, :Tt], in_=dst[:, :Tt], func=AF.Sin), reads=[dname], writes=[dname])

    def filters(zsrc, win, n):
        for t0 in range(0, n, 512):
            Tt = min(512, n - t0)

            def body(t0=t0, Tt=Tt):
                P.dma("sp", lambda e: e.dma_start(out=zt[:, :Tt], in_=zsrc[:, t0:t0 + Tt]), writes=["zt"])
                P.dma("act", lambda e: e.dma_start(out=wn[:, :Tt], in_=win[:, t0:t0 + Tt]), writes=["wn"])
                sin_layer(w1s, "w1s", zt, "zt", 33, h1, "h1", 3, Tt)
                sin_layer(w2s, "w2s", h1, "h1", 64, h2, "h2", 4, Tt)
                for side, (dst, dn) in enumerate(((hf, "hf"), (hb, "hb"))):
                    P.op("pe", lambda e, side=side: e.matmul(pB[:, :Tt], lhsT=w3s[:, side * 128:(side + 1) * 128], rhs=h2[:, :Tt], start=True, stop=True), reads=["w3s", "h2"], writes=["pB"])
                    P.op("dve", lambda e, dst=dst: e.tensor_tensor(out=dst[:, t0:t0 + Tt], in0=pB[:, :Tt], in1=wn[:, :Tt], op=ALU.mult), reads=["pB", "wn"], writes=[dn])
            body()

    outs = []

    def run_seq(seg0, n):
        for t0 in range(0, n, 512):
            Tt = min(512, n - t0)

            def body(t0=t0, Tt=Tt):
                g0 = seg0 + t0
                load_halo(P, "sp", raw, "raw", uH, 256, 128, seg0, n, g0, Tt)
                conv3_tile(P, raw, "raw", x0, "x0", cws, "cws", 2, 128, Tt, False)
                load_halo(P, "act", raw, "raw", uH, 128, 128, seg0, n, g0, Tt)
                conv3_tile(P, raw, "raw", x1, "x1", cws, "cws", 1, 128, Tt, False)
                P.op("dve", lambda e: e.tensor_tensor(out=vv[:, t0:t0 + Tt], in0=x0[:, :Tt], in1=x1[:, :Tt], op=ALU.mult), reads=["x0", "x1"], writes=["vv"])
            body()
        P.op("dve", lambda e: e.tensor_scalar(out=yA[:, :n], in0=vv[:, :n], scalar1=hds[:, 0:1], scalar2=None, op0=ALU.mult), reads=["vv", "hds"], writes=["yA"])
        P.op("pool", lambda e: e.memset(yB[:, :n], 0.0), writes=["yB"])
        k = 0
        for m in range(0, n):
            for side in range(2):
                if side == 1 and m == 0:
                    continue
                eng, acc, an = ("dve", yA, "yA")
                k += 1
                if side == 0:
                    P.op(eng, lambda e, m=m, acc=acc: e.scalar_tensor_tensor(out=acc[:, m:n], in0=vv[:, 0:n - m], scalar=hf[:, m:m + 1], in1=acc[:, m:n], op0=ALU.mult, op1=ALU.add),
                         reads=["vv", "hf", an], writes=[an])
                else:
                    P.op(eng, lambda e, m=m, acc=acc: e.scalar_tensor_tensor(out=acc[:, 0:n - m], in0=vv[:, m:n], scalar=hb[:, m:m + 1], in1=acc[:, 0:n - m], op0=ALU.mult, op1=ALU.add),
                         reads=["vv", "hb", an], writes=[an])
        P.op("dve", lambda e: e.tensor_tensor(out=yA[:, :n], in0=yA[:, :n], in1=yB[:, :n], op=ALU.add), reads=["yA", "yB"], writes=["yA"])
        for t0 in range(0, n, 512):
            Tt = min(512, n - t0)

            def body2(t0=t0, Tt=Tt):
                g0 = seg0 + t0
                load_halo(P, "sp", raw, "raw", uH, 0, 128, seg0, n, g0, Tt)
                conv3_tile(P, raw, "raw", x0, "x0", cws, "cws", 0, 128, Tt, False)
                P.op("dve", lambda e: e.tensor_tensor(out=x0[:, :Tt], in0=x0[:, :Tt], in1=yA[:, t0:t0 + Tt], op=ALU.mult), reads=["x0", "yA"], writes=["x0"])
                key = ("out", g0); outs.append(key)
                P.dma("sp", lambda e: e.dma_start(out=yT[:, g0:g0 + Tt], in_=x0[:, :Tt]), reads=["x0"], writes=[key])
            body2()

    filters(zL, winL, S)
    run_seq(0, S)
    run_seq(S, S)
    if need_ctx:
        filters(zC, winC, 256)
        run_seq(2 * S, 256)
        run_seq(2 * S + 256, 256)
    P.finish_wait("sp", outs)
    P.emit()
    return nc


ALPHA_DN = float((2 * 2) ** 0.25)


def ln_affine(P, C, src, skeys, dst, dkeys, Tt, bias_fn, scale_fn, pkeys, tmp, pst):
    ones = C["ones"]
    sq, mean, var, rstd = tmp["sq"], tmp["mean"], tmp["var"], tmp["rstd"]
    P.op("act", lambda e: e.activation(out=sq[:, :, :Tt], in_=src[:, :, :Tt], func=AF.Square), reads=skeys, writes=["sq"])
    for c in range(KC):
        P.op("pe", lambda e, c=c: e.matmul(pst[0][:, :Tt], lhsT=ones[:], rhs=src[:, c, :Tt], start=(c == 0), stop=(c == KC - 1)), reads=[skeys[c], "c_ones"], writes=["pst0"])
    for c in range(KC):
        P.op("pe", lambda e, c=c: e.matmul(pst[1][:, :Tt], lhsT=ones[:], rhs=sq[:, c, :Tt], start=(c == 0), stop=(c == KC - 1)), reads=["sq", "c_ones"], writes=["pst1"])
    P.op("act", lambda e: e.mul(out=mean[:, :Tt], in_=pst[0][:, :Tt], mul=1.0 / D), reads=["pst0"], writes=["mean"])
    P.op("dve", lambda e: e.tensor_tensor(out=var[:, :Tt], in0=mean[:, :Tt], in1=mean[:, :Tt], op=ALU.mult), reads=["mean"], writes=["var"])
    P.op("dve", lambda e: e.scalar_tensor_tensor(out=var[:, :Tt], in0=pst[1][:, :Tt], scalar=1.0 / D, in1=var[:, :Tt], op0=ALU.mult, op1=ALU.subtract), reads=["pst1", "var"], writes=["var"])
    rsqrt_op(P, C, rstd[:, :Tt], "rstd", var[:, :Tt], "var", "eps_ln")
    for c in range(KC):
        P.op("dve", lambda e, c=c: e.tensor_tensor(out=dst[:, c, :Tt], in0=src[:, c, :Tt], in1=mean[:, :Tt], op=ALU.subtract), reads=[skeys[c], "mean"], writes=[dkeys[c]])
        P.op("pool", lambda e, c=c: e.tensor_tensor(out=dst[:, c, :Tt], in0=dst[:, c, :Tt], in1=rstd[:, :Tt], op=ALU.mult), reads=[dkeys[c], "rstd"], writes=[dkeys[c]])
        P.op("act", lambda e, c=c: e.activation(out=dst[:, c, :Tt], in_=dst[:, c, :Tt], func=AF.Identity, bias=bias_fn(c), scale=scale_fn(c)), reads=[dkeys[c]] + pkeys, writes=[dkeys[c]])


def build_stageD(Tc, segs, TT=256):
    nc = new_nc()
    nseg = len(segs)
    din = lambda name, shp: nc.dram_tensor(name, shp, F32, kind="ExternalInput").ap()
    xT = din("xT", [D, Tc]); ybT = din("ybT", [4, 1024, Tc]); modD = din("modD", [128, KC, nseg, 5]); lnp = din("lnp", [128, KC, 2]); ssg = din("ssg", [128, 8])
    Wg = din("Wg", [D, 8192]); Wbr = din("Wbr", [4, 1024, D]); Wo = din("Wo", [D, D]); Wr = din("Wr", [D, 16])
    xl1T = nc.dram_tensor("xl1T", [D, Tc], F32, kind="ExternalOutput").ap()
    affT = nc.dram_tensor("affT", [16, Tc], F32, kind="ExternalOutput").ap()
    P = Prog(nc)
    P.setup_sems()
    C = consts(P)
    ones = C["ones"]
    tmp = {"sq": P.sb("sq", [128, KC, TT]), "mean": P.sb("mean", [128, TT]), "var": P.sb("var", [128, TT]), "rstd": P.sb("rstd", [128, TT])}
    pst = [P.ps("pst0", [128, TT]), P.ps("pst1", [128, TT])]
    mods = P.sb("mods", [128, KC, nseg, 5]); lns = P.sb("lns", [128, KC, 2]); ssgs = P.sb("ssgs", [128, 8]); wrs = P.sb("wrs", [128, KC, 16])
    P.dma("sp", lambda e: e.dma_start(out=mods[:], in_=modD), writes=["mods"])
    P.dma("sp", lambda e: e.dma_start(out=lns[:], in_=lnp), writes=["lns"])
    P.dma("sp", lambda e: e.dma_start(out=ssgs[:], in_=ssg), writes=["ssgs"])
    P.dma("sp", lambda e: e.dma_start(out=wrs[:], in_=Wr.rearrange("(c p) n -> p c n", p=128)), writes=["wrs"])
    for col in (1, 4):
        P.op("dve", lambda e, col=col: e.tensor_scalar(out=mods[:, :, :, col:col + 1], in0=mods[:, :, :, col:col + 1], scalar1=1.0, scalar2=None, op0=ALU.add), reads=["mods"], writes=["mods"])
    xs = P.sb("xs", [128, KC, TT]); hs = P.sb("hs", [128, KC, TT]); mg = P.sb("mg", [128, KC, TT])
    yb = [P.sb("yb%d" % i, [128, 8, TT]) for i in range(2)]
    wgt = [P.sb("wgt%d" % i, [128, KC, 128]) for i in range(2)]
    wbt = [P.sb("wbt%d" % i, [128, 8, 128]) for i in range(2)]
    gt = P.sb("gt", [128, TT]); tm_ = P.sb("tm_", [128, TT]); ex = P.sb("ex", [16, TT])
    pg = [P.ps("pg%d" % i, [128, TT]) for i in range(2)]
    pb = [P.ps("pb%d" % i, [128, TT]) for i in range(2)]
    xk = [("xs", c) for c in range(KC)]; hk = [("hs", c) for c in range(KC)]; mk_ = [("mg", c) for c in range(KC)]
    xv = xT.rearrange("(c p) t -> p c t", p=128)
    wgv = Wg.rearrange("(c p) n -> p c n", p=128)
    wov = Wo.rearrange("(c p) n -> p c n", p=128)
    outs = []
    cnt = [0]

    def tile_body(t0, Tt, sid):
        P.dma("sp", lambda e: e.dma_start(out=xs[:, :, :Tt], in_=xv[:, :, t0:t0 + Tt]), writes=xk)
        ln_affine(P, C, xs, xk, hs, hk, Tt, lambda c: mods[:, c, sid, 0:1], lambda c: mods[:, c, sid, 1:2], ["mods"], tmp, pst)
        for i in range(4):
            ybi = yb[i % 2]; ybn = "yb%d" % (i % 2)
            P.dma("act", lambda e, i=i, ybi=ybi: e.dma_start(out=ybi[:, :, :Tt], in_=ybT[i].rearrange("(c p) t -> p c t", p=128)[:, :, t0:t0 + Tt]), writes=[ybn])
            if i == 1:
                sq = tmp["sq"]
                P.op("act", lambda e, ybi=ybi: e.activation(out=sq[:, 0:8, :Tt], in_=ybi[:, :, :Tt], func=AF.Square), reads=[ybn], writes=["sq"])
                for g in range(2):
                    for c in range(4):
                        P.op("pe", lambda e, g=g, c=c: e.matmul(pst[g][:, :Tt], lhsT=ones[:], rhs=sq[:, 4 * g + c, :Tt], start=(c == 0), stop=(c == 3)), reads=["sq", "c_ones"], writes=["pst%d" % g])
                    rs = tmp["mean"] if g == 0 else tmp["var"]
                    rsn = "mean" if g == 0 else "var"
                    rsqrt_op(P, C, rs[:, :Tt], rsn, pst[g][:, :Tt], "pst%d" % g, "eps_1e5", scale=1.0 / 512)
                    for c in range(4):
                        P.op("dve", lambda e, g=g, c=c, rs=rs, ybi=ybi: e.scalar_tensor_tensor(out=ybi[:, 4 * g + c, :Tt], in0=ybi[:, 4 * g + c, :Tt], scalar=ssgs[:, 4 * g + c:4 * g + c + 1], in1=rs[:, :Tt], op0=ALU.mult, op1=ALU.mult),
                             reads=[ybn, rsn, "ssgs"], writes=[ybn])
            for dt in range(KC):
                b = cnt[0] % 2
                cnt[0] += 1
                col = i * D + dt * 128
                P.dma("sp", lambda e, b=b, col=col: e.dma_start(out=wgt[b][:], in_=wgv[:, :, col:col + 128]), writes=["wgt%d" % b])
                P.dma("pool", lambda e, b=b, i=i, dt=dt: e.dma_start(out=wbt[b][:], in_=Wbr[i].rearrange("(c p) n -> p c n", p=128)[:, :, dt * 128:(dt + 1) * 128]), writes=["wbt%d" % b])
                for c in range(KC):
                    P.op("pe", lambda e, b=b, c=c: e.matmul(pg[b][:, :Tt], lhsT=wgt[b][:, c, :], rhs=hs[:, c, :Tt], start=(c == 0), stop=(c == KC - 1)), reads=["wgt%d" % b, hk[c]], writes=["pg%d" % b])
                for c in range(8):
                    P.op("pe", lambda e, b=b, c=c, ybi=ybi: e.matmul(pb[b][:, :Tt], lhsT=wbt[b][:, c, :], rhs=ybi[:, c, :Tt], start=(c == 0), stop=(c == 7)), reads=["wbt%d" % b, ybn], writes=["pb%d" % b])
                P.op("act", lambda e, b=b: e.activation(out=gt[:, :Tt], in_=pg[b][:, :Tt], func=AF.Sigmoid), reads=["pg%d" % b], writes=["gt"])
                if i == 0:
                    P.op("dve", lambda e, b=b, dt=dt: e.tensor_tensor(out=mg[:, dt, :Tt], in0=gt[:, :Tt], in1=pb[b][:, :Tt], op=ALU.mult), reads=["gt", "pb%d" % b], writes=[mk_[dt]])
                else:
                    P.op("dve", lambda e, b=b: e.tensor_tensor(out=tm_[:, :Tt], in0=gt[:, :Tt], in1=pb[b][:, :Tt], op=ALU.mult), reads=["gt", "pb%d" % b], writes=["tm_"])
                    P.op("pool", lambda e, dt=dt: e.tensor_tensor(out=mg[:, dt, :Tt], in0=mg[:, dt, :Tt], in1=tm_[:, :Tt], op=ALU.add), reads=["tm_", mk_[dt]], writes=[mk_[dt]])
        for dt in range(KC):
            b = cnt[0] % 2
            cnt[0] += 1
            P.dma("sp", lambda e, b=b, dt=dt: e.dma_start(out=wgt[b][:], in_=wov[:, :, dt * 128:(dt + 1) * 128]), writes=["wgt%d" % b])
            for c in range(KC):
                P.op("pe", lambda e, b=b, c=c: e.matmul(pg[b][:, :Tt], lhsT=wgt[b][:, c, :], rhs=mg[:, c, :Tt], start=(c == 0), stop=(c == KC - 1)), reads=["wgt%d" % b, mk_[c]], writes=["pg%d" % b])
            P.op("dve", lambda e, b=b, dt=dt: e.tensor_scalar(out=tm_[:, :Tt], in0=pg[b][:, :Tt], scalar1=mods[:, dt, sid, 2:3], scalar2=None, op0=ALU.mult), reads=["pg%d" % b, "mods"], writes=["tm_"])
            P.op("dve", lambda e, dt=dt: e.scalar_tensor_tensor(out=hs[:, dt, :Tt], in0=xs[:, dt, :Tt], scalar=ALPHA_DN, in1=tm_[:, :Tt], op0=ALU.mult, op1=ALU.add), reads=["tm_", xk[dt]], writes=[hk[dt]])
        ln_affine(P, C, hs, hk, xs, xk, Tt, lambda c: lns[:, c, 0:1], lambda c: lns[:, c, 1:2], ["lns"], tmp, pst)
        key = ("xl1", t0); outs.append(key)
        P.dma("sp", lambda e: e.dma_start(out=xl1T.rearrange("(c p) t -> p c t", p=128)[:, :, t0:t0 + Tt], in_=xs[:, :, :Tt]), reads=xk, writes=[key])
        ln_affine(P, C, xs, xk, hs, hk, Tt, lambda c: mods[:, c, sid, 3:4], lambda c: mods[:, c, sid, 4:5], ["mods"], tmp, pst)
        for c in range(KC):
            P.op("pe", lambda e, c=c: e.matmul(pg[0][:16, :Tt], lhsT=wrs[:, c, :], rhs=hs[:, c, :Tt], start=(c == 0), stop=(c == KC - 1)), reads=["wrs", hk[c]], writes=["pg0"])
        P.op("act", lambda e: e.activation(out=ex[:, :Tt], in_=pg[0][:16, :Tt], func=AF.Exp), reads=["pg0"], writes=["ex"])
        P.op("pe", lambda e: e.matmul(pb[0][:16, :Tt], lhsT=ones[:16, :16], rhs=ex[:, :Tt], start=True, stop=True), reads=["ex", "c_ones"], writes=["pb0"])
        P.op("dve", lambda e: e.reciprocal(out=gt[:16, :Tt], in_=pb[0][:16, :Tt]), reads=["pb0"], writes=["gt"])
        P.op("dve", lambda e: e.tensor_tensor(out=ex[:, :Tt], in0=ex[:, :Tt], in1=gt[:16, :Tt], op=ALU.mult), reads=["ex", "gt"], writes=["ex"])
        key = ("aff", t0); outs.append(key)
        P.dma("sp", lambda e: e.dma_start(out=affT[:, t0:t0 + Tt], in_=ex[:, :Tt]), reads=["ex"], writes=[key])

    for (t0_, Tt_, sid_) in token_tiles(segs, TT):
        tile_body(t0_, Tt_, sid_)
    P.finish_wait("sp", outs)
    P.emit()
    return nc


def build_stageE(Tc, segs, S, NE=16, TT=256):
    nc = new_nc()
    nseg = len(segs)
    FF = 1408
    FC = FF // 128
    din = lambda name, shp: nc.dram_tensor(name, shp, F32, kind="ExternalInput").ap()
    xl1T = din("xl1T", [D, Tc]); modE = din("modE", [128, KC, nseg, 3]); lnp = din("lnp", [128, KC, 2])
    affL = din("affL", [16, S]); affC = din("affC", [16, 256]); affown = din("affown", [16, Tc]); sel = din("sel", [16, 16, 128])
    Wge = din("Wge", [NE, D, FF]); Wue = din("Wue", [NE, D, FF]); Wde = din("Wde", [NE, FF, D])
    xoT = nc.dram_tensor("xoT", [D, Tc], F32, kind="ExternalOutput").ap()
    P = Prog(nc)
    P.setup_sems()
    C = consts(P)
    tmp = {"sq": P.sb("sq", [128, KC, TT]), "mean": P.sb("mean", [128, TT]), "var": P.sb("var", [128, TT]), "rstd": P.sb("rstd", [128, TT])}
    pst = [P.ps("pst0", [128, TT]), P.ps("pst1", [128, TT])]
    mods = P.sb("mods", [128, KC, nseg, 3]); lns = P.sb("lns", [128, KC, 2]); sels = P.sb("sels", [16, 16, 128])
    P.dma("sp", lambda e: e.dma_start(out=mods[:], in_=modE), writes=["mods"])
    P.dma("sp", lambda e: e.dma_start(out=lns[:], in_=lnp), writes=["lns"])
    P.dma("sp", lambda e: e.dma_start(out=sels[:], in_=sel), writes=["sels"])
    P.op("dve", lambda e: e.tensor_scalar(out=mods[:, :, :, 1:2], in0=mods[:, :, :, 1:2], scalar1=1.0, scalar2=None, op0=ALU.add), reads=["mods"], writes=["mods"])
    work = P.sb("work", [16, S]); mx = P.sb("mx", [16, 8]); thr = P.sb("thr", [16, 2]); wown = P.sb("wown", [16, Tc]); msk = P.sb("msk", [16, Tc])

    def threshold(src, n, col):
        cap = n // 8
        P.dma("sp", lambda e: e.dma_start(out=work[:, :n], in_=src), writes=["work"])
        for it in range(cap // 8):
            P.op("dve", lambda e: e.max(out=mx[:], in_=work[:, :n]), reads=["work"], writes=["mx"])
            if it < cap // 8 - 1:
                P.op("dve", lambda e: e.match_replace(out=work[:, :n], in_to_replace=mx[:], in_values=work[:, :n], imm_value=-1.0), reads=["mx", "work"], writes=["work"])
        P.op("dve", lambda e: e.tensor_reduce(out=thr[:, col:col + 1], in_=mx[:], axis=AX.X, op=ALU.min), reads=["mx"], writes=["thr"])

    threshold(affL, S, 0)
    threshold(affC, 256, 1)
    P.dma("sp", lambda e: e.dma_start(out=wown[:], in_=affown), writes=["wown"])
    for (s0, sn, sid) in segs:
        P.op("dve", lambda e, s0=s0, sn=sn, sid=sid: e.tensor_scalar(out=msk[:, s0:s0 + sn], in0=wown[:, s0:s0 + sn], scalar1=thr[:, sid:sid + 1], scalar2=None, op0=ALU.is_ge), reads=["wown", "thr"], writes=["msk"])
    P.op("dve", lambda e: e.tensor_tensor(out=wown[:], in0=wown[:], in1=msk[:], op=ALU.mult), reads=["wown", "msk"], writes=["wown"])

    xs = P.sb("xs", [128, KC, TT]); hs = P.sb("hs", [128, KC, TT]); acc = P.sb("acc", [128, KC, TT]); hid = P.sb("hid", [128, FC, TT])
    wg = [P.sb("wg%d" % i, [128, KC, 128]) for i in range(2)]; wu = [P.sb("wu%d" % i, [128, KC, 128]) for i in range(2)]; wd = [P.sb("wd%d" % i, [128, FC, 128]) for i in range(2)]
    wbc = P.sb("wbc", [128, TT]); sg = P.sb("sg", [128, TT]); tm_ = P.sb("tm_", [128, TT])
    pg = P.ps("pg", [128, TT]); pu = P.ps("pu", [128, TT]); pd = [P.ps("pd%d" % i, [128, TT]) for i in range(2)]; pw = P.ps("pw", [128, TT])
    xk = [("xs", c) for c in range(KC)]; hk = [("hs", c) for c in range(KC)]; ak = [("acc", c) for c in range(KC)]; hidk = [("hid", f) for f in range(FC)]
    xv = xl1T.rearrange("(c p) t -> p c t", p=128)
    outs = []
    cnt = [0, 0]

    def tile_body(t0, Tt, sid):
        P.dma("sp", lambda e: e.dma_start(out=xs[:, :, :Tt], in_=xv[:, :, t0:t0 + Tt]), writes=xk)
        ln_affine(P, C, xs, xk, hs, hk, Tt, lambda c: mods[:, c, sid, 0:1], lambda c: mods[:, c, sid, 1:2], ["mods"], tmp, pst)
        for ex in range(NE):
            P.op("pe", lambda e, ex=ex: e.matmul(pw[:, :Tt], lhsT=sels[:, ex, :], rhs=wown[:, t0:t0 + Tt], start=True, stop=True), reads=["sels", "wown"], writes=["pw"])
            P.op("act", lambda e: e.copy(out=wbc[:, :Tt], in_=pw[:, :Tt]), reads=["pw"], writes=["wbc"])
            wgv = Wge[ex].rearrange("(c p) f -> p c f", p=128); wuv = Wue[ex].rearrange("(c p) f -> p c f", p=128); wdv = Wde[ex].rearrange("(c p) d -> p c d", p=128)
            for f in range(FC):
                b = cnt[0] % 2
                cnt[0] += 1
                P.dma("sp", lambda e, b=b, f=f, wgv=wgv: e.dma_start(out=wg[b][:], in_=wgv[:, :, f * 128:(f + 1) * 128]), writes=["wg%d" % b])
                P.dma("act", lambda e, b=b, f=f, wuv=wuv: e.dma_start(out=wu[b][:], in_=wuv[:, :, f * 128:(f + 1) * 128]), writes=["wu%d" % b])
                for c in range(KC):
                    P.op("pe", lambda e, b=b, c=c: e.matmul(pg[:, :Tt], lhsT=wg[b][:, c, :], rhs=hs[:, c, :Tt], start=(c == 0), stop=(c == KC - 1)), reads=["wg%d" % b, hk[c]], writes=["pg"])
                for c in range(KC):
                    P.op("pe", lambda e, b=b, c=c: e.matmul(pu[:, :Tt], lhsT=wu[b][:, c, :], rhs=hs[:, c, :Tt], start=(c == 0), stop=(c == KC - 1)), reads=["wu%d" % b, hk[c]], writes=["pu"])
                P.op("act", lambda e: e.activation(out=sg[:, :Tt], in_=pg[:, :Tt], func=AF.Silu), reads=["pg"], writes=["sg"])
                P.op("dve", lambda e: e.tensor_tensor(out=sg[:, :Tt], in0=sg[:, :Tt], in1=pu[:, :Tt], op=ALU.mult), reads=["sg", "pu"], writes=["sg"])
                P.op("pool", lambda e, f=f: e.tensor_tensor(out=hid[:, f, :Tt], in0=sg[:, :Tt], in1=wbc[:, :Tt], op=ALU.mult), reads=["sg", "wbc"], writes=[hidk[f]])
            for dt in range(KC):
                b = cnt[1] % 2
                cnt[1] += 1
                P.dma("pool", lambda e, b=b, dt=dt, wdv=wdv: e.dma_start(out=wd[b][:], in_=wdv[:, :, dt * 128:(dt + 1) * 128]), writes=["wd%d" % b])
                for f in range(FC):
                    P.op("pe", lambda e, b=b, f=f: e.matmul(pd[b][:, :Tt], lhsT=wd[b][:, f, :], rhs=hid[:, f, :Tt], start=(f == 0), stop=(f == FC - 1)), reads=["wd%d" % b, hidk[f]], writes=["pd%d" % b])
                if ex == 0:
                    P.op("dve", lambda e, b=b, dt=dt: e.tensor_copy(out=acc[:, dt, :Tt], in_=pd[b][:, :Tt]), reads=["pd%d" % b], writes=[ak[dt]])
                else:
                    P.op("dve", lambda e, b=b, dt=dt: e.tensor_tensor(out=acc[:, dt, :Tt], in0=acc[:, dt, :Tt], in1=pd[b][:, :Tt], op=ALU.add), reads=["pd%d" % b, ak[dt]], writes=[ak[dt]])
        for dt in range(KC):
            P.op("dve", lambda e, dt=dt: e.tensor_scalar(out=tm_[:, :Tt], in0=acc[:, dt, :Tt], scalar1=mods[:, dt, sid, 2:3], scalar2=None, op0=ALU.mult), reads=[ak[dt], "mods"], writes=["tm_"])
            P.op("dve", lambda e, dt=dt: e.scalar_tensor_tensor(out=hs[:, dt, :Tt], in0=xs[:, dt, :Tt], scalar=ALPHA_DN, in1=tm_[:, :Tt], op0=ALU.mult, op1=ALU.add), reads=["tm_", xk[dt]], writes=[hk[dt]])
        ln_affine(P, C, hs, hk, xs, xk, Tt, lambda c: lns[:, c, 0:1], lambda c: lns[:, c, 1:2], ["lns"], tmp, pst)
        key = ("xo", t0); outs.append(key)
        P.dma("sp", lambda e: e.dma_start(out=xoT.rearrange("(c p) t -> p c t", p=128)[:, :, t0:t0 + Tt], in_=xs[:, :, :Tt]), reads=xk, writes=[key])

    for (t0_, Tt_, sid_) in token_tiles(segs, TT):
        tile_body(t0_, Tt_, sid_)
    P.finish_wait("sp", outs)
    P.emit()
    return nc


SEQ = 8192
CTX = 256
MLA_COLS, SSM_COLS, HY_COLS, RW_COLS = 1088, 2592, 3072, 3488
N_CORES = 8


def _c(a):
    return np.ascontiguousarray(a, dtype=np.float32)


def _run(nc, in_maps):
    return run_bass_kernel_spmd(nc, in_maps, core_ids=list(range(N_CORES))).results


def _stageA(c, c_ctx, w_ada, b_ada):
    ncols = 2 * 12288 // N_CORES
    cT = _c(np.stack([c[0], c[1], c_ctx], 1))
    maps = []
    for k in range(N_CORES):
        sl = slice(k * 1536, (k + 1) * 1536)
        wA = np.concatenate([w_ada[0][:, sl], w_ada[1][:, sl]], 1)
        bA = np.concatenate([b_ada[0][sl], b_ada[1][sl]]).reshape(ncols // 128, 128).T
        maps.append({"cT": cT, "wA": _c(wA), "bA": _c(bA)})
    res = _run(build_stageA(ncols), maps)
    mod = np.zeros((2, 12288, 3), np.float32)
    for k in range(N_CORES):
        m = res[k]["modT"].transpose(1, 0, 2).reshape(ncols, 3)
        mod[0, k * 1536:(k + 1) * 1536] = m[:1536]
        mod[1, k * 1536:(k + 1) * 1536] = m[1536:]
    return mod


_SWAP = np.arange(64).reshape(2, 2, 16)[:, ::-1, :].reshape(64)


def _stageB(x, ctx, mod, w_in_l, S):
    Tl = S // 4
    Tc = Tl + 64
    cols = np.concatenate([np.arange(MLA_COLS), 1024 + _SWAP, np.arange(MLA_COLS, MLA_COLS + SSM_COLS + HY_COLS + RW_COLS)])
    NB = ((len(cols) + 127) // 128) * 128
    Wb = np.zeros((D, NB), np.float32)
    Wb[:, :len(cols)] = w_in_l[:, cols]
    maps = []
    for k in range(N_CORES):
        b, q = k // 4, k % 4
        xT = np.concatenate([x[b, q * Tl:(q + 1) * Tl], ctx[b, q * 64:(q + 1) * 64]], 0).T
        md = np.stack([np.stack([mod[:D, b], mod[D:2 * D, b]], -1), np.stack([mod[:D, 2], mod[D:2 * D, 2]], -1)], 1)
        md = md.reshape(KC, 128, 2, 2).transpose(1, 0, 2, 3)
        maps.append({"xT": _c(xT), "modB": _c(md), "Wb": Wb})
    res = _run(build_stageB(Tc, [(0, Tl, 0), (Tl, 64, 1)], NB), maps)
    TOT = 2 * S + 512
    u = np.zeros((TOT, len(cols)), np.float32)
    for k in range(N_CORES):
        b, q = k // 4, k % 4
        uk = res[k]["uT"].T[:, :len(cols)]
        u[b * S + q * Tl:b * S + (q + 1) * Tl] = uk[:Tl]
        u[2 * S + b * 256 + q * 64:2 * S + b * 256 + (q + 1) * 64] = uk[Tl:]
    return u


def _rope_tables(S):
    rows = S // 64
    row = np.repeat(np.arange(rows), 64).astype(np.float32)
    col = np.tile(np.arange(64), rows).astype(np.float32)
    inv = (10000.0 ** (-np.arange(0, 32, 2, dtype=np.float32) / 32)).astype(np.float32)
    ang = np.stack([row[:, None] * inv, col[:, None] * inv], 1)
    cos, sin = np.cos(ang).astype(np.float32), np.sin(ang).astype(np.float32)
    cosT = np.stack([cos, cos], 2).reshape(S, 64).T
    sinT = np.stack([-sin, sin], 2).reshape(S, 64).T
    return _c(cosT), _c(sinT)


def _mla_maps(u, p, S, need_ctx):
    um = u[:, :MLA_COLS + 64]
    cosT, sinT = _rope_tables(S)
    wqu = p["mla_w_q_up"].reshape(512, 8, 192)
    wkvu = p["mla_w_kv_up"].reshape(512, 8, 256)
    shared = {"cqT": _c(um[:, :512].T), "ckvT": _c(um[:, 512:1024].T), "kpeT": _c(um[:, 1024:1088].T), "kpeswT": _c(um[:, 1088:1152].T),
              "gq": _c(p["mla_q_norm"].reshape(4, 128).T), "gkv": _c(p["mla_kv_norm"].reshape(4, 128).T), "cosT": cosT, "sinT": sinT}
    maps = []
    for h in range(8):
        wq = np.concatenate([wqu[:, h, :128], wqu[:, h, 128:], wqu[:, h, 128:][:, _SWAP]], 1)
        maps.append(dict(shared, wq=_c(wq), wkv=_c(wkvu[:, h])))
    return maps


def _ssd_maps(u, p, S):
    us = u[:, MLA_COLS + 64:MLA_COLS + 64 + SSM_COLS]
    maps = []
    for core in range(8):
        ch = np.arange(core * 128, (core + 1) * 128)
        g = core // 4
        allc = np.concatenate([1024 + ch, 2048 + g * 128 + np.arange(128), 2304 + g * 128 + np.arange(128)])
        cwf = np.concatenate([p["ssm_conv_w"], p["ssm_conv_b"][None]], 0)[:, allc - 1024]
        cw = cwf.T.reshape(3, 128, 4).transpose(1, 0, 2)
        hs = [2 * core, 2 * core + 1]
        dtc = 2560 + np.array([hs[0], hs[1], 16 + hs[0], 16 + hs[1]])
        sel = ([0, 0, 1, 1], [hs[0], hs[1], hs[0], hs[1]])
        maps.append({"zT": _c(us[:, ch].T), "xbcT": _c(us[:, allc].T), "cw": _c(cw), "dttok": _c(us[:, dtc]),
                     "dtb": _c(np.broadcast_to(p["ssm_dt_bias"][sel], (128, 4))), "alog": _c(np.broadcast_to(p["ssm_a_log"][sel], (128, 4))),
                     "Dp": _c(np.repeat(p["ssm_d"][hs], 64)[:, None]), "ident": np.eye(128, dtype=np.float32), "triU": _c(np.triu(np.ones((128, 128))))})
    return maps


def _hy_consts(n):
    t = np.linspace(0.0, 1.0, n, dtype=np.float32)[:, None]
    wpos = (2.0 * math.pi * np.arange(n, dtype=np.float32)[:, None] / n).astype(np.float32)
    f = np.linspace(1e-4, 15, 16, dtype=np.float32)[None]
    z = np.concatenate([t, np.cos(f * wpos), -np.sin(f * wpos)], -1).astype(np.float32)
    lo, hi = math.log(1e-2) / 1.5, math.log(1e-2) / 0.3
    deltas = np.abs(np.linspace(lo, hi, 1024, dtype=np.float32))
    return _c(z.T), np.exp(-t * deltas).astype(np.float32)


def _hyena_maps(u, p, S, need_ctx):
    c0 = MLA_COLS + 64 + SSM_COLS
    uh = u[:, c0:c0 + HY_COLS]
    zL, winL = _hy_consts(S)
    zC, winC = _hy_consts(256)
    maps = []
    for core in range(8):
        ch = np.arange(core * 128, (core + 1) * 128)
        cols = np.concatenate([ch, 1024 + ch, 2048 + ch])
        cwf = np.concatenate([p["hy_conv_w"], p["hy_conv_b"][None]], 0)[:, cols]
        cw = cwf.T.reshape(3, 128, 4).transpose(1, 0, 2)
        w3c = np.concatenate([p["hy_w3"][:, ch], p["hy_w3"][:, 1024 + ch]], 1)
        fp = np.stack([p["hy_b1"], p["hy_b2"], p["hy_freq"], np.zeros(64, np.float32)], 1)
        maps.append({"uH": _c(uh[:, cols].T), "cw": _c(cw), "hd": _c(p["hy_d"][ch][:, None]), "zL": zL, "zC": zC, "w1": _c(p["hy_w1"]), "w2": _c(p["hy_w2"]),
                     "w3": _c(w3c), "fp": _c(fp), "winL": _c(winL[:, ch].T), "winC": _c(winC[:, ch].T)})
    return maps


def _rwkv_maps(u, p, S):
    c0 = MLA_COLS + 64 + SSM_COLS + HY_COLS
    ur = u[:, c0:c0 + RW_COLS]
    blk = np.kron(np.eye(2, dtype=np.float32), np.ones((64, 64), np.float32))
    maps = []
    for core in range(8):
        ch = slice(core * 128, (core + 1) * 128)
        cols = np.concatenate([np.arange(1024)[ch], 1024 + np.arange(1024)[ch], 2048 + np.arange(1024)[ch], np.arange(3072, 3488)])
        mup = np.zeros((2, 896), np.float32)
        mup[:, :800] = p["rw_mu"][:, cols]
        pc = np.zeros((128, 16), np.float32)
        pc[:, 0] = p["rw_kk"][ch]; pc[:, 1] = p["rw_ka"][ch]; pc[:, 2] = p["rw_rk"].reshape(-1)[ch]
        pc[:, 3] = p["rw_ln_g"][ch]; pc[:, 4] = p["rw_ln_b"][ch]
        pc[:, 5] = p["rw_w0"][0, ch]; pc[:, 6] = p["rw_w0"][1, ch]; pc[:, 7] = p["rw_a0"][0, ch]; pc[:, 8] = p["rw_a0"][1, ch]
        maps.append({"uR": _c(ur[:, cols].T), "mu": _c(mup.reshape(2, 7, 128).transpose(2, 1, 0)), "pc": pc,
                     "wup": _c(p["rw_w_up"][:, :, ch].reshape(128, 128)), "aup": _c(p["rw_a_up"][:, :, ch].reshape(128, 128)),
                     "gup": _c(p["rw_g_up"][:, ch]), "ident": np.eye(128, dtype=np.float32), "blk": blk})
    return maps


def _mixers(u, p, S):
    parts = (("mla_", _mla_maps(u, p, S, True)), ("ssd_", _ssd_maps(u, p, S)), ("hy_", _hyena_maps(u, p, S, True)), ("rw_", _rwkv_maps(u, p, S)))
    maps = []
    for k in range(N_CORES):
        m = {}
        for prefix, mp in parts:
            for name, arr in mp[k].items():
                m[prefix + name] = arr
        maps.append(m)
    res = _run(build_mixers(S), maps)
    y_mla = np.concatenate([res[h]["mla_o"] for h in range(8)], 1)
    rest = [np.concatenate([res[k][pre + "yT"].T for k in range(8)], 1) for pre in ("ssd_", "hy_", "rw_")]
    return [y_mla] + rest


def _core_tokens(arr_lat, arr_ctx, k, S):
    Tl = S // 4
    b, q = k // 4, k % 4
    return np.concatenate([arr_lat[b, q * Tl:(q + 1) * Tl], arr_ctx[b, q * 64:(q + 1) * 64]], 0)


def _stageD(x, ctx, ys, mod, p, S):
    Tl = S // 4
    Tc = Tl + 64
    lnp = _c(np.stack([p["ln1_b"], p["ln1_g"]], -1).reshape(KC, 128, 2).transpose(1, 0, 2))
    shared = {"lnp": lnp, "ssg": _c(p["ssm_norm"].reshape(8, 128).T), "Wg": _c(p["w_in"][:, 10240:]), "Wbr": _c(p["w_branch"]), "Wo": _c(p["w_out"]), "Wr": _c(p["w_router"])}
    m6 = mod.reshape(6, D, 3)
    maps = []
    for k in range(N_CORES):
        b = k // 4
        xT = _core_tokens(x, ctx, k, S).T
        ybT = np.stack([_core_tokens(y[:2 * S].reshape(2, S, 1024), y[2 * S:].reshape(2, 256, 1024), k, S).T for y in ys], 0)
        md = np.stack([m6[[0, 1, 2, 3, 4], :, b].T, m6[[0, 1, 2, 3, 4], :, 2].T], 1).reshape(KC, 128, 2, 5).transpose(1, 0, 2, 3)
        maps.append(dict(shared, xT=_c(xT), ybT=_c(ybT), modD=_c(md)))
    res = _run(build_stageD(Tc, [(0, Tl, 0), (Tl, 64, 1)]), maps)
    return [res[k]["xl1T"] for k in range(N_CORES)], [res[k]["affT"] for k in range(N_CORES)]


def _stageE(xl1T, affT, mod, p, S):
    Tl = S // 4
    Tc = Tl + 64
    lnp = _c(np.stack([p["ln2_b"], p["ln2_g"]], -1).reshape(KC, 128, 2).transpose(1, 0, 2))
    sel = np.zeros((16, 16, 128), np.float32)
    for e in range(16):
        sel[e, e, :] = 1.0
    shared = {"lnp": lnp, "sel": sel, "Wge": _c(p["w_gate_e"]), "Wue": _c(p["w_up_e"]), "Wde": _c(p["w_down_e"])}
    affL = [np.concatenate([affT[4 * b + q][:, :Tl] for q in range(4)], 1) for b in range(2)]
    affC = [np.concatenate([affT[4 * b + q][:, Tl:] for q in range(4)], 1) for b in range(2)]
    m6 = mod.reshape(6, D, 3)
    maps = []
    for k in range(N_CORES):
        b = k // 4
        md = np.stack([m6[[3, 4, 5], :, b].T, m6[[3, 4, 5], :, 2].T], 1).reshape(KC, 128, 2, 3).transpose(1, 0, 2, 3)
        maps.append(dict(shared, xl1T=_c(xl1T[k]), modE=_c(md), affL=_c(affL[b]), affC=_c(affC[b]), affown=_c(affT[k])))
    res = _run(build_stageE(Tc, [(0, Tl, 0), (Tl, 64, 1)], S), maps)
    x_new = np.zeros((2, S, D), np.float32)
    c_new = np.zeros((2, 256, D), np.float32)
    for k in range(N_CORES):
        b, q = k // 4, k % 4
        o = res[k]["xoT"].T
        x_new[b, q * Tl:(q + 1) * Tl] = o[:Tl]
        c_new[b, q * 64:(q + 1) * 64] = o[Tl:]
    return x_new, c_new


def kernel(**inputs):
    inp = {k: np.asarray(v) for k, v in inputs.items()}
    x, c, ctx, c_ctx = inp["x"], inp["c"], inp["ctx"], inp["c_ctx"]
    S = x.shape[1]
    depth = inp["w_in"].shape[0]
    mod = _stageA(c, c_ctx, inp["w_ada"], inp["b_ada"])
    skip = ("x", "c", "ctx", "c_ctx", "w_ada", "b_ada")
    for layer in range(depth):
        p = {k: v[layer] for k, v in inp.items() if k not in skip}
        u = _stageB(x, ctx, mod[layer], p["w_in"], S)
        ys = _mixers(u, p, S)
        del u
        xl1T, affT = _stageD(x, ctx, ys, mod[layer], p, S)
        del ys
        x, ctx = _stageE(xl1T, affT, mod[layer], p, S)
    return np.ascontiguousarray(x, dtype=np.float32)
```
